# Optimizing a Trainium2 kernel written in Bass

```python
import math
import jax, jax.numpy as jnp
from jax import lax
import numpy as np

D_MODEL = 1024
BATCH = 8
SEQ = 4096
DEPTH = 1

D_MIX = D_MODEL
ATT_WIDTH = D_MIX // 2
RET_WIDTH = D_MIX - ATT_WIDTH
ATT_HEADS = 8
ATT_KV_HEADS = 2
ATT_HEAD_DIM = ATT_WIDTH // ATT_HEADS
IDX_HEADS = 4
IDX_DIM = 64
TOPK_MAX = 256
Q_BLOCK = 128
RET_HEADS = 4
RET_HEAD_DIM = RET_WIDTH // RET_HEADS
RET_CHUNK = 128
ROPE_BASE = 10000.0
N_BUCKETS = 32
MAX_DISTANCE = 128
N_EXPERTS = 32
TOP_K = 4
D_FF = D_MODEL
SWIGLU_ALPHA = 1.702
SWIGLU_LIMIT = 7.0
MOE_BLOCK = 256
LN_EPS = 1e-5
DN_ALPHA = (2 * DEPTH) ** 0.25
DN_BETA = (8 * DEPTH) ** -0.25
IN_SPLITS = (ATT_WIDTH, ATT_KV_HEADS * ATT_HEAD_DIM, ATT_KV_HEADS * ATT_HEAD_DIM,
             IDX_HEADS * IDX_DIM, IDX_DIM, IDX_HEADS,
             RET_WIDTH, RET_WIDTH, RET_WIDTH, RET_WIDTH)
D_IN = sum(IN_SPLITS)

kernel_name = "dsa_retention_hybrid_moe_deepnorm"


def layer_norm(x, g, b):
    xf = x.astype(jnp.float32)
    mu = jnp.mean(xf, -1, keepdims=True)
    var = jnp.mean(jnp.square(xf - mu), -1, keepdims=True)
    return ((xf - mu) * lax.rsqrt(var + LN_EPS)).astype(x.dtype) * g + b


def head_norm(y):
    yf = y.astype(jnp.float32)
    mu = jnp.mean(yf, -1, keepdims=True)
    var = jnp.mean(jnp.square(yf - mu), -1, keepdims=True)
    return ((yf - mu) * lax.rsqrt(var + LN_EPS)).astype(y.dtype)


def t5_bucket(dist):
    max_exact = N_BUCKETS // 2
    n = jnp.maximum(dist, 0)
    nf = jnp.maximum(n, 1).astype(jnp.float32)
    large = max_exact + (jnp.log(nf / max_exact) / math.log(MAX_DISTANCE / max_exact)
                         * (N_BUCKETS - max_exact)).astype(jnp.int32)
    large = jnp.minimum(large, N_BUCKETS - 1)
    return jnp.where(n < max_exact, n, large)


def rotary(x):
    L, d = x.shape[1], x.shape[-1]
    inv = 1.0 / (ROPE_BASE ** (jnp.arange(0, d, 2, dtype=jnp.float32) / d))
    ang = jnp.arange(L, dtype=jnp.float32)[:, None] * inv[None, :]
    cos = jnp.cos(ang)[None, :, None, :]
    sin = jnp.sin(ang)[None, :, None, :]
    xf = x.astype(jnp.float32)
    x1, x2 = xf[..., : d // 2], xf[..., d // 2:]
    return jnp.concatenate([x1 * cos - x2 * sin, x1 * sin + x2 * cos], -1).astype(x.dtype)


def dsa_attention(q, k, v, q_idx, k_idx, w_idx, rel_bias):
    B, L = q.shape[0], q.shape[1]
    k_sel = min(TOPK_MAX, L // 4)
    nqb = L // Q_BLOCK
    rep = ATT_HEADS // ATT_KV_HEADS
    key_pos = jnp.arange(L, dtype=jnp.int32)
    b_ix = jnp.arange(B)[:, None, None]

    def blocks(a):
        return jnp.moveaxis(a.reshape(B, nqb, Q_BLOCK, *a.shape[2:]), 1, 0)

    def one_block(args):
        qb, qib, wb, tpos = args
        rel = jax.nn.relu(jnp.einsum('bthd,bsd->bths', qib, k_idx) * IDX_DIM ** -0.5)
        score = jnp.einsum('bths,bth->bts', rel, wb).astype(jnp.float32)
        causal = key_pos[None, :] <= tpos[:, None]
        score = jnp.where(causal[None], score, -jnp.inf)
        _, idx = lax.top_k(score, k_sel)
        ks = k[b_ix, idx]
        vs = v[b_ix, idx]
        qg = qb.reshape(B, Q_BLOCK, ATT_KV_HEADS, rep, ATT_HEAD_DIM)
        logits = jnp.einsum('btgrd,btkgd->btgrk', qg, ks).astype(jnp.float32) * ATT_HEAD_DIM ** -0.5
        dist = tpos[None, :, None] - idx
        bias = rel_bias[t5_bucket(dist)].reshape(B, Q_BLOCK, k_sel, ATT_KV_HEADS, rep)
        logits = logits + jnp.moveaxis(bias, 2, -1).astype(jnp.float32)
        valid = (dist >= 0)[:, :, None, None, :]
        logits = jnp.where(valid, logits, -jnp.inf)
        p = jax.nn.softmax(logits, axis=-1).astype(v.dtype)
        o = jnp.einsum('btgrk,btkgd->btgrd', p, vs)
        return o.reshape(B, Q_BLOCK, ATT_WIDTH)

    out = lax.map(one_block, (blocks(q), blocks(q_idx), blocks(w_idx),
                              key_pos.reshape(nqb, Q_BLOCK)))
    return jnp.moveaxis(out, 0, 1).reshape(B, L, ATT_WIDTH)


def retention(q, k, v):
    B, L, H, d = q.shape
    nc = L // RET_CHUNK
    log_g = jnp.log(1.0 - 2.0 ** (-5.0 - jnp.arange(H, dtype=jnp.float32)))
    i = jnp.arange(RET_CHUNK, dtype=jnp.float32)
    diff = i[:, None] - i[None, :]
    inner_decay = jnp.where(diff[None] >= 0,
                            jnp.exp(jnp.maximum(diff, 0.0)[None] * log_g[:, None, None]), 0.0)
    q_decay = jnp.exp((i[None, :] + 1.0) * log_g[:, None])[None, :, :, None]
    k_decay = jnp.exp((RET_CHUNK - 1.0 - i[None, :]) * log_g[:, None])[None, :, :, None]
    chunk_decay = jnp.exp(RET_CHUNK * log_g)[None, :, None, None]

    def to_chunks(a):
        return a.astype(jnp.float32).reshape(B, nc, RET_CHUNK, H, d).transpose(1, 0, 3, 2, 4)

    def step(state, inp):
        qi, ki, vi = inp
        attn = jnp.einsum('bhid,bhjd->bhij', qi, ki) * inner_decay[None]
        inner = jnp.einsum('bhij,bhjv->bhiv', attn, vi)
        cross = jnp.einsum('bhid,bhdv->bhiv', qi, state) * q_decay
        new_state = state * chunk_decay + jnp.einsum('bhjd,bhjv->bhdv', ki * k_decay, vi)
        return new_state, inner + cross

    state0 = jnp.zeros((B, H, d, d), jnp.float32)
    _, out = lax.scan(step, state0, (to_chunks(q), to_chunks(k), to_chunks(v)))
    return out.transpose(1, 0, 3, 2, 4).reshape(B, L, H, d).astype(q.dtype)


def hybrid_mixer(h, w_in, ret_norm_g, w_out, rel_bias):
    B, L, _ = h.shape
    proj = h @ w_in
    cuts = np.cumsum(IN_SPLITS)[:-1].tolist()
    q_a, k_a, v_a, q_i, k_i, w_i, q_r, k_r, v_r, g_r = jnp.split(proj, cuts, axis=-1)
    attn = dsa_attention(
        q_a.reshape(B, L, ATT_HEADS, ATT_HEAD_DIM),
        k_a.reshape(B, L, ATT_KV_HEADS, ATT_HEAD_DIM),
        v_a.reshape(B, L, ATT_KV_HEADS, ATT_HEAD_DIM),
        q_i.reshape(B, L, IDX_HEADS, IDX_DIM), k_i, w_i * IDX_HEADS ** -0.5, rel_bias)
    qr = rotary(q_r.reshape(B, L, RET_HEADS, RET_HEAD_DIM))
    kr = rotary(k_r.reshape(B, L, RET_HEADS, RET_HEAD_DIM)) * RET_HEAD_DIM ** -0.5
    ret = retention(qr, kr, v_r.reshape(B, L, RET_HEADS, RET_HEAD_DIM))
    ret = head_norm(ret).reshape(B, L, RET_WIDTH) * ret_norm_g
    ret = jax.nn.silu(g_r) * ret
    return jnp.concatenate([attn, ret], axis=-1) @ w_out


def clamped_swiglu(gu):
    g, u = gu[..., :D_FF], gu[..., D_FF:]
    g = jnp.minimum(g, SWIGLU_LIMIT)
    u = jnp.clip(u, -SWIGLU_LIMIT, SWIGLU_LIMIT)
    return (u + 1.0) * (g * jax.nn.sigmoid(g * SWIGLU_ALPHA))


def moe(h, w_router, b_router, w_gate_up, b_gate_up, w_down, b_down):
    B, L, D = h.shape
    N = B * L
    hf = h.reshape(N, D)
    logits = hf @ w_router + b_router
    top_val, top_idx = lax.top_k(logits, TOP_K)
    gates = jax.nn.softmax(top_val.astype(jnp.float32), axis=-1).astype(h.dtype)
    e_flat = top_idx.reshape(-1).astype(jnp.int32)
    tok_flat = jnp.arange(N * TOP_K, dtype=jnp.int32) // TOP_K
    g_flat = gates.reshape(-1)
    order = jnp.argsort(e_flat)
    e_sorted = e_flat[order]
    counts = jnp.zeros((N_EXPERTS,), jnp.int32).at[e_flat].add(1)
    starts = jnp.cumsum(counts) - counts
    padded = (counts + MOE_BLOCK - 1) // MOE_BLOCK * MOE_BLOCK
    pends = jnp.cumsum(padded)
    pstarts = pends - padded
    rank = jnp.arange(N * TOP_K, dtype=jnp.int32) - starts[e_sorted]
    dest = pstarts[e_sorted] + rank
    n_blocks = -(-(N * TOP_K) // MOE_BLOCK) + N_EXPERTS
    P = n_blocks * MOE_BLOCK
    row_tok = jnp.full((P,), N, jnp.int32).at[dest].set(tok_flat[order])
    row_gate = jnp.zeros((P,), h.dtype).at[dest].set(g_flat[order])
    block_start = jnp.arange(n_blocks, dtype=jnp.int32) * MOE_BLOCK
    block_exp = jnp.minimum(jnp.searchsorted(pends, block_start, side='right'),
                            N_EXPERTS - 1).astype(jnp.int32)

    def one_block(args):
        tok, gate, e = args
        xb = jnp.take(hf, tok, axis=0, mode='fill', fill_value=0)
        gu = xb @ w_gate_up[e] + b_gate_up[e]
        y = clamped_swiglu(gu) @ w_down[e] + b_down[e]
        return y * gate[:, None]

    ys = lax.map(one_block, (row_tok.reshape(n_blocks, MOE_BLOCK),
                             row_gate.reshape(n_blocks, MOE_BLOCK), block_exp))
    out = jnp.zeros((N, D), h.dtype).at[row_tok].add(ys.reshape(P, D), mode='drop')
    return out.reshape(B, L, D)


def setup_inputs(seed: int = 0) -> dict:
    key = jax.random.key(seed)
    ks = jax.random.split(key, 20)
    f32 = jnp.float32
    D = D_MODEL
    nrm = lambda k, s: jax.random.normal(k, s, f32)
    col_scale = np.ones((D_IN,), np.float32)
    offs = np.concatenate([[0], np.cumsum(IN_SPLITS)])
    for j in (2, 8):
        col_scale[offs[j]:offs[j + 1]] = DN_BETA
    w_in = nrm(ks[3], (DEPTH, D, D_IN)) * D ** -0.5 * jnp.asarray(col_scale)
    return {
        "x": nrm(ks[0], (BATCH, SEQ, D)),
        "c": nrm(ks[1], (BATCH, D)),
        "rel_bias": nrm(ks[2], (N_BUCKETS, ATT_HEADS)) * 0.5,
        "w_ada": nrm(ks[4], (DEPTH, D, 6 * D)) * D ** -0.5 * 0.2,
        "b_ada": nrm(ks[5], (DEPTH, 6 * D)) * 0.01,
        "w_in": w_in,
        "ret_norm_g": 1.0 + 0.05 * nrm(ks[6], (DEPTH, RET_WIDTH)),
        "w_out": nrm(ks[7], (DEPTH, D_MIX, D)) * D_MIX ** -0.5 * DN_BETA,
        "ln1_g": 1.0 + 0.05 * nrm(ks[8], (DEPTH, D)),
        "ln1_b": 0.01 * nrm(ks[9], (DEPTH, D)),
        "w_router": nrm(ks[10], (DEPTH, D, N_EXPERTS)) * D ** -0.5,
        "b_router": 0.01 * nrm(ks[11], (DEPTH, N_EXPERTS)),
        "w_gate_up": nrm(ks[12], (DEPTH, N_EXPERTS, D, 2 * D_FF)) * D ** -0.5 * DN_BETA,
        "b_gate_up": 0.01 * nrm(ks[13], (DEPTH, N_EXPERTS, 2 * D_FF)),
        "w_down": nrm(ks[14], (DEPTH, N_EXPERTS, D_FF, D)) * D_FF ** -0.5 * DN_BETA,
        "b_down": 0.01 * nrm(ks[15], (DEPTH, N_EXPERTS, D)),
        "ln2_g": 1.0 + 0.05 * nrm(ks[16], (DEPTH, D)),
        "ln2_b": 0.01 * nrm(ks[17], (DEPTH, D)),
    }


def reference(x, c, rel_bias, w_ada, b_ada, w_in, ret_norm_g, w_out, ln1_g, ln1_b,
              w_router, b_router, w_gate_up, b_gate_up, w_down, b_down, ln2_g, ln2_b):
    for l in range(DEPTH):
        mod = jax.nn.silu(c) @ w_ada[l] + b_ada[l]
        sh_a, sc_a, g_a, sh_f, sc_f, g_f = jnp.split(mod[:, None, :], 6, axis=-1)
        h = x * (1.0 + sc_a) + sh_a
        mix = hybrid_mixer(h, w_in[l], ret_norm_g[l], w_out[l], rel_bias)
        x = layer_norm(DN_ALPHA * x + (1.0 + g_a) * mix, ln1_g[l], ln1_b[l])
        h = x * (1.0 + sc_f) + sh_f
        ff = moe(h, w_router[l], b_router[l], w_gate_up[l], b_gate_up[l], w_down[l], b_down[l])
        x = layer_norm(DN_ALPHA * x + (1.0 + g_f) * ff, ln2_g[l], ln2_b[l])
    return x
```

```python
import os
import contextlib
import numpy as np
import ml_dtypes
import concourse.bass as bass
import concourse.mybir as mybir
from concourse.bass_utils import run_bass_kernel_spmd

F32 = mybir.dt.float32
BF16 = mybir.dt.bfloat16
I32 = mybir.dt.int32
AF = mybir.ActivationFunctionType
ALU = mybir.AluOpType
AX = mybir.AxisListType

L = 4096
D = 1024
NT = 32
NCOL = 3204
NE = 32
CAP = 1024
NSLOT = CAP // 128
NIT = 18
ALPHA = float(2.0 ** 0.25)
EPS = 1e-5
ENGS = ['pe', 'act', 'dve', 'pool', 'sp']
NDMA = 8


class Sched:
    def __init__(self):
        self.prog = {e: [] for e in ENGS}
        self.seq = {e: 0 for e in ENGS}
        self.res = {}
        self.waited = {e: {} for e in ENGS}
        self.dma_rr = {e: 0 for e in ENGS}
        self.dma_cnt = {}
        self.pending = {e: {} for e in ENGS}
        self.enabled = True

    def add(self, eng, fn, r=(), w=(), dma=False, grp=None):
        if not self.enabled:
            return None
        needs = dict(self.pending[eng])
        self.pending[eng] = {}

        def need(tok):
            if tok is None:
                return
            k, v = tok
            if needs.get(k, 0) < v:
                needs[k] = v
        for key in r:
            st = self.res.get(key)
            if st:
                need(st[0])
        for key in w:
            st = self.res.get(key)
            if st:
                need(st[0])
                for k, v in st[1].items():
                    need((k, v))
        if dma:
            gname = grp or eng
            j = self.dma_rr.get(gname, 0)
            self.dma_rr[gname] = (j + 1) % NDMA
            semk = ('d', gname, j)
            prev = self.dma_cnt.get(semk, 0)
            if prev:
                need((semk, 16 * prev))
            self.dma_cnt[semk] = prev + 1
            tok = (semk, 16 * (prev + 1))
            inc = 16
        else:
            self.seq[eng] += 1
            tok = (('e', eng), self.seq[eng])
            inc = 1
        waits = []
        for k, v in needs.items():
            if k == ('e', 'pe') and eng == 'pe' and not dma:
                continue
            if self.waited[eng].get(k, 0) >= v:
                continue
            self.waited[eng][k] = v
            waits.append((k, v))
        self.prog[eng].append((waits, fn, tok, inc))
        for key in w:
            self.res[key] = [tok, {}]
        for key in r:
            if key in w:
                continue
            st = self.res.setdefault(key, [None, {}])
            if st[1].get(tok[0], 0) < tok[1]:
                st[1][tok[0]] = tok[1]
        return tok

    def cut(self, name):
        if os.environ.get('KCUT', '') == name:
            self.enabled = False

    def all_tokens(self):
        toks = {}
        for e in ENGS:
            if self.seq[e]:
                toks[('e', e)] = self.seq[e]
        for k, c in self.dma_cnt.items():
            toks[k] = 16 * c
        return toks

    def barrier(self, exclude=()):
        toks = self.all_tokens()
        for e in ENGS:
            for k, v in toks.items():
                if k[0] == 'd' and k[1] in exclude:
                    continue
                if self.pending[e].get(k, 0) < v:
                    self.pending[e][k] = v


def t5_bucket_np(n):
    n = np.maximum(n, 0)
    nf = np.maximum(n, 1).astype(np.float32)
    large = 16 + (np.log(nf / np.float32(16)) / np.float32(np.log(128 / 16)) * np.float32(16)).astype(np.int32)
    large = np.minimum(large, 31)
    return np.where(n < 16, n, large)


def build_nc(dbg=(), stage='D'):
    nc = bass.Bass("TRN2", target_bir_lowering=False)
    S = Sched()

    def din(name, shape, dt=F32):
        return nc.dram_tensor(name, list(shape), dt, kind="ExternalInput").ap()

    x_d = din("x", [L, D])
    c8_d = din("c8", [128, 8])
    wada_d = din("w_ada", [D, 6 * D])
    bada_d = din("b_ada", [1, 6 * D])
    win_d = din("w_in", [D, NCOL])
    wout_d = din("w_out", [D, D])
    bt_d = din("bt", [128, 2048])
    c31_d = din("c31", [128, 8])
    cos_d = din("cos", [L, 64])
    sin_d = din("sin", [L, 64])
    dt_d = din("dtab", [128, 512])
    qd_d = din("qdtab", [128, 512])
    ch_d = din("chtab", [128, 512])
    kd_d = din("kdtab", [128, 4])
    rng_d = din("rng", [1, 512])
    ln1g_d = din("ln1g", [1, D])
    ln1b_d = din("ln1b", [1, D])
    ln2g_d = din("ln2g", [1, D])
    ln2b_d = din("ln2b", [1, D])
    wr_d = din("wr", [D, NE])
    br_d = din("br", [1, NE])
    wgu_d = din("wgu", [NE, D, 2 * D])
    bgu_d = din("bgu", [128, NE * 16])
    wd_d = din("wd", [NE, D, D])
    bd_d = din("bd", [NE, D])
    negd_d = din("negd", [128, 128])
    ut_d = din("ut", [128, 128])
    pow_d = din("pow2", [128, NIT + 2])
    ec_d = din("ec", [128, NE])
    id_d = din("ident", [128, 128])
    out_d = nc.dram_tensor("out", [L, D], F32, kind="ExternalOutput").ap()
    dbg_d = {}
    for name, shape in dbg:
        dbg_d[name] = nc.dram_tensor(name, list(shape), F32, kind="ExternalOutput").ap()

    qs_d = nc.dram_tensor("qs_scr", [NT, 128, 768], BF16, kind="Internal").ap()
    ret_d = nc.dram_tensor("ret_scr", [L, 512], BF16, kind="Internal").ap()
    x1_d = nc.dram_tensor("x1_scr", [L, D], F32, kind="Internal").ap()
    xg_d = nc.dram_tensor("xg_scr", [NE * CAP, D], BF16, kind="Internal").ap()
    y_d = nc.dram_tensor("y_scr", [NE * CAP, D], F32, kind="Internal").ap()

    with contextlib.ExitStack() as es:
        ARENA_W = 53000
        arena = es.enter_context(nc.sbuf_tensor("arena", [128, ARENA_W], F32))
        PS = [es.enter_context(nc.psum_tensor(f"ps{i}", [128, 1024], F32)) for i in range(4)]
        off = [0]

        def alloc(n, dt=F32):
            n32 = n if dt in (F32, I32) else (n + 1) // 2
            assert off[0] + n32 <= ARENA_W, (off[0], n32)
            a = arena[:, off[0]:off[0] + n32]
            off[0] += n32
            if dt == F32:
                return a
            return a.bitcast(dt)

        def bank(b):
            return PS[b // 2][:, (b % 2) * 512:(b % 2) * 512 + 512]

        def bankb(b):
            return bank(b).bitcast(BF16)

        def pk(b):
            return f"ps{b}"

        def mm(out, lhsT, rhs, start, stop, r, w, skip=False):
            if skip:
                S.add('pe', lambda e: e.matmul(out, lhsT=lhsT, rhs=rhs, start=start, stop=stop, skip_group_check=True), r, w)
            else:
                S.add('pe', lambda e: e.matmul(out, lhsT=lhsT, rhs=rhs, start=start, stop=stop), r, w)

        def tp(out, in_, idn, r, w):
            S.add('pe', lambda e: e.transpose(out=out, in_=in_, identity=idn), r, w)

        def dma(eng, out, in_, r, w):
            return S.add(eng, lambda e: e.dma_start(out=out, in_=in_), r, w, dma=True)

        def act(out, in_, func, r, w, bias=0.0, scale=1.0, accum=None):
            if accum is None:
                S.add('act', lambda e: e.activation(out=out, in_=in_, func=func, bias=bias, scale=scale), r, w)
            else:
                S.add('act', lambda e: e.activation(out=out, in_=in_, func=func, bias=bias, scale=scale, accum_out=accum), r, w)

        def cp(eng, out, in_, r, w):
            if eng == 'act':
                S.add('act', lambda e: e.copy(out=out, in_=in_), r, w)
            else:
                S.add(eng, lambda e: e.tensor_copy(out=out, in_=in_), r, w)

        def tt(eng, out, in0, in1, op, r, w):
            S.add(eng, lambda e: e.tensor_tensor(out=out, in0=in0, in1=in1, op=op), r, w)

        def ts(eng, out, in0, s1, op0, r, w, s2=None, op1=None, accum=None):
            if op1 is None:
                S.add(eng, lambda e: e.tensor_scalar(out=out, in0=in0, scalar1=s1, scalar2=None, op0=op0), r, w)
            elif accum is None:
                S.add(eng, lambda e: e.tensor_scalar(out=out, in0=in0, scalar1=s1, scalar2=s2, op0=op0, op1=op1), r, w)
            else:
                S.add(eng, lambda e: e.tensor_scalar(out=out, in0=in0, scalar1=s1, scalar2=s2, op0=op0, op1=op1, accum_out=accum), r, w)

        def stt(eng, out, in0, scalar, in1, op0, op1, r, w, accum=None):
            if accum is None:
                S.add(eng, lambda e: e.scalar_tensor_tensor(out=out, in0=in0, scalar=scalar, in1=in1, op0=op0, op1=op1), r, w)
            else:
                S.add(eng, lambda e: e.scalar_tensor_tensor(out=out, in0=in0, scalar=scalar, in1=in1, op0=op0, op1=op1, accum_out=accum), r, w)

        def op(eng, name, r, w, dma=False, **kw):
            return S.add(eng, lambda e: getattr(e, name)(**kw), r, w, dma=dma)

        def memset(eng, out, val, r, w):
            S.add(eng, lambda e: e.memset(out, val), r, w)

        def dbg_out(name, src, r, rows=None):
            if name in dbg_d:
                dst = dbg_d[name] if rows is None else dbg_d[name][rows[0]:rows[1], :]
                dma('sp', dst, src, r, [])

        modB = alloc(4096)
        GA1 = modB[:, 0:1024]
        SHF = modB[:, 1024:2048]
        SCF1 = modB[:, 2048:3072]
        GF1 = modB[:, 3072:4096]
        ident = alloc(128)
        identb = alloc(128, BF16)
        SHA = alloc(8)
        SCA1 = alloc(8)
        WI = alloc(NT * 4)
        GATES = alloc(NT * 4)
        IDXT = alloc(NT * 4, I32)
        carry = alloc(NE)
        ZT = alloc(4 * 1024, BF16)
        persist_small = off[0]
        KT = alloc(L, BF16)
        KIT = alloc(L, BF16)
        V1 = alloc(NT * 2 * 65 + 1, BF16)[:, 0:NT * 2 * 65]
        V1v = V1.rearrange("p (n g d) -> p n g d", g=2, d=65)
        wout = alloc(8 * 1024, BF16)
        woutv = wout.rearrange("p (kc n) -> p kc n", n=1024)
        wrb = alloc(8 * NE, BF16)
        wrv = wrb.rearrange("p (kc n) -> p kc n", n=NE)
        UT = alloc(128, BF16)
        persist_off = off[0]

        dma('sp', ident, id_d[:, :], [], ['ident'])
        cp('dve', identb, ident, ['ident'], ['identb'])
        memset('pool', V1, 1.0, [], ['V1'])
        memset('pool', carry, 0.0, [], ['carry'])

        c8 = alloc(8)
        csil = alloc(8)
        ones = alloc(128)
        cB = alloc(8 * 128)
        modA = alloc(2048)
        wab = [alloc(8 * 512), alloc(8 * 512)]
        badab = [alloc(512), alloc(512)]
        dma('sp', c8, c8_d[:, :], [], ['c8'])
        act(csil, c8, AF.Silu, ['c8'], ['csil'])
        memset('dve', ones, 1.0, [], ['ones'])
        for kc in range(8):
            ts('dve', cB[:, kc * 128:(kc + 1) * 128], ones, csil[:, kc:kc + 1], ALU.mult, ['ones', 'csil'], ['cB'])
        wada_v = wada_d.rearrange("(kc p) n -> p kc n", p=128)
        for ch in range(12):
            wb = wab[ch % 2]
            bb = badab[ch % 2]
            wbv = wb.rearrange("p (kc n) -> p kc n", n=512)
            dma('sp', wbv, wada_v[:, :, ch * 512:(ch + 1) * 512], [], [f'wab{ch % 2}'])
            dma('sp', bb, bada_d[:, ch * 512:(ch + 1) * 512].partition_broadcast(128), [], [f'badab{ch % 2}'])
            b = ch % 2
            for kc in range(8):
                mm(bank(b), cB[:, kc * 128:(kc + 1) * 128], wbv[:, kc, :], kc == 0, kc == 7,
                   ['cB', f'wab{ch % 2}'], [pk(b)])
            if ch < 4:
                dst = modA[:, ch * 512:(ch + 1) * 512]
                dkey = 'modA'
            else:
                dst = modB[:, (ch - 4) * 512:(ch - 3) * 512]
                dkey = 'modB'
            tt('dve', dst, bank(b), bb, ALU.add, [pk(b), f'badab{ch % 2}'], [dkey])
        ts('dve', modA[:, 1024:2048], modA[:, 1024:2048], 1.0, ALU.add, ['modA'], ['modA'])
        ts('dve', GA1, GA1, 1.0, ALU.add, ['modB'], ['modB'])
        ts('dve', SCF1, SCF1, 1.0, ALU.add, ['modB'], ['modB'])
        ts('dve', GF1, GF1, 1.0, ALU.add, ['modB'], ['modB'])
        for c in range(8):
            tp(bank(2 + (c % 2))[:, 0:128], modA[:, c * 128:(c + 1) * 128], ident, ['modA', 'ident'], [pk(2 + (c % 2))])
            cp('dve', SHA[:, c:c + 1], bank(2 + (c % 2))[:, 0:1], [pk(2 + (c % 2))], ['SHA'])
        for c in range(8):
            tp(bank(2 + (c % 2))[:, 0:128], modA[:, 1024 + c * 128:1024 + (c + 1) * 128], ident, ['modA', 'ident'], [pk(2 + (c % 2))])
            cp('dve', SCA1[:, c:c + 1], bank(2 + (c % 2))[:, 0:1], [pk(2 + (c % 2))], ['SCA1'])
        dbg_out("d_modB", modB, ['modB'])
        S.barrier(exclude=('zf',))
        off[0] = persist_off

        if stage == 'A':
            S.enabled = False
        win = alloc(8 * NCOL, BF16)
        winv = win.rearrange("p (kc n) -> p kc n", n=NCOL)
        xtb = [alloc(1024), alloc(1024)]
        csb = [alloc(64), alloc(64)]
        snb = [alloc(64), alloc(64)]
        hT = alloc(1024, BF16)
        tmA = alloc(512, BF16)
        tmB = alloc(512, BF16)
        QS = alloc(768, BF16)
        t1 = alloc(256)
        t2 = alloc(256)
        t3 = alloc(256)
        t4 = alloc(256)
        qkrot_b = [alloc(1024, BF16), alloc(1024, BF16)]
        qkT = alloc(1024, BF16)
        qdT = alloc(512, BF16)
        kdk = alloc(512, BF16)
        ATb = alloc(512, BF16)
        VR_b = [alloc(512, BF16), alloc(512, BF16)]
        SG_b = [alloc(512), alloc(512)]
        S32 = alloc(512)
        S16 = alloc(512, BF16)
        DTt = alloc(512)
        QDt = alloc(512)
        CHt = alloc(512)
        KDt = alloc(4)
        RNG = alloc(512)
        rn = alloc(512)
        retb = alloc(512, BF16)
        bst = alloc(24)
        bmv = alloc(8)
        sd4 = alloc(4)
        rs4 = alloc(4)

        win_v = win_d.rearrange("(kc p) n -> p kc n", p=128)
        for kc in range(8):
            dma('pool', winv[:, kc, :], win_v[:, kc, :], [], [f'win{kc}'])
        for kc in range(8):
            dma('pool', woutv[:, kc, :], wout_d[kc * 128:(kc + 1) * 128, :], [], [f'wout{kc}'])
        dma('pool', wrv, wr_d.rearrange("(kc p) n -> p kc n", p=128), [], ['wrb'])
        dma('pool', UT, ut_d[:, :], [], ['UT'])
        memset('dve', ZT, 0.0, [], ['ZT'])
        ZKEYS = []
        for zi in range(NE * CAP // 512):
            ZKEYS.append(f'xgz{zi}')
            S.add('pool', (lambda z: (lambda e: e.dma_start(out=xg_d[z * 512:(z + 1) * 512, :].rearrange("(a p) f -> p a f", p=128),
                                                            in_=ZT.rearrange("p (a f) -> p a f", f=1024))))(zi),
                  ['ZT'], [f'xgz{zi}'], dma=True, grp='zf')
        dma('sp', DTt, dt_d[:, :], [], ['DTt'])
        dma('sp', QDt, qd_d[:, :], [], ['QDt'])
        dma('sp', CHt, ch_d[:, :], [], ['CHt'])
        dma('sp', KDt, kd_d[:, :], [], ['KDt'])
        dma('sp', RNG, rng_d.partition_broadcast(128), [], ['RNG'])

        S.cut('pro')
        CH = [(0, 512), (512, 512), (1024, 132), (1156, 512), (1668, 512), (2180, 512), (2692, 512)]

        def proj_chunk(ci, b):
            c0, wd = CH[ci]
            for kc in range(8):
                mm(bank(b)[:, 0:wd], hT[:, kc * 128:(kc + 1) * 128], winv[:, kc, c0:c0 + wd], kc == 0, kc == 7,
                   ['hT', f'win{kc}'], [pk(b)])

        def b1_proj(n):
            xt = xtb[n % 2]
            xk = f'xt{n % 2}'
            qkrot, VR, SG = qkrot_b[n % 2], VR_b[n % 2], SG_b[n % 2]
            cs = csb[n % 2]
            sn = snb[n % 2]
            dma('sp', xt, x_d[n * 128:(n + 1) * 128, :], [], [xk])
            dma('sp', cs, cos_d[n * 128:(n + 1) * 128, :], [], [f'cs{n % 2}'])
            dma('sp', sn, sin_d[n * 128:(n + 1) * 128, :], [], [f'sn{n % 2}'])
            for c in range(8):
                b = 0 if c < 4 else 1
                tp(bank(b)[:, (c % 4) * 128:(c % 4 + 1) * 128], xt[:, c * 128:(c + 1) * 128], ident, [xk, 'ident'], [pk(b)])
            for c in range(8):
                b = 0 if c < 4 else 1
                ts('dve', hT[:, c * 128:(c + 1) * 128], bank(b)[:, (c % 4) * 128:(c % 4 + 1) * 128], SCA1[:, c:c + 1], ALU.mult,
                   [pk(b), 'SHA', 'SCA1'], ['hT'], s2=SHA[:, c:c + 1], op1=ALU.add)
            yield
            proj_chunk(0, 2)
            cp('dve', tmA, bank(2), [pk(2)], ['tmA'])
            proj_chunk(1, 3)
            cp('act', tmB, bank(3), [pk(3)], ['tmB'])
            for c in range(4):
                tp(bankb(4)[:, c * 128:(c + 1) * 128], tmA[:, c * 128:(c + 1) * 128], identb, ['tmA', 'identb'], [pk(4)])
            for c in range(4):
                tp(bankb(4)[:, 512 + c * 128:512 + (c + 1) * 128], tmB[:, c * 128:(c + 1) * 128], identb, ['tmB', 'identb'], [pk(4)])
            cp('dve', QS[:, 0:512], bankb(4)[:, 0:512], [pk(4)], ['QS'])
            cp('dve', QS[:, 512:768], bankb(4)[:, 640:896], [pk(4)], ['QS'])
            cp('dve', KT[:, n * 128:(n + 1) * 128], bankb(4)[:, 512:640], [pk(4)], [f'KT{n}'])
            cp('dve', KIT[:, n * 128:(n + 1) * 128], bankb(4)[:, 896:1024], [pk(4)], [f'KIT{n}'])
            dma('sp', qs_d[n, :, :], QS, ['QS'], [f'qsd{n}'])
            yield
            proj_chunk(2, 2)
            cp('dve', V1v[:, n, :, 0:64], bank(2)[:, 0:128].rearrange("p (g d) -> p g d", d=64), [pk(2), 'V1'], [f'V1_{n}'])
            ts('dve', WI[:, n * 4:(n + 1) * 4], bank(2)[:, 128:132], 0.0625, ALU.mult, [pk(2)], [f'WI{n}'])
            yield
            proj_chunk(3, 3)
            proj_chunk(4, 2)
            cosB = cs.unsqueeze(1).to_broadcast([128, 4, 64])
            sinB = sn.unsqueeze(1).to_broadcast([128, 4, 64])
            qkv = qkrot.rearrange("p (a two d) -> p a two d", two=2, d=64)
            for which, b in ((0, 3), (1, 2)):
                pv = bank(b).rearrange("p (h two d) -> p h two d", two=2, d=64)
                x1v = pv[:, :, 0, :]
                x2v = pv[:, :, 1, :]
                t1v = t1.rearrange("p (h d) -> p h d", d=64)
                t2v = t2.rearrange("p (h d) -> p h d", d=64)
                t3v = t3.rearrange("p (h d) -> p h d", d=64)
                t4v = t4.rearrange("p (h d) -> p h d", d=64)
                ck = [f'cs{n % 2}', f'sn{n % 2}']
                tt('dve', t1v, x1v, cosB, ALU.mult, [pk(b)] + ck, ['t1'])
                tt('dve', t2v, x2v, sinB, ALU.mult, [pk(b)] + ck, ['t2'])
                tt('dve', t3v, x1v, sinB, ALU.mult, [pk(b)] + ck, ['t3'])
                tt('dve', t4v, x2v, cosB, ALU.mult, [pk(b)] + ck, ['t4'])
                tt('dve', qkv[:, which * 4:(which + 1) * 4, 0, :], t1v, t2v, ALU.subtract, ['t1', 't2'], ['qkrot' + str(n % 2)])
                tt('dve', qkv[:, which * 4:(which + 1) * 4, 1, :], t3v, t4v, ALU.add, ['t3', 't4'], ['qkrot' + str(n % 2)])
            yield
            proj_chunk(5, 3)
            cp('act', VR, bank(3), [pk(3)], ['VR' + str(n % 2)])
            proj_chunk(6, 2)
            act(SG, bank(2), AF.Silu, [pk(2)], ['SG' + str(n % 2)])
            yield

        def b1_ret(n):
            qkrot, VR, SG = qkrot_b[n % 2], VR_b[n % 2], SG_b[n % 2]
            for a in range(8):
                tp(bankb(4)[:, a * 128:(a + 1) * 128], qkrot[:, a * 128:(a + 1) * 128], identb, ['qkrot' + str(n % 2), 'identb'], [pk(4)])
            cp('dve', qkT, bankb(4), [pk(4)], ['qkT'])
            tt('dve', qdT, bankb(4)[:, 0:512], QDt, ALU.mult, [pk(4), 'QDt'], ['qdT'])
            tt('dve', kdk.rearrange("p (h d) -> p h d", d=128), qkrot[:, 512:1024].rearrange("p (h d) -> p h d", d=128),
               KDt.unsqueeze(2).to_broadcast([128, 4, 128]), ALU.mult, ['qkrot' + str(n % 2), 'KDt'], ['kdk'])
            yield
            for h in range(4):
                mm(bank(5)[:, h * 128:(h + 1) * 128], qkT[:, 512 + h * 128:512 + (h + 1) * 128], qkT[:, h * 128:(h + 1) * 128],
                   True, True, ['qkT'], [pk(5)])
            tt('dve', ATb, bank(5), DTt, ALU.mult, [pk(5), 'DTt'], ['ATb'])
            for h in range(4):
                hs = slice(h * 128, (h + 1) * 128)
                mm(bank(6)[:, hs], ATb[:, hs], VR[:, hs], True, n == 0, ['ATb', 'VR' + str(n % 2)], [pk(6)])
                if n > 0:
                    mm(bank(6)[:, hs], qdT[:, hs], S16[:, hs], False, True, ['qdT', 'S16'], [pk(6)])
            yield
            for h in range(4):
                hs = slice(h * 128, (h + 1) * 128)
                mm(bank(7)[:, hs], kdk[:, hs], VR[:, hs], True, True, ['kdk', 'VR' + str(n % 2)], [pk(7)])
            if n == 0:
                cp('dve', S32, bank(7), [pk(7)], ['S32'])
            else:
                tt('dve', S32, S32, CHt, ALU.mult, ['S32', 'CHt'], ['S32'])
                tt('dve', S32, S32, bank(7), ALU.add, ['S32', pk(7)], ['S32'])
            if n < NT - 1:
                cp('act', S16, S32, ['S32'], ['S16'])
            yield
            for h in range(4):
                op('dve', 'bn_stats', [pk(6)], ['bst'], out=bst[:, h * 6:(h + 1) * 6], in_=bank(6)[:, h * 128:(h + 1) * 128])
            for h in range(4):
                op('dve', 'bn_aggr', ['bst'], ['bmv'], out=bmv[:, h * 2:(h + 1) * 2], in_=bst[:, h * 6:(h + 1) * 6])
            bmvv = bmv.rearrange("p (h two) -> p h two", two=2)
            ts('dve', sd4, bmvv[:, :, 1], EPS, ALU.add, ['bmv'], ['sd4'])
            act(sd4, sd4, AF.Sqrt, ['sd4'], ['sd4'])
            op('dve', 'reciprocal', ['sd4'], ['rs4'], out=rs4, in_=sd4)
            for h in range(4):
                hs = slice(h * 128, (h + 1) * 128)
                ts('dve', rn[:, hs], bank(6)[:, hs], bmv[:, 2 * h:2 * h + 1], ALU.subtract, [pk(6), 'bmv', 'rs4'], ['rn'],
                   s2=rs4[:, h:h + 1], op1=ALU.mult)
            tt('dve', rn, rn, RNG, ALU.mult, ['rn', 'RNG'], ['rn'])
            tt('dve', retb, rn, SG, ALU.mult, ['rn', 'SG' + str(n % 2)], ['retb'])
            dma('sp', ret_d[n * 128:(n + 1) * 128, :], retb, ['retb'], [f'retd{n}'])
            yield

        def run_gen(g):
            for _ in g:
                pass

        def interleave2(ga, gb):
            da = db = False
            while not (da and db):
                if not da:
                    try:
                        next(ga)
                    except StopIteration:
                        da = True
                if not db:
                    try:
                        next(gb)
                    except StopIteration:
                        db = True

        run_gen(b1_proj(0))
        for n in range(NT):
            interleave2(b1_ret(n), b1_proj(n + 1) if n + 1 < NT else iter(()))
        S.barrier(exclude=('zf',))
        off[0] = persist_off

        if stage == 'B1':
            S.enabled = False
        scoreb = [alloc(L), alloc(L)]
        penb = [alloc(L, BF16), alloc(L, BF16), alloc(L, BF16)]
        junk_b = [alloc(L, BF16), alloc(L, BF16)]
        xtb = [alloc(1024), alloc(1024)]
        QSb = [alloc(768, BF16), alloc(768, BF16), alloc(768, BF16)]
        rb = [alloc(512), alloc(512)]
        pTb = [alloc(1024, BF16), alloc(1024, BF16)]
        BT8 = alloc(2048, BF16)
        IREP = alloc(512, BF16)
        nrmin_b = [alloc(1), alloc(1)]
        hw__b = [alloc(1), alloc(1)]
        C31 = alloc(8)
        NEGD = alloc(128)
        POW = alloc(NIT + 2)
        W2_b = [alloc(NIT + 2), alloc(NIT + 2)]
        W2h_b = [alloc(NIT + 2), alloc(NIT + 2)]
        mid_b = [alloc(NIT + 2), alloc(NIT + 2)]
        cnt_b = [alloc(NIT + 2), alloc(NIT + 2)]
        uu_b = [alloc(NIT + 2), alloc(NIT + 2)]
        rmax_b = [alloc(1), alloc(1)]
        rmin_b = [alloc(1), alloc(1)]
        rrng_b = [alloc(1), alloc(1)]
        thr_b = [alloc(1), alloc(1)]
        rec8 = alloc(8)
        cat = alloc(1024, BF16)
        catT = alloc(1024, BF16)
        tmp_off = off[0]
        tmp = alloc(1024)
        yv = alloc(1024)
        BTt = arena[:, tmp_off:tmp_off + 2048]
        x1 = alloc(1024)
        LN1G = alloc(1024)
        LN1B = alloc(1024)
        h2 = alloc(1024, BF16)
        h2T = alloc(1024, BF16)
        BR = alloc(NE)
        ONEb = alloc(128, BF16)
        EC = alloc(NE)
        lg = alloc(NE)
        mx8 = alloc(8)
        nm = alloc(1)
        ex4 = alloc(4)
        sm = alloc(1)
        rsm = alloc(1)
        selb = alloc(NE, BF16)
        pos = alloc(NE)
        flat = alloc(NE)
        ohj = alloc(NE)
        idxf = alloc(4)
        lst = alloc(12)
        lmv = alloc(2)
        lsd = alloc(1)
        lrs = alloc(1)

        memset('dve', ONEb, 1.0, [], ['ONEb'])
        dma('sp', BTt, bt_d[:, :], [], ['BTt', 'tmp', 'yv'])
        dma('sp', C31, c31_d[:, :], [], ['C31'])
        dma('sp', NEGD, negd_d[:, :], [], ['NEGD'])
        dma('sp', POW, pow_d[:, :], [], ['POW'])
        dma('sp', EC, ec_d[:, :], [], ['EC'])
        dma('sp', LN1G, ln1g_d.partition_broadcast(128), [], ['LN1G'])
        dma('sp', LN1B, ln1b_d.partition_broadcast(128), [], ['LN1B'])
        dma('sp', BR, br_d.partition_broadcast(128), [], ['BR'])
        BTv = BTt.rearrange("p (k h t) -> p k h t", k=2, h=8)
        for k in range(2):
            tt('dve', BTv[:, k, :, :], BTv[:, k, :, :], C31.unsqueeze(2).to_broadcast([128, 8, 128]), ALU.subtract,
               ['BTt', 'C31'], ['BTt'])

        ts('dve', BT8, BTt, 8.0, ALU.mult, ['BTt', 'tmp', 'yv'], ['BT8'])
        for c in range(4):
            cp('dve', IREP[:, c * 128:(c + 1) * 128], identb, ['identb'], ['IREP'])

        def layer_norm(eng_src, src, gtab, btab, dst, rkeys, wkey, gk, bk):
            op('dve', 'bn_stats', rkeys, ['lst'], out=lst[:, 0:6], in_=src[:, 0:512])
            op('dve', 'bn_stats', rkeys, ['lst'], out=lst[:, 6:12], in_=src[:, 512:1024])
            op('dve', 'bn_aggr', ['lst'], ['lmv'], out=lmv, in_=lst.rearrange("p (a s) -> p a s", s=6))
            ts('dve', lsd, lmv[:, 1:2], EPS, ALU.add, ['lmv'], ['lsd'])
            act(lsd, lsd, AF.Sqrt, ['lsd'], ['lsd'])
            op('dve', 'reciprocal', ['lsd'], ['lrs'], out=lrs, in_=lsd)
            ts('dve', src, src, lmv[:, 0:1], ALU.subtract, rkeys + ['lmv', 'lrs'], rkeys, s2=lrs[:, 0:1], op1=ALU.mult)
            tt('dve', src, src, gtab, ALU.mult, rkeys + [gk], rkeys)
            tt('dve', dst, src, btab, ALU.add, rkeys + [bk], [wkey])

        def stage_scores(n):
            Sn = (n + 1) * 128
            QSn = QSb[n % 3]
            qk = f'QSb{n % 3}'
            pen = penb[n % 3]
            pnk = f'pen{n % 3}'
            q2 = n % 2
            score = scoreb[q2]
            junk = junk_b[q2]
            W2, mid, cnt, uu = W2_b[q2], mid_b[q2], cnt_b[q2], uu_b[q2]
            rmax, rmin, rrng, thr, nrmin, hw_ = rmax_b[q2], rmin_b[q2], rrng_b[q2], thr_b[q2], nrmin_b[q2], hw__b[q2]
            dma('sp', QSn, qs_d[n, :, :], [f'qsd{n}'], [qk])
            QITc = QSn[:, 512:768]
            kit_keys = [f'KIT{j}' for j in range(n + 1)]
            nchunk = (Sn + 511) // 512
            it = 0
            for c in range(nchunk):
                wd = min(512, Sn - c * 512)
                for h in range(4):
                    b = it % 2
                    it += 1
                    half = slice((h % 2) * 64, (h % 2) * 64 + 64)
                    mm(bank(b)[:, 0:wd], QITc[half, (h // 2) * 128:(h // 2 + 1) * 128], KIT[half, c * 512:c * 512 + wd], True, True,
                       [qk] + kit_keys[c * 4:c * 4 + 4], [pk(b)])
                    act(rb[b][:, 0:wd], bank(b)[:, 0:wd], AF.Relu, [pk(b)], [f'rb{b}'])
                    if h == 0:
                        ts('dve', score[:, c * 512:c * 512 + wd], rb[b][:, 0:wd], WI[:, n * 4:n * 4 + 1], ALU.mult,
                           [f'rb{b}', f'WI{n}'], ['score' + str(q2)])
                    else:
                        stt('dve', score[:, c * 512:c * 512 + wd], rb[b][:, 0:wd], WI[:, n * 4 + h:n * 4 + h + 1],
                            score[:, c * 512:c * 512 + wd], ALU.mult, ALU.add, [f'rb{b}', f'WI{n}', 'score' + str(q2)], ['score' + str(q2)])
                yield
            if Sn <= 256:
                tt('dve', score[:, n * 128:(n + 1) * 128], score[:, n * 128:(n + 1) * 128], NEGD, ALU.add, ['score' + str(q2), 'NEGD'], ['score' + str(q2)])
                memset('dve', thr, -1e29, [], ['thr' + str(q2)])
                yield
            else:
                op('dve', 'tensor_reduce', ['score' + str(q2)], ['rmax' + str(q2)], out=rmax, in_=score[:, 0:Sn], axis=AX.X, op=ALU.max)
                op('dve', 'tensor_reduce', ['score' + str(q2)], ['rmin' + str(q2)], out=rmin, in_=score[:, 0:Sn], axis=AX.X, op=ALU.min)
                tt('dve', score[:, n * 128:(n + 1) * 128], score[:, n * 128:(n + 1) * 128], NEGD, ALU.add, ['score' + str(q2), 'NEGD'], ['score' + str(q2)])
                on_dve = (n % 2 == 1)
                if on_dve:
                    tt('dve', rrng, rmax, rmin, ALU.subtract, ['rmax' + str(q2), 'rmin' + str(q2)], ['rrng' + str(q2)])
                    ts('dve', nrmin, rmin, 1.0, ALU.mult, ['rmin' + str(q2)], ['nrmin' + str(q2)])
                else:
                    tt('dve', rrng, rmin, rmax, ALU.subtract, ['rmax' + str(q2), 'rmin' + str(q2)], ['rrng' + str(q2)])
                    ts('dve', nrmin, rmin, -1.0, ALU.mult, ['rmin' + str(q2)], ['nrmin' + str(q2)])
                ts('dve', W2, POW, rrng[:, 0:1], ALU.mult, ['POW', 'rrng' + str(q2)], ['W2' + str(q2)])
                stt('dve', mid[:, 0:1], W2[:, 1:2], 1.0, nrmin, ALU.mult, ALU.add, ['W2' + str(q2), 'nrmin' + str(q2)], ['mid' + str(q2)])
                W2h = W2h_b[q2]
                ts('dve', W2h, W2, 0.5, ALU.mult, ['W2' + str(q2)], ['W2' + str(q2)])
                memset('dve', cnt, 0.0, [], ['cnt' + str(q2)])
                yield
                for k in range(NIT):
                    if on_dve:
                        ts('dve', junk[:, 0:Sn], score[:, 0:Sn], mid[:, k:k + 1], ALU.is_ge, ['score' + str(q2), 'mid' + str(q2), 'cnt' + str(q2)],
                           ['junk' + str(q2), 'cnt' + str(q2)], s2=0.0, op1=ALU.add, accum=cnt[:, k:k + 1])
                        ts('dve', uu[:, k:k + 1], cnt[:, k:k + 1], 255.5, ALU.is_ge, ['cnt' + str(q2)], ['uu' + str(q2)], s2=0.5, op1=ALU.subtract)
                    else:
                        act(junk[:, 0:Sn], score[:, 0:Sn], AF.Sign, ['score' + str(q2), 'mid' + str(q2), 'cnt' + str(q2)], ['junk' + str(q2), 'cnt' + str(q2)], bias=mid[:, k:k + 1], scale=1.0,
                            accum=cnt[:, k:k + 1])
                        act(uu[:, k:k + 1], cnt[:, k:k + 1], AF.Sign, ['cnt' + str(q2)], ['uu' + str(q2)], bias=float(Sn - 510.5), scale=1.0)
                        act(mid[:, k + 1:k + 2], uu[:, k:k + 1], AF.Identity, ['uu' + str(q2), 'W2' + str(q2), 'mid' + str(q2)], ['mid' + str(q2)],
                            bias=mid[:, k:k + 1], scale=W2h[:, k + 1:k + 2])
                        yield
                        continue
                    stt('dve', mid[:, k + 1:k + 2], uu[:, k:k + 1], W2[:, k + 1:k + 2], mid[:, k:k + 1], ALU.mult, ALU.add,
                        ['uu' + str(q2), 'W2' + str(q2), 'mid' + str(q2)], ['mid' + str(q2)])
                    yield
                ts('dve', hw_, W2[:, NIT:NIT + 1], 0.5, ALU.mult, ['W2' + str(q2)], ['hw_' + str(q2)])
                if on_dve:
                    tt('dve', thr, mid[:, NIT:NIT + 1], hw_, ALU.subtract, ['hw_' + str(q2), 'mid' + str(q2)], ['thr' + str(q2)])
                else:
                    stt('dve', thr, mid[:, NIT:NIT + 1], -1.0, hw_, ALU.mult, ALU.add, ['hw_' + str(q2), 'mid' + str(q2)], ['thr' + str(q2)])
            ts('dve', pen[:, 0:Sn], score[:, 0:Sn], thr[:, 0:1], ALU.is_lt, ['score' + str(q2), 'thr' + str(q2)], [pnk], s2=-240000.0, op1=ALU.mult)
            yield

        def stage_attn(n):
            xt = xtb[n % 2]
            xk = f'xt{n % 2}'
            QSn = QSb[n % 3]
            qk = f'QSb{n % 3}'
            pen = penb[n % 3]
            pnk = f'pen{n % 3}'
            QTc = QSn[:, 0:512]
            for j in range(n + 1):
                jb = j % 2
                Lp = PS[1 + jb][:, :]
                lk = [pk(2 + 2 * jb), pk(3 + 2 * jb)]
                near = j >= n - 1
                kind = 0 if j == n else 1
                for g in range(2):
                    hp = slice(g * 64, g * 64 + 64)
                    og = Lp[:, g * 512:(g + 1) * 512]
                    mm(og, KT[hp, j * 128:(j + 1) * 128], QTc[hp, :], True, False, [qk, f'KT{j}'], lk)
                    mm(og, pen[:, j * 128:(j + 1) * 128], IREP, False, not near, [pnk, 'IREP'], lk)
                    if near:
                        mm(og, identb, BT8[:, kind * 1024 + g * 512:kind * 1024 + (g + 1) * 512], False, True, ['identb', 'BT8'], lk)
                pT = pTb[jb]
                pk_ = f'pT{jb}'
                act(pT, Lp, AF.Exp, lk, [pk_], scale=0.125)
                for h in range(8):
                    g = h // 4
                    ob = 6 + g
                    mm(bank(ob)[:, (h % 4) * 65:(h % 4) * 65 + 65], pT[:, h * 128:(h + 1) * 128], V1v[:, j, g, :],
                       j == 0 and h % 4 == 0, j == n, [pk_, f'V1_{j}', 'V1'], [pk(ob)], skip=True)
                yield
            for g in range(2):
                ov = bank(6 + g)[:, 0:260].rearrange("p (h d) -> p h d", d=65)
                op('dve', 'reciprocal', [pk(6 + g)], ['rec8'], out=rec8[:, g * 4:(g + 1) * 4], in_=ov[:, :, 64])
                tt('dve', cat[:, g * 256:(g + 1) * 256].rearrange("p (h d) -> p h d", d=64), ov[:, :, 0:64],
                   rec8[:, g * 4:(g + 1) * 4].unsqueeze(2).to_broadcast([128, 4, 64]), ALU.mult, [pk(6 + g), 'rec8'], ['cat_a'])
            yield

        def stage_tail(n):
            xt = xtb[n % 2]
            xk = f'xt{n % 2}'
            dma('sp', xt, x_d[n * 128:(n + 1) * 128, :], [], [xk])
            dma('sp', cat[:, 512:1024], ret_d[n * 128:(n + 1) * 128, :], [f'retd{n}'], ['cat_r'])
            for c in range(8):
                tp(bankb(0)[:, c * 128:(c + 1) * 128], cat[:, c * 128:(c + 1) * 128], identb, ['cat_a', 'cat_r', 'identb'], [pk(0)])
            cp('dve', catT, bankb(0), [pk(0)], ['catT'])
            yield
            Mp = PS[0][:, :]
            mk = [pk(0), pk(1)]
            for half in range(2):
                for kc in range(8):
                    mm(Mp[:, half * 512:(half + 1) * 512], catT[:, kc * 128:(kc + 1) * 128], woutv[:, kc, half * 512:(half + 1) * 512],
                       kc == 0, kc == 7, ['catT', f'wout{kc}'], mk)
            tt('dve', tmp, Mp, GA1, ALU.mult, mk + ['modB'], ['tmp'])
            stt('dve', yv, xt, ALPHA, tmp, ALU.mult, ALU.add, [xk, 'tmp'], ['yv'])
            yield
            layer_norm('dve', yv, LN1G, LN1B, x1, ['yv'], 'x1', 'LN1G', 'LN1B')
            dma('sp', x1_d[n * 128:(n + 1) * 128, :], x1, ['x1'], [f'x1d{n}'])
            dbg_out("d_x1", x1, ['x1'], rows=(n * 128, (n + 1) * 128))
            yield
            tt('dve', tmp, x1, SCF1, ALU.mult, ['x1', 'modB'], ['tmp'])
            tt('dve', h2, tmp, SHF, ALU.add, ['tmp', 'modB'], ['h2'])
            for c in range(8):
                tp(bankb(1)[:, c * 128:(c + 1) * 128], h2[:, c * 128:(c + 1) * 128], identb, ['h2', 'identb'], [pk(1)])
            cp('dve', h2T, bankb(1), [pk(1)], ['h2T'])
            yield
            for kc in range(8):
                mm(bank(0)[:, 0:NE], h2T[:, kc * 128:(kc + 1) * 128], wrv[:, kc, :], kc == 0, kc == 7, ['h2T', 'wrb'], [pk(0)])
            tt('dve', lg, bank(0)[:, 0:NE], BR, ALU.add, [pk(0), 'BR'], ['lg'])
            op('dve', 'max', ['lg'], ['mx8'], out=mx8, in_=lg)
            ts('dve', nm, mx8[:, 0:1], -1.0, ALU.mult, ['mx8'], ['nm'])
            memset('dve', sm, 0.0, [], ['sm'])
            act(ex4, mx8[:, 0:4], AF.Exp, ['mx8', 'nm', 'sm'], ['ex4', 'sm'], bias=nm[:, 0:1], scale=1.0, accum=sm)
            op('dve', 'reciprocal', ['sm'], ['rsm'], out=rsm, in_=sm)
            ts('dve', GATES[:, n * 4:(n + 1) * 4], ex4, rsm[:, 0:1], ALU.mult, ['ex4', 'rsm'], [f'GATES{n}'])
            yield
            ts('dve', selb, lg, mx8[:, 3:4], ALU.is_ge, ['lg', 'mx8'], ['selb'])
            mm(bank(0)[:, 64:64 + NE], UT, selb, True, True, ['UT', 'selb'], [pk(0)])
            mm(bank(0)[:, 128:128 + NE], ONEb, selb, True, True, ['ONEb', 'selb'], [pk(0)])
            tt('dve', pos, bank(0)[:, 64:64 + NE], carry, ALU.add, [pk(0), 'carry'], ['pos'])
            tt('dve', carry, carry, bank(0)[:, 128:128 + NE], ALU.add, [pk(0), 'carry'], ['carry'])
            yield
            stt('dve', flat, pos, float(CAP - 1), EC, ALU.min, ALU.add, ['pos', 'EC'], ['flat'])
            memset('dve', idxf, 0.0, [], ['idxf'])
            for k in range(4):
                stt('dve', ohj, lg, mx8[:, k:k + 1], flat, ALU.is_equal, ALU.mult, ['lg', 'mx8', 'flat', 'idxf'], ['ohj', 'idxf'],
                    accum=idxf[:, k:k + 1])
            cp('dve', IDXT[:, n * 4:(n + 1) * 4], idxf, ['idxf'], [f'IDX{n}'])
            for k in range(4):
                op('pool', 'indirect_dma_start', ['h2', f'IDX{n}'] + ZKEYS, [f'xgd{n}_{k}'], dma=True,
                   out=xg_d[:, :], out_offset=bass.IndirectOffsetOnAxis(ap=IDXT[:, n * 4 + k:n * 4 + k + 1], axis=0),
                   in_=h2, in_offset=None)
            yield

        def run_all(g):
            for _ in g:
                pass

        def nsteps_scores(n):
            return ((n + 1) * 128 + 511) // 512 + NIT + 3

        def interleave(items):
            done = [0] * len(items)
            while True:
                best = None
                for i, (g, q) in enumerate(items):
                    if done[i] >= q:
                        continue
                    frac = done[i] / q
                    if best is None or frac < best[0]:
                        best = (frac, i)
                if best is None:
                    break
                i = best[1]
                try:
                    next(items[i][0])
                except StopIteration:
                    pass
                done[i] += 1

        gens = {0: stage_scores(0)}
        prog_ = {0: 0}
        run_all(gens[0])
        prog_[0] = nsteps_scores(0) + 1
        if NT > 1:
            gens[1] = stage_scores(1)
            prog_[1] = 0
        for n in range(NT + 1):
            items = []
            if n < NT:
                items.append([stage_attn(n), n + 3])
            if n >= 1:
                items.append([stage_tail(n - 1), 9])
            if n + 1 < NT:
                tot = nsteps_scores(n + 1) + 1
                items.append([gens[n + 1], tot - prog_[n + 1]])
                prog_[n + 1] = tot
            if n + 2 < NT:
                gens[n + 2] = stage_scores(n + 2)
                half = (nsteps_scores(n + 2) + 1) // 2
                items.append([gens[n + 2], half])
                prog_[n + 2] = half
            interleave(items)
        if "d_gates" in dbg_d:
            dma('sp', dbg_d["d_gates"][:, :], GATES, [f'GATES{n}' for n in range(NT)], [])
        if "d_idx" in dbg_d:
            cp('dve', tmp[:, 0:128], IDXT, [f'IDX{n}' for n in range(NT)], ['tmp'])
            dma('sp', dbg_d["d_idx"][:, :], tmp[:, 0:128], ['tmp'], [])
        S.barrier()
        off[0] = persist_small

        if stage == 'B2':
            S.enabled = False
        Wgu = [alloc(8 * 2048, BF16), alloc(8 * 2048, BF16)]
        Wdn = [alloc(8 * 1024, BF16)]
        XRb = [alloc(1024, BF16) for _ in range(NSLOT)]
        XT = alloc(8 * CAP, BF16)
        XTv = XT.rearrange("p (kc s) -> p kc s", s=CAP)
        Aact = alloc(8 * CAP, BF16)
        Av = Aact.rearrange("p (m s) -> p m s", s=CAP)
        BD = alloc(1024)
        BGU = alloc(NE * 16)
        gq = alloc(512)
        sgm = alloc(512)
        uq = alloc(512)
        tq = alloc(512)
        yo = [alloc(1024), alloc(1024)]
        dma('sp', BGU, bgu_d[:, :], [], ['BGU'])
        BGA = alloc(NE * 16)
        F7 = float(7.0 / (1.0 + np.exp(-1.702 * 7.0)))
        BGUv = BGU.rearrange("p (e c) -> p e c", c=16)
        BGAv = BGA.rearrange("p (e c) -> p e c", c=16)
        ts('dve', BGAv[:, :, 0:8], BGUv[:, :, 0:8], 1.702, ALU.mult, ['BGU'], ['BGA'])
        ts('dve', BGAv[:, :, 8:16], BGUv[:, :, 8:16], 1.0, ALU.add, ['BGU'], ['BGA'])

        STG = [alloc(2048) for _ in range(3)]
        chunks = []
        for kc_ in range(8):
            chunks.append(('g', 0, kc_))
        for q_ in range(4):
            chunks.append(('d', 0, q_))
        for ex_ in range(NE):
            for m_ in range(8):
                if ex_ >= 1 and m_ < 4:
                    chunks.append(('d', ex_, m_))
                if ex_ + 1 < NE:
                    chunks.append(('g', ex_ + 1, m_))
        issued = [0]

        def issue_next():
            i = issued[0]
            if i >= len(chunks):
                return
            issued[0] += 1
            kind, ex_, c_ = chunks[i]
            st = STG[i % 3]
            if kind == 'g':
                dma('act', st, wgu_d[ex_, c_ * 128:(c_ + 1) * 128, :], [], [f'stg{i % 3}'])
            else:
                dma('act', st.rearrange("p (m n) -> p m n", n=1024),
                    wd_d[ex_, 2 * c_ * 128:(2 * c_ + 2) * 128, :].rearrange("(m p) n -> p m n", p=128), [], [f'stg{i % 3}'])

        casted = [0]

        def cast_next():
            i = casted[0]
            casted[0] += 1
            kind, ex_, c_ = chunks[i]
            st = STG[i % 3]
            if kind == 'g':
                dst = Wgu[ex_ % 2].rearrange("p (kc n) -> p kc n", n=2048)[:, c_, :]
                cp('act', dst, st, [f'stg{i % 3}'], [f'Wgu{ex_ % 2}_{c_}'])
            else:
                dst = Wdn[0][:, 2 * c_ * 1024:(2 * c_ + 2) * 1024]
                cp('act', dst, st, [f'stg{i % 3}'], [f'Wdn_{2 * c_}', f'Wdn_{2 * c_ + 1}'])
            issue_next()

        for _ in range(3):
            issue_next()
        for _ in range(12):
            cast_next()
        NH = CAP // 512

        def load_xr(ex):
            for a in range(NSLOT):
                dma('sp', XRb[a], xg_d[ex * CAP + a * 128:ex * CAP + (a + 1) * 128, :], [], [f'XR{a}'])

        def prep_xt(ex):
            for a in range(NSLOT):
                b = a % 2
                XR = XRb[a]
                for kc in range(8):
                    tp(bankb(b)[:, kc * 128:(kc + 1) * 128], XR[:, kc * 128:(kc + 1) * 128], identb,
                       [f'XR{a}', 'identb'], [pk(b)])
                cp('dve', XTv[:, :, a * 128:(a + 1) * 128], bankb(b).rearrange("p (kc s) -> p kc s", s=128),
                   [pk(b)], ['XT'])

        load_xr(0)
        prep_xt(0)
        for ex in range(NE):
            eb = ex % 2
            Wg = Wgu[eb]
            Wgv = Wg.rearrange("p (kc n) -> p kc n", n=2048)
            Wd = Wdn[0]
            Wdv = Wd.rearrange("p (kc n) -> p kc n", n=1024)
            dma('sp', BD, bd_d[ex:ex + 1, :].partition_broadcast(128), [], ['BD'])
            for m in range(8):
                if ex >= 1 and m < 4:
                    cast_next()
                if ex + 1 < NE:
                    cast_next()
                bg = BGA[:, ex * 16 + m:ex * 16 + m + 1]
                bu = BGA[:, ex * 16 + 8 + m:ex * 16 + 8 + m + 1]
                for hh in range(NH):
                    cs_ = slice(hh * 512, (hh + 1) * 512)
                    gb = 2 + hh % 2
                    ub = 4 + hh % 2
                    for (bb_, c0) in ((gb, m * 128), (ub, 1024 + m * 128)):
                        for kc in range(8):
                            mm(bank(bb_), Wgv[:, kc, c0:c0 + 128], XTv[:, kc, cs_], kc == 0, kc == 7,
                               [f'Wgu{eb}_{kc}', 'XT'], [pk(bb_)])
                    act(sgm, bank(gb), AF.Silu, [pk(gb), 'BGA'], ['sgm'], bias=bg, scale=1.702)
                    ts('dve', uq, bank(ub), bu, ALU.add, [pk(ub), 'BGA'], ['uq'], s2=8.0, op1=ALU.min)
                    ts('dve', tq, sgm, 1.0 / 1.702, ALU.mult, ['sgm'], ['tq'], s2=F7, op1=ALU.min)
                    stt('dve', Av[:, m, cs_], uq, -6.0, tq, ALU.max, ALU.mult, ['uq', 'tq'], ['Aact'])
                if m == 0 and ex + 1 < NE:
                    load_xr(ex + 1)
            if ex + 1 < NE:
                prep_xt(ex + 1)
            for a in range(NSLOT):
                Yp = PS[3 if a % 2 == 0 else 0][:, :]
                yk = [pk(6), pk(7)] if a % 2 == 0 else [pk(0), pk(1)]
                for half in range(2):
                    for m in range(8):
                        mm(Yp[:, half * 512:(half + 1) * 512], Av[:, m, a * 128:(a + 1) * 128], Wdv[:, m, half * 512:(half + 1) * 512],
                           m == 0, m == 7, ['Aact', f'Wdn_{m}'], yk)
                tt('dve', yo[a % 2], Yp, BD, ALU.add, yk + ['BD'], [f'yo{a % 2}'])
                dma('sp', y_d[ex * CAP + a * 128:ex * CAP + (a + 1) * 128, :], yo[a % 2], [f'yo{a % 2}'], [f'yd{ex}_{a}'])
        S.barrier()
        off[0] = persist_small

        if stage == 'C':
            S.enabled = False
        YG2 = [[alloc(1024) for _ in range(4)] for _ in range(2)]
        x1b = [alloc(1024), alloc(1024)]
        accb = alloc(1024)
        tmp = alloc(1024)
        ob = [alloc(1024), alloc(1024)]
        LN2G = alloc(1024)
        LN2B = alloc(1024)
        lst = alloc(12)
        lmv = alloc(2)
        lsd = alloc(1)
        lrs = alloc(1)
        dma('sp', LN2G, ln2g_d.partition_broadcast(128), [], ['LN2G'])
        dma('sp', LN2B, ln2b_d.partition_broadcast(128), [], ['LN2B'])
        for n in range(NT):
            x1t = x1b[n % 2]
            YG = YG2[n % 2]
            dma('sp', x1t, x1_d[n * 128:(n + 1) * 128, :], [f'x1d{n}'], [f'x1b{n % 2}'])
            for k in range(4):
                op('pool', 'indirect_dma_start', [f'IDX{n}'], [f'YG{n % 2}_{k}'], dma=True,
                   out=YG[k], out_offset=None, in_=y_d[:, :],
                   in_offset=bass.IndirectOffsetOnAxis(ap=IDXT[:, n * 4 + k:n * 4 + k + 1], axis=0))
            ts('dve', accb, YG[0], GATES[:, n * 4:n * 4 + 1], ALU.mult, [f'YG{n % 2}_0', f'GATES{n}'], ['accb'])
            for k in range(1, 4):
                stt('dve', accb, YG[k], GATES[:, n * 4 + k:n * 4 + k + 1], accb, ALU.mult, ALU.add, [f'YG{n % 2}_{k}', f'GATES{n}', 'accb'], ['accb'])
            if "d_ff" in dbg_d:
                dma('sp', dbg_d["d_ff"][n * 128:(n + 1) * 128, :], accb, ['accb'], [])
            tt('dve', tmp, accb, GF1, ALU.mult, ['accb', 'modB'], ['tmp'])
            stt('dve', tmp, x1t, ALPHA, tmp, ALU.mult, ALU.add, [f'x1b{n % 2}', 'tmp'], ['tmp'])
            layer_norm('dve', tmp, LN2G, LN2B, ob[n % 2], ['tmp'], f'ob{n % 2}', 'LN2G', 'LN2B')
            dma('sp', out_d[n * 128:(n + 1) * 128, :], ob[n % 2], [f'ob{n % 2}'], [f'outd{n}'])

        sems = {}
        for e in ENGS:
            sems[('e', e)] = es.enter_context(nc.semaphore(f"se_{e}"))
        for k in S.dma_cnt:
            sems[k] = es.enter_context(nc.semaphore(f"sd_{k[1]}_{k[2]}"))
        final = S.all_tokens()
        blk = es.enter_context(nc.Block())
        bmap = {'pe': blk.tensor, 'act': blk.scalar, 'dve': blk.vector, 'pool': blk.gpsimd, 'sp': blk.sync}

        def make(eng):
            def body(e):
                for waits, fn, tok, inc in S.prog[eng]:
                    for k, v in waits:
                        e.wait_ge(sems[k], v)
                    inst = fn(e)
                    inst.then_inc(sems[tok[0]], inc)
                if eng == 'sp':
                    for k, v in final.items():
                        e.wait_ge(sems[k], v)
            return body
        for eng in ENGS:
            bmap[eng](make(eng))
    return nc


def host_consts():
    f32 = np.float32
    s = np.arange(128)[:, None]
    t = np.arange(128)[None, :]
    gam = (1.0 - 2.0 ** (-5.0 - np.arange(4))).astype(np.float64)
    dtab = np.zeros((128, 4, 128), f32)
    qd = np.zeros((128, 4, 128), f32)
    chtab = np.zeros((128, 4, 128), f32)
    kd = np.zeros((128, 4), f32)
    for h in range(4):
        diff = (t - s)
        dtab[:, h, :] = np.where(diff >= 0, gam[h] ** np.maximum(diff, 0), 0.0) / np.sqrt(128.0)
        qd[:, h, :] = (gam[h] ** (np.arange(128) + 1.0))[None, :]
        chtab[:, h, :] = gam[h] ** 128.0
        kd[:, h] = gam[h] ** (127.0 - np.arange(128)) / np.sqrt(128.0)
    inv = 1.0 / (10000.0 ** (np.arange(0, 128, 2, dtype=np.float32) / 128.0))
    ang = np.arange(L, dtype=np.float32)[:, None] * inv[None, :].astype(np.float32)
    cos = np.cos(ang).astype(f32)
    sin = np.sin(ang).astype(f32)
    negd = np.where(t > s, -1e30, 0.0).astype(f32)
    negd = np.where(np.arange(128)[None, :] > np.arange(128)[:, None], -1e30, 0.0).astype(f32)
    ut = (np.arange(128)[:, None] < np.arange(128)[None, :]).astype(f32)
    pow2 = np.tile((2.0 ** -np.arange(NIT + 2)).astype(f32)[None, :], (128, 1))
    ec = np.tile((np.arange(NE) * CAP).astype(f32)[None, :], (128, 1))
    ident = np.eye(128, dtype=f32)
    dist0 = t - s
    dist1 = t - s + 128
    b0 = t5_bucket_np(dist0)
    b1 = t5_bucket_np(dist1)
    return dict(dtab=dtab.reshape(128, 512), qdtab=qd.reshape(128, 512), chtab=chtab.reshape(128, 512), kdtab=kd,
                cos=cos, sin=sin, negd=negd, ut=ut, pow2=pow2, ec=ec, ident=ident), b0, b1, (dist0 >= 0)


_PERM = None


def _perm():
    idx = []
    for hh in [0, 4, 1, 5, 2, 6, 3, 7]:
        idx += list(range(hh * 64, hh * 64 + 64))
    idx += list(range(512, 640))
    idx += list(range(768, 1024))
    idx += list(range(1024, 1088)) * 2
    idx += list(range(640, 768))
    idx += list(range(1088, 1092))
    idx += list(range(1092, 2116))
    idx += list(range(2116, 2628))
    idx += list(range(2628, 3140))
    assert len(idx) == NCOL
    return np.array(idx)


def make_in_maps(inputs, cores):
    f32 = np.float32
    consts, b0, b1, causal = host_consts()
    rel_bias = np.asarray(inputs["rel_bias"], f32)
    bt = np.zeros((128, 2, 8, 128), f32)
    g0 = rel_bias[b0]
    g1 = rel_bias[b1]
    bt[:, 0] = np.where(causal[:, None, :], np.transpose(g0, (0, 2, 1)), 0.0)
    bt[:, 1] = np.transpose(g1, (0, 2, 1))
    c31 = np.tile(rel_bias[31][None, :], (128, 1)).astype(f32)
    w_in = np.ascontiguousarray(np.asarray(inputs["w_in"][0], f32)[:, _perm()])
    bgu = np.ascontiguousarray(np.asarray(inputs["b_gate_up"][0], f32).reshape(NE, 16, 128).transpose(2, 0, 1)).reshape(128, NE * 16)
    shared = dict(
        w_ada=np.ascontiguousarray(inputs["w_ada"][0], dtype=f32), b_ada=np.ascontiguousarray(inputs["b_ada"], dtype=f32).reshape(1, -1),
        w_in=w_in, w_out=np.ascontiguousarray(inputs["w_out"][0], dtype=f32),
        bt=bt.reshape(128, 2048), c31=c31,
        rng=np.asarray(inputs["ret_norm_g"], f32).reshape(1, 512),
        ln1g=np.asarray(inputs["ln1_g"], f32).reshape(1, D), ln1b=np.asarray(inputs["ln1_b"], f32).reshape(1, D),
        ln2g=np.asarray(inputs["ln2_g"], f32).reshape(1, D), ln2b=np.asarray(inputs["ln2_b"], f32).reshape(1, D),
        wr=np.ascontiguousarray(inputs["w_router"][0], dtype=f32), br=np.asarray(inputs["b_router"], f32).reshape(1, NE),
        wgu=np.ascontiguousarray(inputs["w_gate_up"][0], dtype=f32), bgu=bgu,
        wd=np.ascontiguousarray(inputs["w_down"][0], dtype=f32), bd=np.ascontiguousarray(inputs["b_down"][0], dtype=f32),
    )
    shared.update(consts)
    maps = []
    for b in cores:
        m = dict(shared)
        m["x"] = np.ascontiguousarray(inputs["x"][b], dtype=f32)
        m["c8"] = np.ascontiguousarray(np.asarray(inputs["c"][b], f32).reshape(8, 128).T)
        maps.append(m)
    return maps


def kernel(**inputs):
    nc = build_nc()
    maps = make_in_maps(inputs, list(range(8)))
    res = run_bass_kernel_spmd(nc, maps, core_ids=list(range(8)))
    out = np.stack([np.asarray(r["out"], np.float32) for r in res.results], axis=0)
    return out
```

```python
import os
import contextlib
import numpy as np
import ml_dtypes
import concourse.bass as bass
import concourse.mybir as mybir
from concourse.bass_utils import run_bass_kernel_spmd

F32 = mybir.dt.float32
BF16 = mybir.dt.bfloat16
I32 = mybir.dt.int32
AF = mybir.ActivationFunctionType
ALU = mybir.AluOpType
AX = mybir.AxisListType

L = 4096
D = 1024
NT = 32
NCOL = 3204
NE = 32
CAP = 1024
NSLOT = CAP // 128
NIT = 18
ALPHA = float(2.0 ** 0.25)
EPS = 1e-5
ENGS = ['pe', 'act', 'dve', 'pool', 'sp']
NDMA = 8


class Sched:
    def __init__(self):
        self.prog = {e: [] for e in ENGS}
        self.seq = {e: 0 for e in ENGS}
        self.res = {}
        self.waited = {e: {} for e in ENGS}
        self.dma_rr = {e: 0 for e in ENGS}
        self.dma_cnt = {}
        self.pending = {e: {} for e in ENGS}
        self.enabled = True

    def add(self, eng, fn, r=(), w=(), dma=False, grp=None):
        if not self.enabled:
            return None
        needs = dict(self.pending[eng])
        self.pending[eng] = {}

        def need(tok):
            if tok is None:
                return
            k, v = tok
            if needs.get(k, 0) < v:
                needs[k] = v
        for key in r:
            st = self.res.get(key)
            if st:
                need(st[0])
        for key in w:
            st = self.res.get(key)
            if st:
                need(st[0])
                for k, v in st[1].items():
                    need((k, v))
        if dma:
            gname = grp or eng
            j = self.dma_rr.get(gname, 0)
            self.dma_rr[gname] = (j + 1) % NDMA
            semk = ('d', gname, j)
            prev = self.dma_cnt.get(semk, 0)
            if prev:
                need((semk, 16 * prev))
            self.dma_cnt[semk] = prev + 1
            tok = (semk, 16 * (prev + 1))
            inc = 16
        else:
            self.seq[eng] += 1
            tok = (('e', eng), self.seq[eng])
            inc = 1
        waits = []
        for k, v in needs.items():
            if k == ('e', 'pe') and eng == 'pe' and not dma:
                continue
            if self.waited[eng].get(k, 0) >= v:
                continue
            self.waited[eng][k] = v
            waits.append((k, v))
        self.prog[eng].append((waits, fn, tok, inc))
        for key in w:
            self.res[key] = [tok, {}]
        for key in r:
            if key in w:
                continue
            st = self.res.setdefault(key, [None, {}])
            if st[1].get(tok[0], 0) < tok[1]:
                st[1][tok[0]] = tok[1]
        return tok

    def cut(self, name):
        if os.environ.get('KCUT', '') == name:
            self.enabled = False

    def all_tokens(self):
        toks = {}
        for e in ENGS:
            if self.seq[e]:
                toks[('e', e)] = self.seq[e]
        for k, c in self.dma_cnt.items():
            toks[k] = 16 * c
        return toks

    def barrier(self, exclude=()):
        toks = self.all_tokens()
        for e in ENGS:
            for k, v in toks.items():
                if k[0] == 'd' and k[1] in exclude:
                    continue
                if self.pending[e].get(k, 0) < v:
                    self.pending[e][k] = v


def t5_bucket_np(n):
    n = np.maximum(n, 0)
    nf = np.maximum(n, 1).astype(np.float32)
    large = 16 + (np.log(nf / np.float32(16)) / np.float32(np.log(128 / 16)) * np.float32(16)).astype(np.int32)
    large = np.minimum(large, 31)
    return np.where(n < 16, n, large)


def build_nc(dbg=(), stage='D'):
    nc = bass.Bass("TRN2", target_bir_lowering=False)
    S = Sched()

    def din(name, shape, dt=F32):
        return nc.dram_tensor(name, list(shape), dt, kind="ExternalInput").ap()

    x_d = din("x", [L, D])
    c8_d = din("c8", [128, 8])
    wada_d = din("w_ada", [D, 6 * D])
    bada_d = din("b_ada", [1, 6 * D])
    win_d = din("w_in", [D, NCOL])
    wout_d = din("w_out", [D, D])
    bt_d = din("bt", [128, 2048])
    c31_d = din("c31", [128, 8])
    cos_d = din("cos", [L, 64])
    sin_d = din("sin", [L, 64])
    dt_d = din("dtab", [128, 512])
    qd_d = din("qdtab", [128, 512])
    ch_d = din("chtab", [128, 512])
    kd_d = din("kdtab", [128, 4])
    rng_d = din("rng", [1, 512])
    ln1g_d = din("ln1g", [1, D])
    ln1b_d = din("ln1b", [1, D])
    ln2g_d = din("ln2g", [1, D])
    ln2b_d = din("ln2b", [1, D])
    wr_d = din("wr", [D, NE])
    br_d = din("br", [1, NE])
    wgu_d = din("wgu", [NE, D, 2 * D])
    bgu_d = din("bgu", [128, NE * 16])
    wd_d = din("wd", [NE, D, D])
    bd_d = din("bd", [NE, D])
    negd_d = din("negd", [128, 128])
    ut_d = din("ut", [128, 128])
    pow_d = din("pow2", [128, NIT + 2])
    ec_d = din("ec", [128, NE])
    id_d = din("ident", [128, 128])
    out_d = nc.dram_tensor("out", [L, D], F32, kind="ExternalOutput").ap()
    dbg_d = {}
    for name, shape in dbg:
        dbg_d[name] = nc.dram_tensor(name, list(shape), F32, kind="ExternalOutput").ap()

    qs_d = nc.dram_tensor("qs_scr", [NT, 128, 768], BF16, kind="Internal").ap()
    ret_d = nc.dram_tensor("ret_scr", [L, 512], BF16, kind="Internal").ap()
    x1_d = nc.dram_tensor("x1_scr", [L, D], F32, kind="Internal").ap()
    xg_d = nc.dram_tensor("xg_scr", [NE * CAP, D], BF16, kind="Internal").ap()
    y_d = nc.dram_tensor("y_scr", [NE * CAP, D], F32, kind="Internal").ap()

    with contextlib.ExitStack() as es:
        ARENA_W = 53000
        arena = es.enter_context(nc.sbuf_tensor("arena", [128, ARENA_W], F32))
        PS = [es.enter_context(nc.psum_tensor(f"ps{i}", [128, 1024], F32)) for i in range(4)]
        off = [0]

        def alloc(n, dt=F32):
            n32 = n if dt in (F32, I32) else (n + 1) // 2
            assert off[0] + n32 <= ARENA_W, (off[0], n32)
            a = arena[:, off[0]:off[0] + n32]
            off[0] += n32
            if dt == F32:
                return a
            return a.bitcast(dt)

        def bank(b):
            return PS[b // 2][:, (b % 2) * 512:(b % 2) * 512 + 512]

        def bankb(b):
            return bank(b).bitcast(BF16)

        def pk(b):
            return f"ps{b}"

        def mm(out, lhsT, rhs, start, stop, r, w, skip=False):
            if skip:
                S.add('pe', lambda e: e.matmul(out, lhsT=lhsT, rhs=rhs, start=start, stop=stop, skip_group_check=True), r, w)
            else:
                S.add('pe', lambda e: e.matmul(out, lhsT=lhsT, rhs=rhs, start=start, stop=stop), r, w)

        def tp(out, in_, idn, r, w):
            S.add('pe', lambda e: e.transpose(out=out, in_=in_, identity=idn), r, w)

        def dma(eng, out, in_, r, w):
            return S.add(eng, lambda e: e.dma_start(out=out, in_=in_), r, w, dma=True)

        def act(out, in_, func, r, w, bias=0.0, scale=1.0, accum=None):
            if accum is None:
                S.add('act', lambda e: e.activation(out=out, in_=in_, func=func, bias=bias, scale=scale), r, w)
            else:
                S.add('act', lambda e: e.activation(out=out, in_=in_, func=func, bias=bias, scale=scale, accum_out=accum), r, w)

        def cp(eng, out, in_, r, w):
            if eng == 'act':
                S.add('act', lambda e: e.copy(out=out, in_=in_), r, w)
            else:
                S.add(eng, lambda e: e.tensor_copy(out=out, in_=in_), r, w)

        def tt(eng, out, in0, in1, op, r, w):
            S.add(eng, lambda e: e.tensor_tensor(out=out, in0=in0, in1=in1, op=op), r, w)

        def ts(eng, out, in0, s1, op0, r, w, s2=None, op1=None, accum=None):
            if op1 is None:
                S.add(eng, lambda e: e.tensor_scalar(out=out, in0=in0, scalar1=s1, scalar2=None, op0=op0), r, w)
            elif accum is None:
                S.add(eng, lambda e: e.tensor_scalar(out=out, in0=in0, scalar1=s1, scalar2=s2, op0=op0, op1=op1), r, w)
            else:
                S.add(eng, lambda e: e.tensor_scalar(out=out, in0=in0, scalar1=s1, scalar2=s2, op0=op0, op1=op1, accum_out=accum), r, w)

        def stt(eng, out, in0, scalar, in1, op0, op1, r, w, accum=None):
            if accum is None:
                S.add(eng, lambda e: e.scalar_tensor_tensor(out=out, in0=in0, scalar=scalar, in1=in1, op0=op0, op1=op1), r, w)
            else:
                S.add(eng, lambda e: e.scalar_tensor_tensor(out=out, in0=in0, scalar=scalar, in1=in1, op0=op0, op1=op1, accum_out=accum), r, w)

        def op(eng, name, r, w, dma=False, **kw):
            return S.add(eng, lambda e: getattr(e, name)(**kw), r, w, dma=dma)

        def memset(eng, out, val, r, w):
            S.add(eng, lambda e: e.memset(out, val), r, w)

        def dbg_out(name, src, r, rows=None):
            if name in dbg_d:
                dst = dbg_d[name] if rows is None else dbg_d[name][rows[0]:rows[1], :]
                dma('sp', dst, src, r, [])

        modB = alloc(4096)
        GA1 = modB[:, 0:1024]
        SHF = modB[:, 1024:2048]
        SCF1 = modB[:, 2048:3072]
        GF1 = modB[:, 3072:4096]
        ident = alloc(128)
        identb = alloc(128, BF16)
        SHA = alloc(8)
        SCA1 = alloc(8)
        WI = alloc(NT * 4)
        GATES = alloc(NT * 4)
        IDXT = alloc(NT * 4, I32)
        carry = alloc(NE)
        ZT = alloc(4 * 1024, BF16)
        persist_small = off[0]
        KT = alloc(L, BF16)
        KIT = alloc(L, BF16)
        V1 = alloc(NT * 2 * 65 + 1, BF16)[:, 0:NT * 2 * 65]
        V1v = V1.rearrange("p (n g d) -> p n g d", g=2, d=65)
        wout = alloc(8 * 1024, BF16)
        woutv = wout.rearrange("p (kc n) -> p kc n", n=1024)
        wrb = alloc(8 * NE, BF16)
        wrv = wrb.rearrange("p (kc n) -> p kc n", n=NE)
        UT = alloc(128, BF16)
        persist_off = off[0]

        dma('sp', ident, id_d[:, :], [], ['ident'])
        cp('dve', identb, ident, ['ident'], ['identb'])
        memset('pool', V1, 1.0, [], ['V1'])
        memset('pool', carry, 0.0, [], ['carry'])

        c8 = alloc(8)
        csil = alloc(8)
        ones = alloc(128)
        cB = alloc(8 * 128)
        modA = alloc(2048)
        wab = [alloc(8 * 512), alloc(8 * 512)]
        badab = [alloc(512), alloc(512)]
        dma('sp', c8, c8_d[:, :], [], ['c8'])
        act(csil, c8, AF.Silu, ['c8'], ['csil'])
        memset('dve', ones, 1.0, [], ['ones'])
        for kc in range(8):
            ts('dve', cB[:, kc * 128:(kc + 1) * 128], ones, csil[:, kc:kc + 1], ALU.mult, ['ones', 'csil'], ['cB'])
        wada_v = wada_d.rearrange("(kc p) n -> p kc n", p=128)
        for ch in range(12):
            wb = wab[ch % 2]
            bb = badab[ch % 2]
            wbv = wb.rearrange("p (kc n) -> p kc n", n=512)
            dma('sp', wbv, wada_v[:, :, ch * 512:(ch + 1) * 512], [], [f'wab{ch % 2}'])
            dma('sp', bb, bada_d[:, ch * 512:(ch + 1) * 512].partition_broadcast(128), [], [f'badab{ch % 2}'])
            b = ch % 2
            for kc in range(8):
                mm(bank(b), cB[:, kc * 128:(kc + 1) * 128], wbv[:, kc, :], kc == 0, kc == 7,
                   ['cB', f'wab{ch % 2}'], [pk(b)])
            if ch < 4:
                dst = modA[:, ch * 512:(ch + 1) * 512]
                dkey = 'modA'
            else:
                dst = modB[:, (ch - 4) * 512:(ch - 3) * 512]
                dkey = 'modB'
            tt('dve', dst, bank(b), bb, ALU.add, [pk(b), f'badab{ch % 2}'], [dkey])
        ts('dve', modA[:, 1024:2048], modA[:, 1024:2048], 1.0, ALU.add, ['modA'], ['modA'])
        ts('dve', GA1, GA1, 1.0, ALU.add, ['modB'], ['modB'])
        ts('dve', SCF1, SCF1, 1.0, ALU.add, ['modB'], ['modB'])
        ts('dve', GF1, GF1, 1.0, ALU.add, ['modB'], ['modB'])
        for c in range(8):
            tp(bank(2 + (c % 2))[:, 0:128], modA[:, c * 128:(c + 1) * 128], ident, ['modA', 'ident'], [pk(2 + (c % 2))])
            cp('dve', SHA[:, c:c + 1], bank(2 + (c % 2))[:, 0:1], [pk(2 + (c % 2))], ['SHA'])
        for c in range(8):
            tp(bank(2 + (c % 2))[:, 0:128], modA[:, 1024 + c * 128:1024 + (c + 1) * 128], ident, ['modA', 'ident'], [pk(2 + (c % 2))])
            cp('dve', SCA1[:, c:c + 1], bank(2 + (c % 2))[:, 0:1], [pk(2 + (c % 2))], ['SCA1'])
        dbg_out("d_modB", modB, ['modB'])
        S.barrier(exclude=('zf',))
        off[0] = persist_off

        if stage == 'A':
            S.enabled = False
        win = alloc(8 * NCOL, BF16)
        winv = win.rearrange("p (kc n) -> p kc n", n=NCOL)
        xtb = [alloc(1024), alloc(1024)]
        csb = [alloc(64), alloc(64)]
        snb = [alloc(64), alloc(64)]
        hT = alloc(1024, BF16)
        tmA = alloc(512, BF16)
        tmB = alloc(512, BF16)
        QS = alloc(768, BF16)
        t1 = alloc(256)
        t2 = alloc(256)
        t3 = alloc(256)
        t4 = alloc(256)
        qkrot_b = [alloc(1024, BF16), alloc(1024, BF16)]
        qkT = alloc(1024, BF16)
        qdT = alloc(512, BF16)
        kdk = alloc(512, BF16)
        ATb = alloc(512, BF16)
        VR_b = [alloc(512, BF16), alloc(512, BF16)]
        SG_b = [alloc(512), alloc(512)]
        S32 = alloc(512)
        S16 = alloc(512, BF16)
        DTt = alloc(512)
        QDt = alloc(512)
        CHt = alloc(512)
        KDt = alloc(4)
        RNG = alloc(512)
        rn = alloc(512)
        retb = alloc(512, BF16)
        bst = alloc(24)
        bmv = alloc(8)
        sd4 = alloc(4)
        rs4 = alloc(4)

        win_v = win_d.rearrange("(kc p) n -> p kc n", p=128)
        for kc in range(8):
            dma('pool', winv[:, kc, :], win_v[:, kc, :], [], [f'win{kc}'])
        for kc in range(8):
            dma('pool', woutv[:, kc, :], wout_d[kc * 128:(kc + 1) * 128, :], [], [f'wout{kc}'])
        dma('pool', wrv, wr_d.rearrange("(kc p) n -> p kc n", p=128), [], ['wrb'])
        dma('pool', UT, ut_d[:, :], [], ['UT'])
        memset('dve', ZT, 0.0, [], ['ZT'])
        ZKEYS = []
        for zi in range(NE * CAP // 512):
            ZKEYS.append(f'xgz{zi}')
            S.add('pool', (lambda z: (lambda e: e.dma_start(out=xg_d[z * 512:(z + 1) * 512, :].rearrange("(a p) f -> p a f", p=128),
                                                            in_=ZT.rearrange("p (a f) -> p a f", f=1024))))(zi),
                  ['ZT'], [f'xgz{zi}'], dma=True, grp='zf')
        dma('sp', DTt, dt_d[:, :], [], ['DTt'])
        dma('sp', QDt, qd_d[:, :], [], ['QDt'])
        dma('sp', CHt, ch_d[:, :], [], ['CHt'])
        dma('sp', KDt, kd_d[:, :], [], ['KDt'])
        dma('sp', RNG, rng_d.partition_broadcast(128), [], ['RNG'])

        S.cut('pro')
        CH = [(0, 512), (512, 512), (1024, 132), (1156, 512), (1668, 512), (2180, 512), (2692, 512)]

        def proj_chunk(ci, b):
            c0, wd = CH[ci]
            for kc in range(8):
                mm(bank(b)[:, 0:wd], hT[:, kc * 128:(kc + 1) * 128], winv[:, kc, c0:c0 + wd], kc == 0, kc == 7,
                   ['hT', f'win{kc}'], [pk(b)])

        def b1_proj(n):
            xt = xtb[n % 2]
            xk = f'xt{n % 2}'
            qkrot, VR, SG = qkrot_b[n % 2], VR_b[n % 2], SG_b[n % 2]
            cs = csb[n % 2]
            sn = snb[n % 2]
            dma('sp', xt, x_d[n * 128:(n + 1) * 128, :], [], [xk])
            dma('sp', cs, cos_d[n * 128:(n + 1) * 128, :], [], [f'cs{n % 2}'])
            dma('sp', sn, sin_d[n * 128:(n + 1) * 128, :], [], [f'sn{n % 2}'])
            for c in range(8):
                b = 0 if c < 4 else 1
                tp(bank(b)[:, (c % 4) * 128:(c % 4 + 1) * 128], xt[:, c * 128:(c + 1) * 128], ident, [xk, 'ident'], [pk(b)])
            for c in range(8):
                b = 0 if c < 4 else 1
                ts('dve', hT[:, c * 128:(c + 1) * 128], bank(b)[:, (c % 4) * 128:(c % 4 + 1) * 128], SCA1[:, c:c + 1], ALU.mult,
                   [pk(b), 'SHA', 'SCA1'], ['hT'], s2=SHA[:, c:c + 1], op1=ALU.add)
            yield
            proj_chunk(0, 2)
            cp('dve', tmA, bank(2), [pk(2)], ['tmA'])
            yield
            proj_chunk(1, 3)
            cp('act', tmB, bank(3), [pk(3)], ['tmB'])
            yield
            for c in range(4):
                tp(bankb(4)[:, c * 128:(c + 1) * 128], tmA[:, c * 128:(c + 1) * 128], identb, ['tmA', 'identb'], [pk(4)])
            for c in range(4):
                tp(bankb(4)[:, 512 + c * 128:512 + (c + 1) * 128], tmB[:, c * 128:(c + 1) * 128], identb, ['tmB', 'identb'], [pk(4)])
            cp('dve', QS[:, 0:512], bankb(4)[:, 0:512], [pk(4)], ['QS'])
            cp('dve', QS[:, 512:768], bankb(4)[:, 640:896], [pk(4)], ['QS'])
            cp('dve', KT[:, n * 128:(n + 1) * 128], bankb(4)[:, 512:640], [pk(4)], [f'KT{n}'])
            cp('dve', KIT[:, n * 128:(n + 1) * 128], bankb(4)[:, 896:1024], [pk(4)], [f'KIT{n}'])
            dma('sp', qs_d[n, :, :], QS, ['QS'], [f'qsd{n}'])
            yield
            proj_chunk(2, 2)
            cp('dve', V1v[:, n, :, 0:64], bank(2)[:, 0:128].rearrange("p (g d) -> p g d", d=64), [pk(2), 'V1'], [f'V1_{n}'])
            ts('dve', WI[:, n * 4:(n + 1) * 4], bank(2)[:, 128:132], 0.0625, ALU.mult, [pk(2)], [f'WI{n}'])
            yield
            proj_chunk(3, 3)
            yield
            proj_chunk(4, 2)
            cosB = cs.unsqueeze(1).to_broadcast([128, 4, 64])
            sinB = sn.unsqueeze(1).to_broadcast([128, 4, 64])
            qkv = qkrot.rearrange("p (a two d) -> p a two d", two=2, d=64)
            for which, b in ((0, 3), (1, 2)):
                pv = bank(b).rearrange("p (h two d) -> p h two d", two=2, d=64)
                x1v = pv[:, :, 0, :]
                x2v = pv[:, :, 1, :]
                t1v = t1.rearrange("p (h d) -> p h d", d=64)
                t2v = t2.rearrange("p (h d) -> p h d", d=64)
                t3v = t3.rearrange("p (h d) -> p h d", d=64)
                t4v = t4.rearrange("p (h d) -> p h d", d=64)
                ck = [f'cs{n % 2}', f'sn{n % 2}']
                tt('dve', t1v, x1v, cosB, ALU.mult, [pk(b)] + ck, ['t1'])
                tt('dve', t2v, x2v, sinB, ALU.mult, [pk(b)] + ck, ['t2'])
                tt('dve', t3v, x1v, sinB, ALU.mult, [pk(b)] + ck, ['t3'])
                tt('dve', t4v, x2v, cosB, ALU.mult, [pk(b)] + ck, ['t4'])
                tt('dve', qkv[:, which * 4:(which + 1) * 4, 0, :], t1v, t2v, ALU.subtract, ['t1', 't2'], ['qkrot' + str(n % 2)])
                tt('dve', qkv[:, which * 4:(which + 1) * 4, 1, :], t3v, t4v, ALU.add, ['t3', 't4'], ['qkrot' + str(n % 2)])
            yield
            proj_chunk(5, 3)
            cp('act', VR, bank(3), [pk(3)], ['VR' + str(n % 2)])
            yield
            proj_chunk(6, 2)
            act(SG, bank(2), AF.Silu, [pk(2)], ['SG' + str(n % 2)])
            yield

        def b1_ret(n):
            qkrot, VR, SG = qkrot_b[n % 2], VR_b[n % 2], SG_b[n % 2]
            for a in range(8):
                tp(bankb(4)[:, a * 128:(a + 1) * 128], qkrot[:, a * 128:(a + 1) * 128], identb, ['qkrot' + str(n % 2), 'identb'], [pk(4)])
            cp('dve', qkT, bankb(4), [pk(4)], ['qkT'])
            tt('dve', qdT, bankb(4)[:, 0:512], QDt, ALU.mult, [pk(4), 'QDt'], ['qdT'])
            tt('dve', kdk.rearrange("p (h d) -> p h d", d=128), qkrot[:, 512:1024].rearrange("p (h d) -> p h d", d=128),
               KDt.unsqueeze(2).to_broadcast([128, 4, 128]), ALU.mult, ['qkrot' + str(n % 2), 'KDt'], ['kdk'])
            yield
            for h in range(4):
                mm(bank(5)[:, h * 128:(h + 1) * 128], qkT[:, 512 + h * 128:512 + (h + 1) * 128], qkT[:, h * 128:(h + 1) * 128],
                   True, True, ['qkT'], [pk(5)])
            tt('dve', ATb, bank(5), DTt, ALU.mult, [pk(5), 'DTt'], ['ATb'])
            for h in range(4):
                hs = slice(h * 128, (h + 1) * 128)
                mm(bank(6)[:, hs], ATb[:, hs], VR[:, hs], True, n == 0, ['ATb', 'VR' + str(n % 2)], [pk(6)])
                if n > 0:
                    mm(bank(6)[:, hs], qdT[:, hs], S16[:, hs], False, True, ['qdT', 'S16'], [pk(6)])
            yield
            yield
            for h in range(4):
                hs = slice(h * 128, (h + 1) * 128)
                mm(bank(7)[:, hs], kdk[:, hs], VR[:, hs], True, True, ['kdk', 'VR' + str(n % 2)], [pk(7)])
            if n == 0:
                cp('dve', S32, bank(7), [pk(7)], ['S32'])
            else:
                tt('dve', S32, S32, CHt, ALU.mult, ['S32', 'CHt'], ['S32'])
                tt('dve', S32, S32, bank(7), ALU.add, ['S32', pk(7)], ['S32'])
            if n < NT - 1:
                cp('act', S16, S32, ['S32'], ['S16'])
            yield
            for h in range(4):
                op('dve', 'bn_stats', [pk(6)], ['bst'], out=bst[:, h * 6:(h + 1) * 6], in_=bank(6)[:, h * 128:(h + 1) * 128])
            for h in range(4):
                op('dve', 'bn_aggr', ['bst'], ['bmv'], out=bmv[:, h * 2:(h + 1) * 2], in_=bst[:, h * 6:(h + 1) * 6])
            yield
            bmvv = bmv.rearrange("p (h two) -> p h two", two=2)
            ts('dve', sd4, bmvv[:, :, 1], EPS, ALU.add, ['bmv'], ['sd4'])
            act(sd4, sd4, AF.Sqrt, ['sd4'], ['sd4'])
            op('dve', 'reciprocal', ['sd4'], ['rs4'], out=rs4, in_=sd4)
            for h in range(4):
                hs = slice(h * 128, (h + 1) * 128)
                ts('dve', rn[:, hs], bank(6)[:, hs], bmv[:, 2 * h:2 * h + 1], ALU.subtract, [pk(6), 'bmv', 'rs4'], ['rn'],
                   s2=rs4[:, h:h + 1], op1=ALU.mult)
            yield
            tt('dve', rn, rn, RNG, ALU.mult, ['rn', 'RNG'], ['rn'])
            tt('dve', retb, rn, SG, ALU.mult, ['rn', 'SG' + str(n % 2)], ['retb'])
            dma('sp', ret_d[n * 128:(n + 1) * 128, :], retb, ['retb'], [f'retd{n}'])
            yield

        def run_gen(g):
            for _ in g:
                pass

        def interleave2(ga, gb):
            da = db = False
            while not (da and db):
                if not da:
                    try:
                        next(ga)
                    except StopIteration:
                        da = True
                if not db:
                    try:
                        next(gb)
                    except StopIteration:
                        db = True

        run_gen(b1_proj(0))
        for n in range(NT):
            interleave2(b1_ret(n), b1_proj(n + 1) if n + 1 < NT else iter(()))
        S.barrier(exclude=('zf',))
        off[0] = persist_off

        if stage == 'B1':
            S.enabled = False
        scoreb = [alloc(L), alloc(L)]
        penb = [alloc(L, BF16), alloc(L, BF16), alloc(L, BF16)]
        junk_b = [alloc(L, BF16), alloc(L, BF16)]
        xtb = [alloc(1024), alloc(1024)]
        QSb = [alloc(768, BF16), alloc(768, BF16), alloc(768, BF16)]
        rb = [alloc(512), alloc(512)]
        pTb = [alloc(1024, BF16), alloc(1024, BF16)]
        BT8 = alloc(2048, BF16)
        IREP = alloc(512, BF16)
        nrmin_b = [alloc(1), alloc(1)]
        hw__b = [alloc(1), alloc(1)]
        C31 = alloc(8)
        NEGD = alloc(128)
        POW = alloc(NIT + 2)
        W2_b = [alloc(NIT + 2), alloc(NIT + 2)]
        W2h_b = [alloc(NIT + 2), alloc(NIT + 2)]
        mid_b = [alloc(NIT + 2), alloc(NIT + 2)]
        cnt_b = [alloc(NIT + 2), alloc(NIT + 2)]
        uu_b = [alloc(NIT + 2), alloc(NIT + 2)]
        rmax_b = [alloc(1), alloc(1)]
        rmin_b = [alloc(1), alloc(1)]
        rrng_b = [alloc(1), alloc(1)]
        thr_b = [alloc(1), alloc(1)]
        rec8 = alloc(8)
        cat = alloc(1024, BF16)
        catT = alloc(1024, BF16)
        tmp_off = off[0]
        tmp = alloc(1024)
        yv = alloc(1024)
        BTt = arena[:, tmp_off:tmp_off + 2048]
        x1 = alloc(1024)
        LN1G = alloc(1024)
        LN1B = alloc(1024)
        h2 = alloc(1024, BF16)
        h2T = alloc(1024, BF16)
        BR = alloc(NE)
        ONEb = alloc(128, BF16)
        EC = alloc(NE)
        lg = alloc(NE)
        mx8 = alloc(8)
        nm = alloc(1)
        ex4 = alloc(4)
        sm = alloc(1)
        rsm = alloc(1)
        selb = alloc(NE, BF16)
        pos = alloc(NE)
        flat = alloc(NE)
        ohj = alloc(NE)
        idxf = alloc(4)
        lst = alloc(12)
        lmv = alloc(2)
        lsd = alloc(1)
        lrs = alloc(1)

        memset('dve', ONEb, 1.0, [], ['ONEb'])
        dma('sp', BTt, bt_d[:, :], [], ['BTt', 'tmp', 'yv'])
        dma('sp', C31, c31_d[:, :], [], ['C31'])
        dma('sp', NEGD, negd_d[:, :], [], ['NEGD'])
        dma('sp', POW, pow_d[:, :], [], ['POW'])
        dma('sp', EC, ec_d[:, :], [], ['EC'])
        dma('sp', LN1G, ln1g_d.partition_broadcast(128), [], ['LN1G'])
        dma('sp', LN1B, ln1b_d.partition_broadcast(128), [], ['LN1B'])
        dma('sp', BR, br_d.partition_broadcast(128), [], ['BR'])
        BTv = BTt.rearrange("p (k h t) -> p k h t", k=2, h=8)
        for k in range(2):
            tt('dve', BTv[:, k, :, :], BTv[:, k, :, :], C31.unsqueeze(2).to_broadcast([128, 8, 128]), ALU.subtract,
               ['BTt', 'C31'], ['BTt'])

        ts('dve', BT8, BTt, 8.0, ALU.mult, ['BTt', 'tmp', 'yv'], ['BT8'])
        for c in range(4):
            cp('dve', IREP[:, c * 128:(c + 1) * 128], identb, ['identb'], ['IREP'])

        def layer_norm(eng_src, src, gtab, btab, dst, rkeys, wkey, gk, bk):
            op('dve', 'bn_stats', rkeys, ['lst'], out=lst[:, 0:6], in_=src[:, 0:512])
            op('dve', 'bn_stats', rkeys, ['lst'], out=lst[:, 6:12], in_=src[:, 512:1024])
            op('dve', 'bn_aggr', ['lst'], ['lmv'], out=lmv, in_=lst.rearrange("p (a s) -> p a s", s=6))
            ts('dve', lsd, lmv[:, 1:2], EPS, ALU.add, ['lmv'], ['lsd'])
            act(lsd, lsd, AF.Sqrt, ['lsd'], ['lsd'])
            op('dve', 'reciprocal', ['lsd'], ['lrs'], out=lrs, in_=lsd)
            ts('dve', src, src, lmv[:, 0:1], ALU.subtract, rkeys + ['lmv', 'lrs'], rkeys, s2=lrs[:, 0:1], op1=ALU.mult)
            tt('dve', src, src, gtab, ALU.mult, rkeys + [gk], rkeys)
            tt('dve', dst, src, btab, ALU.add, rkeys + [bk], [wkey])

        def stage_scores(n):
            Sn = (n + 1) * 128
            QSn = QSb[n % 3]
            qk = f'QSb{n % 3}'
            pen = penb[n % 3]
            pnk = f'pen{n % 3}'
            q2 = n % 2
            score = scoreb[q2]
            junk = junk_b[q2]
            W2, mid, cnt, uu = W2_b[q2], mid_b[q2], cnt_b[q2], uu_b[q2]
            rmax, rmin, rrng, thr, nrmin, hw_ = rmax_b[q2], rmin_b[q2], rrng_b[q2], thr_b[q2], nrmin_b[q2], hw__b[q2]
            dma('sp', QSn, qs_d[n, :, :], [f'qsd{n}'], [qk])
            QITc = QSn[:, 512:768]
            kit_keys = [f'KIT{j}' for j in range(n + 1)]
            nchunk = (Sn + 511) // 512
            it = 0
            for c in range(nchunk):
                wd = min(512, Sn - c * 512)
                for h in range(4):
                    b = it % 2
                    it += 1
                    half = slice((h % 2) * 64, (h % 2) * 64 + 64)
                    mm(bank(b)[:, 0:wd], QITc[half, (h // 2) * 128:(h // 2 + 1) * 128], KIT[half, c * 512:c * 512 + wd], True, True,
                       [qk] + kit_keys[c * 4:c * 4 + 4], [pk(b)])
                    act(rb[b][:, 0:wd], bank(b)[:, 0:wd], AF.Relu, [pk(b)], [f'rb{b}'])
                    if h == 0:
                        ts('dve', score[:, c * 512:c * 512 + wd], rb[b][:, 0:wd], WI[:, n * 4:n * 4 + 1], ALU.mult,
                           [f'rb{b}', f'WI{n}'], ['score' + str(q2)])
                    else:
                        stt('dve', score[:, c * 512:c * 512 + wd], rb[b][:, 0:wd], WI[:, n * 4 + h:n * 4 + h + 1],
                            score[:, c * 512:c * 512 + wd], ALU.mult, ALU.add, [f'rb{b}', f'WI{n}', 'score' + str(q2)], ['score' + str(q2)])
                yield
            if Sn <= 256:
                tt('dve', score[:, n * 128:(n + 1) * 128], score[:, n * 128:(n + 1) * 128], NEGD, ALU.add, ['score' + str(q2), 'NEGD'], ['score' + str(q2)])
                memset('dve', thr, -1e29, [], ['thr' + str(q2)])
                yield
            else:
                op('dve', 'tensor_reduce', ['score' + str(q2)], ['rmax' + str(q2)], out=rmax, in_=score[:, 0:Sn], axis=AX.X, op=ALU.max)
                op('dve', 'tensor_reduce', ['score' + str(q2)], ['rmin' + str(q2)], out=rmin, in_=score[:, 0:Sn], axis=AX.X, op=ALU.min)
                tt('dve', score[:, n * 128:(n + 1) * 128], score[:, n * 128:(n + 1) * 128], NEGD, ALU.add, ['score' + str(q2), 'NEGD'], ['score' + str(q2)])
                on_dve = (n % 2 == 1)
                if on_dve:
                    tt('dve', rrng, rmax, rmin, ALU.subtract, ['rmax' + str(q2), 'rmin' + str(q2)], ['rrng' + str(q2)])
                    ts('dve', nrmin, rmin, 1.0, ALU.mult, ['rmin' + str(q2)], ['nrmin' + str(q2)])
                else:
                    tt('dve', rrng, rmin, rmax, ALU.subtract, ['rmax' + str(q2), 'rmin' + str(q2)], ['rrng' + str(q2)])
                    ts('dve', nrmin, rmin, -1.0, ALU.mult, ['rmin' + str(q2)], ['nrmin' + str(q2)])
                ts('dve', W2, POW, rrng[:, 0:1], ALU.mult, ['POW', 'rrng' + str(q2)], ['W2' + str(q2)])
                stt('dve', mid[:, 0:1], W2[:, 1:2], 1.0, nrmin, ALU.mult, ALU.add, ['W2' + str(q2), 'nrmin' + str(q2)], ['mid' + str(q2)])
                W2h = W2h_b[q2]
                ts('dve', W2h, W2, 0.5, ALU.mult, ['W2' + str(q2)], ['W2' + str(q2)])
                memset('dve', cnt, 0.0, [], ['cnt' + str(q2)])
                yield
                for k in range(NIT):
                    if on_dve:
                        ts('dve', junk[:, 0:Sn], score[:, 0:Sn], mid[:, k:k + 1], ALU.is_ge, ['score' + str(q2), 'mid' + str(q2), 'cnt' + str(q2)],
                           ['junk' + str(q2), 'cnt' + str(q2)], s2=0.0, op1=ALU.add, accum=cnt[:, k:k + 1])
                        ts('dve', uu[:, k:k + 1], cnt[:, k:k + 1], 255.5, ALU.is_ge, ['cnt' + str(q2)], ['uu' + str(q2)], s2=0.5, op1=ALU.subtract)
                    else:
                        act(junk[:, 0:Sn], score[:, 0:Sn], AF.Sign, ['score' + str(q2), 'mid' + str(q2), 'cnt' + str(q2)], ['junk' + str(q2), 'cnt' + str(q2)], bias=mid[:, k:k + 1], scale=1.0,
                            accum=cnt[:, k:k + 1])
                        act(uu[:, k:k + 1], cnt[:, k:k + 1], AF.Sign, ['cnt' + str(q2)], ['uu' + str(q2)], bias=float(Sn - 510.5), scale=1.0)
                        act(mid[:, k + 1:k + 2], uu[:, k:k + 1], AF.Identity, ['uu' + str(q2), 'W2' + str(q2), 'mid' + str(q2)], ['mid' + str(q2)],
                            bias=mid[:, k:k + 1], scale=W2h[:, k + 1:k + 2])
                        yield
                        continue
                    stt('dve', mid[:, k + 1:k + 2], uu[:, k:k + 1], W2[:, k + 1:k + 2], mid[:, k:k + 1], ALU.mult, ALU.add,
                        ['uu' + str(q2), 'W2' + str(q2), 'mid' + str(q2)], ['mid' + str(q2)])
                    yield
                ts('dve', hw_, W2[:, NIT:NIT + 1], 0.5, ALU.mult, ['W2' + str(q2)], ['hw_' + str(q2)])
                if on_dve:
                    tt('dve', thr, mid[:, NIT:NIT + 1], hw_, ALU.subtract, ['hw_' + str(q2), 'mid' + str(q2)], ['thr' + str(q2)])
                else:
                    stt('dve', thr, mid[:, NIT:NIT + 1], -1.0, hw_, ALU.mult, ALU.add, ['hw_' + str(q2), 'mid' + str(q2)], ['thr' + str(q2)])
            ts('dve', pen[:, 0:Sn], score[:, 0:Sn], thr[:, 0:1], ALU.is_lt, ['score' + str(q2), 'thr' + str(q2)], [pnk], s2=-240000.0, op1=ALU.mult)
            yield

        def stage_attn(n):
            xt = xtb[n % 2]
            xk = f'xt{n % 2}'
            QSn = QSb[n % 3]
            qk = f'QSb{n % 3}'
            pen = penb[n % 3]
            pnk = f'pen{n % 3}'
            QTc = QSn[:, 0:512]
            for j in range(n + 1):
                jb = j % 2
                Lp = PS[1 + jb][:, :]
                lk = [pk(2 + 2 * jb), pk(3 + 2 * jb)]
                near = j >= n - 1
                kind = 0 if j == n else 1
                for g in range(2):
                    hp = slice(g * 64, g * 64 + 64)
                    og = Lp[:, g * 512:(g + 1) * 512]
                    mm(og, KT[hp, j * 128:(j + 1) * 128], QTc[hp, :], True, False, [qk, f'KT{j}'], lk)
                    mm(og, pen[:, j * 128:(j + 1) * 128], IREP, False, not near, [pnk, 'IREP'], lk)
                    if near:
                        mm(og, identb, BT8[:, kind * 1024 + g * 512:kind * 1024 + (g + 1) * 512], False, True, ['identb', 'BT8'], lk)
                pT = pTb[jb]
                pk_ = f'pT{jb}'
                act(pT, Lp, AF.Exp, lk, [pk_], scale=0.125)
                for h in range(8):
                    g = h // 4
                    ob = 6 + g
                    mm(bank(ob)[:, (h % 4) * 65:(h % 4) * 65 + 65], pT[:, h * 128:(h + 1) * 128], V1v[:, j, g, :],
                       j == 0 and h % 4 == 0, j == n, [pk_, f'V1_{j}', 'V1'], [pk(ob)], skip=True)
                yield
            for g in range(2):
                ov = bank(6 + g)[:, 0:260].rearrange("p (h d) -> p h d", d=65)
                op('dve', 'reciprocal', [pk(6 + g)], ['rec8'], out=rec8[:, g * 4:(g + 1) * 4], in_=ov[:, :, 64])
                tt('dve', cat[:, g * 256:(g + 1) * 256].rearrange("p (h d) -> p h d", d=64), ov[:, :, 0:64],
                   rec8[:, g * 4:(g + 1) * 4].unsqueeze(2).to_broadcast([128, 4, 64]), ALU.mult, [pk(6 + g), 'rec8'], ['cat_a'])
            yield

        def stage_tail(n):
            xt = xtb[n % 2]
            xk = f'xt{n % 2}'
            dma('sp', xt, x_d[n * 128:(n + 1) * 128, :], [], [xk])
            dma('sp', cat[:, 512:1024], ret_d[n * 128:(n + 1) * 128, :], [f'retd{n}'], ['cat_r'])
            for c in range(8):
                tp(bankb(0)[:, c * 128:(c + 1) * 128], cat[:, c * 128:(c + 1) * 128], identb, ['cat_a', 'cat_r', 'identb'], [pk(0)])
            cp('dve', catT, bankb(0), [pk(0)], ['catT'])
            yield
            Mp = PS[0][:, :]
            mk = [pk(0), pk(1)]
            for half in range(2):
                for kc in range(8):
                    mm(Mp[:, half * 512:(half + 1) * 512], catT[:, kc * 128:(kc + 1) * 128], woutv[:, kc, half * 512:(half + 1) * 512],
                       kc == 0, kc == 7, ['catT', f'wout{kc}'], mk)
            tt('dve', tmp, Mp, GA1, ALU.mult, mk + ['modB'], ['tmp'])
            stt('dve', yv, xt, ALPHA, tmp, ALU.mult, ALU.add, [xk, 'tmp'], ['yv'])
            yield
            layer_norm('dve', yv, LN1G, LN1B, x1, ['yv'], 'x1', 'LN1G', 'LN1B')
            dma('sp', x1_d[n * 128:(n + 1) * 128, :], x1, ['x1'], [f'x1d{n}'])
            dbg_out("d_x1", x1, ['x1'], rows=(n * 128, (n + 1) * 128))
            yield
            tt('dve', tmp, x1, SCF1, ALU.mult, ['x1', 'modB'], ['tmp'])
            tt('dve', h2, tmp, SHF, ALU.add, ['tmp', 'modB'], ['h2'])
            for c in range(8):
                tp(bankb(1)[:, c * 128:(c + 1) * 128], h2[:, c * 128:(c + 1) * 128], identb, ['h2', 'identb'], [pk(1)])
            cp('dve', h2T, bankb(1), [pk(1)], ['h2T'])
            yield
            for kc in range(8):
                mm(bank(0)[:, 0:NE], h2T[:, kc * 128:(kc + 1) * 128], wrv[:, kc, :], kc == 0, kc == 7, ['h2T', 'wrb'], [pk(0)])
            tt('dve', lg, bank(0)[:, 0:NE], BR, ALU.add, [pk(0), 'BR'], ['lg'])
            op('dve', 'max', ['lg'], ['mx8'], out=mx8, in_=lg)
            ts('dve', nm, mx8[:, 0:1], -1.0, ALU.mult, ['mx8'], ['nm'])
            memset('dve', sm, 0.0, [], ['sm'])
            act(ex4, mx8[:, 0:4], AF.Exp, ['mx8', 'nm', 'sm'], ['ex4', 'sm'], bias=nm[:, 0:1], scale=1.0, accum=sm)
            op('dve', 'reciprocal', ['sm'], ['rsm'], out=rsm, in_=sm)
            ts('dve', GATES[:, n * 4:(n + 1) * 4], ex4, rsm[:, 0:1], ALU.mult, ['ex4', 'rsm'], [f'GATES{n}'])
            yield
            ts('dve', selb, lg, mx8[:, 3:4], ALU.is_ge, ['lg', 'mx8'], ['selb'])
            mm(bank(0)[:, 64:64 + NE], UT, selb, True, True, ['UT', 'selb'], [pk(0)])
            mm(bank(0)[:, 128:128 + NE], ONEb, selb, True, True, ['ONEb', 'selb'], [pk(0)])
            tt('dve', pos, bank(0)[:, 64:64 + NE], carry, ALU.add, [pk(0), 'carry'], ['pos'])
            tt('dve', carry, carry, bank(0)[:, 128:128 + NE], ALU.add, [pk(0), 'carry'], ['carry'])
            yield
            stt('dve', flat, pos, float(CAP - 1), EC, ALU.min, ALU.add, ['pos', 'EC'], ['flat'])
            memset('dve', idxf, 0.0, [], ['idxf'])
            for k in range(4):
                stt('dve', ohj, lg, mx8[:, k:k + 1], flat, ALU.is_equal, ALU.mult, ['lg', 'mx8', 'flat', 'idxf'], ['ohj', 'idxf'],
                    accum=idxf[:, k:k + 1])
            cp('dve', IDXT[:, n * 4:(n + 1) * 4], idxf, ['idxf'], [f'IDX{n}'])
            for k in range(4):
                op('pool', 'indirect_dma_start', ['h2', f'IDX{n}'] + ZKEYS, [f'xgd{n}_{k}'], dma=True,
                   out=xg_d[:, :], out_offset=bass.IndirectOffsetOnAxis(ap=IDXT[:, n * 4 + k:n * 4 + k + 1], axis=0),
                   in_=h2, in_offset=None)
            yield

        def run_all(g):
            for _ in g:
                pass

        def nsteps_scores(n):
            return ((n + 1) * 128 + 511) // 512 + NIT + 3

        def interleave(items):
            done = [0] * len(items)
            while True:
                best = None
                for i, (g, q) in enumerate(items):
                    if done[i] >= q:
                        continue
                    frac = done[i] / q
                    if best is None or frac < best[0]:
                        best = (frac, i)
                if best is None:
                    break
                i = best[1]
                try:
                    next(items[i][0])
                except StopIteration:
                    pass
                done[i] += 1

        gens = {0: stage_scores(0)}
        prog_ = {0: 0}
        run_all(gens[0])
        prog_[0] = nsteps_scores(0) + 1
        if NT > 1:
            gens[1] = stage_scores(1)
            prog_[1] = 0
        for n in range(NT + 1):
            items = []
            if n < NT:
                items.append([stage_attn(n), n + 3])
            if n >= 1:
                items.append([stage_tail(n - 1), 9])
            if n + 1 < NT:
                tot = nsteps_scores(n + 1) + 1
                items.append([gens[n + 1], tot - prog_[n + 1]])
                prog_[n + 1] = tot
            if n + 2 < NT:
                gens[n + 2] = stage_scores(n + 2)
                half = (nsteps_scores(n + 2) + 1) // 2
                items.append([gens[n + 2], half])
                prog_[n + 2] = half
            interleave(items)
        if "d_gates" in dbg_d:
            dma('sp', dbg_d["d_gates"][:, :], GATES, [f'GATES{n}' for n in range(NT)], [])
        if "d_idx" in dbg_d:
            cp('dve', tmp[:, 0:128], IDXT, [f'IDX{n}' for n in range(NT)], ['tmp'])
            dma('sp', dbg_d["d_idx"][:, :], tmp[:, 0:128], ['tmp'], [])
        S.barrier()
        off[0] = persist_small

        if stage == 'B2':
            S.enabled = False
        Wgu = [alloc(8 * 2048, BF16), alloc(8 * 2048, BF16)]
        Wdn = [alloc(8 * 1024, BF16)]
        XRb = [alloc(1024, BF16) for _ in range(NSLOT)]
        XT = alloc(8 * CAP, BF16)
        XTv = XT.rearrange("p (kc s) -> p kc s", s=CAP)
        Aact = alloc(8 * CAP, BF16)
        Av = Aact.rearrange("p (m s) -> p m s", s=CAP)
        BD = alloc(1024)
        BGU = alloc(NE * 16)
        gq = alloc(512)
        sgm = alloc(512)
        uq = alloc(512)
        tq = alloc(512)
        yo = [alloc(1024), alloc(1024)]
        dma('sp', BGU, bgu_d[:, :], [], ['BGU'])
        BGA = alloc(NE * 16)
        F7 = float(7.0 / (1.0 + np.exp(-1.702 * 7.0)))
        BGUv = BGU.rearrange("p (e c) -> p e c", c=16)
        BGAv = BGA.rearrange("p (e c) -> p e c", c=16)
        ts('dve', BGAv[:, :, 0:8], BGUv[:, :, 0:8], 1.702, ALU.mult, ['BGU'], ['BGA'])
        ts('dve', BGAv[:, :, 8:16], BGUv[:, :, 8:16], 1.0, ALU.add, ['BGU'], ['BGA'])

        STG = [alloc(2048) for _ in range(3)]
        chunks = []
        for kc_ in range(8):
            chunks.append(('g', 0, kc_))
        for q_ in range(4):
            chunks.append(('d', 0, q_))
        for ex_ in range(NE):
            for m_ in range(8):
                if ex_ >= 1 and m_ < 4:
                    chunks.append(('d', ex_, m_))
                if ex_ + 1 < NE:
                    chunks.append(('g', ex_ + 1, m_))
        issued = [0]

        def issue_next():
            i = issued[0]
            if i >= len(chunks):
                return
            issued[0] += 1
            kind, ex_, c_ = chunks[i]
            st = STG[i % 3]
            if kind == 'g':
                dma('act', st, wgu_d[ex_, c_ * 128:(c_ + 1) * 128, :], [], [f'stg{i % 3}'])
            else:
                dma('act', st.rearrange("p (m n) -> p m n", n=1024),
                    wd_d[ex_, 2 * c_ * 128:(2 * c_ + 2) * 128, :].rearrange("(m p) n -> p m n", p=128), [], [f'stg{i % 3}'])

        casted = [0]

        def cast_next():
            i = casted[0]
            casted[0] += 1
            kind, ex_, c_ = chunks[i]
            st = STG[i % 3]
            if kind == 'g':
                dst = Wgu[ex_ % 2].rearrange("p (kc n) -> p kc n", n=2048)[:, c_, :]
                cp('act', dst, st, [f'stg{i % 3}'], [f'Wgu{ex_ % 2}_{c_}'])
            else:
                dst = Wdn[0][:, 2 * c_ * 1024:(2 * c_ + 2) * 1024]
                cp('act', dst, st, [f'stg{i % 3}'], [f'Wdn_{2 * c_}', f'Wdn_{2 * c_ + 1}'])
            issue_next()

        for _ in range(3):
            issue_next()
        for _ in range(12):
            cast_next()
        NH = CAP // 512

        def load_xr(ex):
            for a in range(NSLOT):
                dma('sp', XRb[a], xg_d[ex * CAP + a * 128:ex * CAP + (a + 1) * 128, :], [], [f'XR{a}'])

        def prep_xt(ex):
            for a in range(NSLOT):
                b = a % 2
                XR = XRb[a]
                for kc in range(8):
                    tp(bankb(b)[:, kc * 128:(kc + 1) * 128], XR[:, kc * 128:(kc + 1) * 128], identb,
                       [f'XR{a}', 'identb'], [pk(b)])
                cp('dve', XTv[:, :, a * 128:(a + 1) * 128], bankb(b).rearrange("p (kc s) -> p kc s", s=128),
                   [pk(b)], ['XT'])

        load_xr(0)
        prep_xt(0)
        for ex in range(NE):
            eb = ex % 2
            Wg = Wgu[eb]
            Wgv = Wg.rearrange("p (kc n) -> p kc n", n=2048)
            Wd = Wdn[0]
            Wdv = Wd.rearrange("p (kc n) -> p kc n", n=1024)
            dma('sp', BD, bd_d[ex:ex + 1, :].partition_broadcast(128), [], ['BD'])
            for m in range(8):
                if ex >= 1 and m < 4:
                    cast_next()
                if ex + 1 < NE:
                    cast_next()
                bg = BGA[:, ex * 16 + m:ex * 16 + m + 1]
                bu = BGA[:, ex * 16 + 8 + m:ex * 16 + 8 + m + 1]
                for hh in range(NH):
                    cs_ = slice(hh * 512, (hh + 1) * 512)
                    gb = 2 + hh % 2
                    ub = 4 + hh % 2
                    for (bb_, c0) in ((gb, m * 128), (ub, 1024 + m * 128)):
                        for kc in range(8):
                            mm(bank(bb_), Wgv[:, kc, c0:c0 + 128], XTv[:, kc, cs_], kc == 0, kc == 7,
                               [f'Wgu{eb}_{kc}', 'XT'], [pk(bb_)])
                    act(sgm, bank(gb), AF.Silu, [pk(gb), 'BGA'], ['sgm'], bias=bg, scale=1.702)
                    ts('dve', uq, bank(ub), bu, ALU.add, [pk(ub), 'BGA'], ['uq'], s2=8.0, op1=ALU.min)
                    ts('dve', tq, sgm, 1.0 / 1.702, ALU.mult, ['sgm'], ['tq'], s2=F7, op1=ALU.min)
                    stt('dve', Av[:, m, cs_], uq, -6.0, tq, ALU.max, ALU.mult, ['uq', 'tq'], ['Aact'])
                if m == 0 and ex + 1 < NE:
                    load_xr(ex + 1)
            if ex + 1 < NE:
                prep_xt(ex + 1)
            for a in range(NSLOT):
                Yp = PS[3 if a % 2 == 0 else 0][:, :]
                yk = [pk(6), pk(7)] if a % 2 == 0 else [pk(0), pk(1)]
                for half in range(2):
                    for m in range(8):
                        mm(Yp[:, half * 512:(half + 1) * 512], Av[:, m, a * 128:(a + 1) * 128], Wdv[:, m, half * 512:(half + 1) * 512],
                           m == 0, m == 7, ['Aact', f'Wdn_{m}'], yk)
                tt('dve', yo[a % 2], Yp, BD, ALU.add, yk + ['BD'], [f'yo{a % 2}'])
                dma('sp', y_d[ex * CAP + a * 128:ex * CAP + (a + 1) * 128, :], yo[a % 2], [f'yo{a % 2}'], [f'yd{ex}_{a}'])
        S.barrier()
        off[0] = persist_small

        if stage == 'C':
            S.enabled = False
        YG2 = [[alloc(1024) for _ in range(4)] for _ in range(2)]
        x1b = [alloc(1024), alloc(1024)]
        accb = alloc(1024)
        tmp = alloc(1024)
        ob = [alloc(1024), alloc(1024)]
        LN2G = alloc(1024)
        LN2B = alloc(1024)
        lst = alloc(12)
        lmv = alloc(2)
        lsd = alloc(1)
        lrs = alloc(1)
        dma('sp', LN2G, ln2g_d.partition_broadcast(128), [], ['LN2G'])
        dma('sp', LN2B, ln2b_d.partition_broadcast(128), [], ['LN2B'])
        for n in range(NT):
            x1t = x1b[n % 2]
            YG = YG2[n % 2]
            dma('sp', x1t, x1_d[n * 128:(n + 1) * 128, :], [f'x1d{n}'], [f'x1b{n % 2}'])
            for k in range(4):
                op('pool', 'indirect_dma_start', [f'IDX{n}'], [f'YG{n % 2}_{k}'], dma=True,
                   out=YG[k], out_offset=None, in_=y_d[:, :],
                   in_offset=bass.IndirectOffsetOnAxis(ap=IDXT[:, n * 4 + k:n * 4 + k + 1], axis=0))
            ts('dve', accb, YG[0], GATES[:, n * 4:n * 4 + 1], ALU.mult, [f'YG{n % 2}_0', f'GATES{n}'], ['accb'])
            for k in range(1, 4):
                stt('dve', accb, YG[k], GATES[:, n * 4 + k:n * 4 + k + 1], accb, ALU.mult, ALU.add, [f'YG{n % 2}_{k}', f'GATES{n}', 'accb'], ['accb'])
            if "d_ff" in dbg_d:
                dma('sp', dbg_d["d_ff"][n * 128:(n + 1) * 128, :], accb, ['accb'], [])
            tt('dve', tmp, accb, GF1, ALU.mult, ['accb', 'modB'], ['tmp'])
            stt('dve', tmp, x1t, ALPHA, tmp, ALU.mult, ALU.add, [f'x1b{n % 2}', 'tmp'], ['tmp'])
            layer_norm('dve', tmp, LN2G, LN2B, ob[n % 2], ['tmp'], f'ob{n % 2}', 'LN2G', 'LN2B')
            dma('sp', out_d[n * 128:(n + 1) * 128, :], ob[n % 2], [f'ob{n % 2}'], [f'outd{n}'])

        sems = {}
        for e in ENGS:
            sems[('e', e)] = es.enter_context(nc.semaphore(f"se_{e}"))
        for k in S.dma_cnt:
            sems[k] = es.enter_context(nc.semaphore(f"sd_{k[1]}_{k[2]}"))
        final = S.all_tokens()
        blk = es.enter_context(nc.Block())
        bmap = {'pe': blk.tensor, 'act': blk.scalar, 'dve': blk.vector, 'pool': blk.gpsimd, 'sp': blk.sync}

        def make(eng):
            def body(e):
                for waits, fn, tok, inc in S.prog[eng]:
                    for k, v in waits:
                        e.wait_ge(sems[k], v)
                    inst = fn(e)
                    inst.then_inc(sems[tok[0]], inc)
                if eng == 'sp':
                    for k, v in final.items():
                        e.wait_ge(sems[k], v)
            return body
        for eng in ENGS:
            bmap[eng](make(eng))
    return nc


def host_consts():
    f32 = np.float32
    s = np.arange(128)[:, None]
    t = np.arange(128)[None, :]
    gam = (1.0 - 2.0 ** (-5.0 - np.arange(4))).astype(np.float64)
    dtab = np.zeros((128, 4, 128), f32)
    qd = np.zeros((128, 4, 128), f32)
    chtab = np.zeros((128, 4, 128), f32)
    kd = np.zeros((128, 4), f32)
    for h in range(4):
        diff = (t - s)
        dtab[:, h, :] = np.where(diff >= 0, gam[h] ** np.maximum(diff, 0), 0.0) / np.sqrt(128.0)
        qd[:, h, :] = (gam[h] ** (np.arange(128) + 1.0))[None, :]
        chtab[:, h, :] = gam[h] ** 128.0
        kd[:, h] = gam[h] ** (127.0 - np.arange(128)) / np.sqrt(128.0)
    inv = 1.0 / (10000.0 ** (np.arange(0, 128, 2, dtype=np.float32) / 128.0))
    ang = np.arange(L, dtype=np.float32)[:, None] * inv[None, :].astype(np.float32)
    cos = np.cos(ang).astype(f32)
    sin = np.sin(ang).astype(f32)
    negd = np.where(t > s, -1e30, 0.0).astype(f32)
    negd = np.where(np.arange(128)[None, :] > np.arange(128)[:, None], -1e30, 0.0).astype(f32)
    ut = (np.arange(128)[:, None] < np.arange(128)[None, :]).astype(f32)
    pow2 = np.tile((2.0 ** -np.arange(NIT + 2)).astype(f32)[None, :], (128, 1))
    ec = np.tile((np.arange(NE) * CAP).astype(f32)[None, :], (128, 1))
    ident = np.eye(128, dtype=f32)
    dist0 = t - s
    dist1 = t - s + 128
    b0 = t5_bucket_np(dist0)
    b1 = t5_bucket_np(dist1)
    return dict(dtab=dtab.reshape(128, 512), qdtab=qd.reshape(128, 512), chtab=chtab.reshape(128, 512), kdtab=kd,
                cos=cos, sin=sin, negd=negd, ut=ut, pow2=pow2, ec=ec, ident=ident), b0, b1, (dist0 >= 0)


_PERM = None


def _perm():
    idx = []
    for hh in [0, 4, 1, 5, 2, 6, 3, 7]:
        idx += list(range(hh * 64, hh * 64 + 64))
    idx += list(range(512, 640))
    idx += list(range(768, 1024))
    idx += list(range(1024, 1088)) * 2
    idx += list(range(640, 768))
    idx += list(range(1088, 1092))
    idx += list(range(1092, 2116))
    idx += list(range(2116, 2628))
    idx += list(range(2628, 3140))
    assert len(idx) == NCOL
    return np.array(idx)


def make_in_maps(inputs, cores):
    f32 = np.float32
    consts, b0, b1, causal = host_consts()
    rel_bias = np.asarray(inputs["rel_bias"], f32)
    bt = np.zeros((128, 2, 8, 128), f32)
    g0 = rel_bias[b0]
    g1 = rel_bias[b1]
    bt[:, 0] = np.where(causal[:, None, :], np.transpose(g0, (0, 2, 1)), 0.0)
    bt[:, 1] = np.transpose(g1, (0, 2, 1))
    c31 = np.tile(rel_bias[31][None, :], (128, 1)).astype(f32)
    w_in = np.ascontiguousarray(np.asarray(inputs["w_in"][0], f32)[:, _perm()])
    bgu = np.ascontiguousarray(np.asarray(inputs["b_gate_up"][0], f32).reshape(NE, 16, 128).transpose(2, 0, 1)).reshape(128, NE * 16)
    shared = dict(
        w_ada=np.ascontiguousarray(inputs["w_ada"][0], dtype=f32), b_ada=np.ascontiguousarray(inputs["b_ada"], dtype=f32).reshape(1, -1),
        w_in=w_in, w_out=np.ascontiguousarray(inputs["w_out"][0], dtype=f32),
        bt=bt.reshape(128, 2048), c31=c31,
        rng=np.asarray(inputs["ret_norm_g"], f32).reshape(1, 512),
        ln1g=np.asarray(inputs["ln1_g"], f32).reshape(1, D), ln1b=np.asarray(inputs["ln1_b"], f32).reshape(1, D),
        ln2g=np.asarray(inputs["ln2_g"], f32).reshape(1, D), ln2b=np.asarray(inputs["ln2_b"], f32).reshape(1, D),
        wr=np.ascontiguousarray(inputs["w_router"][0], dtype=f32), br=np.asarray(inputs["b_router"], f32).reshape(1, NE),
        wgu=np.ascontiguousarray(inputs["w_gate_up"][0], dtype=f32), bgu=bgu,
        wd=np.ascontiguousarray(inputs["w_down"][0], dtype=f32), bd=np.ascontiguousarray(inputs["b_down"][0], dtype=f32),
    )
    shared.update(consts)
    maps = []
    for b in cores:
        m = dict(shared)
        m["x"] = np.ascontiguousarray(inputs["x"][b], dtype=f32)
        m["c8"] = np.ascontiguousarray(np.asarray(inputs["c"][b], f32).reshape(8, 128).T)
        maps.append(m)
    return maps


def kernel(**inputs):
    nc = build_nc()
    maps = make_in_maps(inputs, list(range(8)))
    res = run_bass_kernel_spmd(nc, maps, core_ids=list(range(8)))
    out = np.stack([np.asarray(r["out"], np.float32) for r in res.results], axis=0)
    return out
```

```python
import os
import contextlib
import numpy as np
import ml_dtypes
import concourse.bass as bass
import concourse.mybir as mybir
from concourse.bass_utils import run_bass_kernel_spmd

F32 = mybir.dt.float32
BF16 = mybir.dt.bfloat16
I32 = mybir.dt.int32
AF = mybir.ActivationFunctionType
ALU = mybir.AluOpType
AX = mybir.AxisListType

L = 4096
D = 1024
NT = 32
NCOL = 3204
NE = 32
CAP = 896
NSLOT = CAP // 128
NIT = 18
ALPHA = float(2.0 ** 0.25)
EPS = 1e-5
ENGS = ['pe', 'act', 'dve', 'pool', 'sp']
NDMA = 8


class Sched:
    def __init__(self):
        self.prog = {e: [] for e in ENGS}
        self.seq = {e: 0 for e in ENGS}
        self.res = {}
        self.waited = {e: {} for e in ENGS}
        self.dma_rr = {e: 0 for e in ENGS}
        self.dma_cnt = {}
        self.pending = {e: {} for e in ENGS}
        self.enabled = True

    def add(self, eng, fn, r=(), w=(), dma=False, grp=None):
        if not self.enabled:
            return None
        needs = dict(self.pending[eng])
        self.pending[eng] = {}

        def need(tok):
            if tok is None:
                return
            k, v = tok
            if needs.get(k, 0) < v:
                needs[k] = v
        for key in r:
            st = self.res.get(key)
            if st:
                need(st[0])
        for key in w:
            st = self.res.get(key)
            if st:
                need(st[0])
                for k, v in st[1].items():
                    need((k, v))
        if dma:
            gname = grp or eng
            j = self.dma_rr.get(gname, 0)
            self.dma_rr[gname] = (j + 1) % NDMA
            semk = ('d', gname, j)
            prev = self.dma_cnt.get(semk, 0)
            if prev:
                need((semk, 16 * prev))
            self.dma_cnt[semk] = prev + 1
            tok = (semk, 16 * (prev + 1))
            inc = 16
        else:
            self.seq[eng] += 1
            tok = (('e', eng), self.seq[eng])
            inc = 1
        waits = []
        for k, v in needs.items():
            if k == ('e', 'pe') and eng == 'pe' and not dma:
                continue
            if self.waited[eng].get(k, 0) >= v:
                continue
            self.waited[eng][k] = v
            waits.append((k, v))
        self.prog[eng].append((waits, fn, tok, inc))
        for key in w:
            self.res[key] = [tok, {}]
        for key in r:
            if key in w:
                continue
            st = self.res.setdefault(key, [None, {}])
            if st[1].get(tok[0], 0) < tok[1]:
                st[1][tok[0]] = tok[1]
        return tok

    def cut(self, name):
        if os.environ.get('KCUT', '') == name:
            self.enabled = False

    def all_tokens(self):
        toks = {}
        for e in ENGS:
            if self.seq[e]:
                toks[('e', e)] = self.seq[e]
        for k, c in self.dma_cnt.items():
            toks[k] = 16 * c
        return toks

    def barrier(self, exclude=()):
        toks = self.all_tokens()
        for e in ENGS:
            for k, v in toks.items():
                if k[0] == 'd' and k[1] in exclude:
                    continue
                if self.pending[e].get(k, 0) < v:
                    self.pending[e][k] = v


def t5_bucket_np(n):
    n = np.maximum(n, 0)
    nf = np.maximum(n, 1).astype(np.float32)
    large = 16 + (np.log(nf / np.float32(16)) / np.float32(np.log(128 / 16)) * np.float32(16)).astype(np.int32)
    large = np.minimum(large, 31)
    return np.where(n < 16, n, large)


def build_nc(dbg=(), stage='D'):
    nc = bass.Bass("TRN2", target_bir_lowering=False)
    S = Sched()

    def din(name, shape, dt=F32):
        return nc.dram_tensor(name, list(shape), dt, kind="ExternalInput").ap()

    x_d = din("x", [L, D])
    c8_d = din("c8", [128, 8])
    wada_d = din("w_ada", [D, 6 * D])
    bada_d = din("b_ada", [1, 6 * D])
    win_d = din("w_in", [D, NCOL])
    wout_d = din("w_out", [D, D])
    bt_d = din("bt", [128, 2048])
    c31_d = din("c31", [128, 8])
    cos_d = din("cos", [L, 64])
    sin_d = din("sin", [L, 64])
    dt_d = din("dtab", [128, 512])
    qd_d = din("qdtab", [128, 512])
    ch_d = din("chtab", [128, 512])
    kd_d = din("kdtab", [128, 4])
    rng_d = din("rng", [1, 512])
    ln1g_d = din("ln1g", [1, D])
    ln1b_d = din("ln1b", [1, D])
    ln2g_d = din("ln2g", [1, D])
    ln2b_d = din("ln2b", [1, D])
    wr_d = din("wr", [D, NE])
    br_d = din("br", [1, NE])
    wgu_d = din("wgu", [NE, D, 2 * D])
    bgu_d = din("bgu", [128, NE * 16])
    wd_d = din("wd", [NE, D, D])
    bd_d = din("bd", [NE, D])
    negd_d = din("negd", [128, 128])
    ut_d = din("ut", [128, 128])
    pow_d = din("pow2", [128, NIT + 2])
    ec_d = din("ec", [128, NE])
    id_d = din("ident", [128, 128])
    out_d = nc.dram_tensor("out", [L, D], F32, kind="ExternalOutput").ap()
    dbg_d = {}
    for name, shape in dbg:
        dbg_d[name] = nc.dram_tensor(name, list(shape), F32, kind="ExternalOutput").ap()

    qs_d = nc.dram_tensor("qs_scr", [NT, 128, 768], BF16, kind="Internal").ap()
    ret_d = nc.dram_tensor("ret_scr", [L, 512], BF16, kind="Internal").ap()
    x1_d = nc.dram_tensor("x1_scr", [L, D], F32, kind="Internal").ap()
    xg_d = nc.dram_tensor("xg_scr", [NE * CAP, D], BF16, kind="Internal").ap()
    y_d = nc.dram_tensor("y_scr", [NE * CAP, D], F32, kind="Internal").ap()

    with contextlib.ExitStack() as es:
        ARENA_W = 53000
        arena = es.enter_context(nc.sbuf_tensor("arena", [128, ARENA_W], F32))
        PS = [es.enter_context(nc.psum_tensor(f"ps{i}", [128, 1024], F32)) for i in range(4)]
        off = [0]

        def alloc(n, dt=F32):
            n32 = n if dt in (F32, I32) else (n + 1) // 2
            assert off[0] + n32 <= ARENA_W, (off[0], n32)
            a = arena[:, off[0]:off[0] + n32]
            off[0] += n32
            if dt == F32:
                return a
            return a.bitcast(dt)

        def bank(b):
            return PS[b // 2][:, (b % 2) * 512:(b % 2) * 512 + 512]

        def bankb(b):
            return bank(b).bitcast(BF16)

        def pk(b):
            return f"ps{b}"

        def mm(out, lhsT, rhs, start, stop, r, w, skip=False):
            if skip:
                S.add('pe', lambda e: e.matmul(out, lhsT=lhsT, rhs=rhs, start=start, stop=stop, skip_group_check=True), r, w)
            else:
                S.add('pe', lambda e: e.matmul(out, lhsT=lhsT, rhs=rhs, start=start, stop=stop), r, w)

        def tp(out, in_, idn, r, w):
            S.add('pe', lambda e: e.transpose(out=out, in_=in_, identity=idn), r, w)

        def dma(eng, out, in_, r, w):
            return S.add(eng, lambda e: e.dma_start(out=out, in_=in_), r, w, dma=True)

        def act(out, in_, func, r, w, bias=0.0, scale=1.0, accum=None):
            if accum is None:
                S.add('act', lambda e: e.activation(out=out, in_=in_, func=func, bias=bias, scale=scale), r, w)
            else:
                S.add('act', lambda e: e.activation(out=out, in_=in_, func=func, bias=bias, scale=scale, accum_out=accum), r, w)

        def cp(eng, out, in_, r, w):
            if eng == 'act':
                S.add('act', lambda e: e.copy(out=out, in_=in_), r, w)
            else:
                S.add(eng, lambda e: e.tensor_copy(out=out, in_=in_), r, w)

        def tt(eng, out, in0, in1, op, r, w):
            S.add(eng, lambda e: e.tensor_tensor(out=out, in0=in0, in1=in1, op=op), r, w)

        def ts(eng, out, in0, s1, op0, r, w, s2=None, op1=None, accum=None):
            if op1 is None:
                S.add(eng, lambda e: e.tensor_scalar(out=out, in0=in0, scalar1=s1, scalar2=None, op0=op0), r, w)
            elif accum is None:
                S.add(eng, lambda e: e.tensor_scalar(out=out, in0=in0, scalar1=s1, scalar2=s2, op0=op0, op1=op1), r, w)
            else:
                S.add(eng, lambda e: e.tensor_scalar(out=out, in0=in0, scalar1=s1, scalar2=s2, op0=op0, op1=op1, accum_out=accum), r, w)

        def stt(eng, out, in0, scalar, in1, op0, op1, r, w, accum=None):
            if accum is None:
                S.add(eng, lambda e: e.scalar_tensor_tensor(out=out, in0=in0, scalar=scalar, in1=in1, op0=op0, op1=op1), r, w)
            else:
                S.add(eng, lambda e: e.scalar_tensor_tensor(out=out, in0=in0, scalar=scalar, in1=in1, op0=op0, op1=op1, accum_out=accum), r, w)

        def op(eng, name, r, w, dma=False, **kw):
            return S.add(eng, lambda e: getattr(e, name)(**kw), r, w, dma=dma)

        def memset(eng, out, val, r, w):
            S.add(eng, lambda e: e.memset(out, val), r, w)

        def dbg_out(name, src, r, rows=None):
            if name in dbg_d:
                dst = dbg_d[name] if rows is None else dbg_d[name][rows[0]:rows[1], :]
                dma('sp', dst, src, r, [])

        modB = alloc(4096)
        GA1 = modB[:, 0:1024]
        SHF = modB[:, 1024:2048]
        SCF1 = modB[:, 2048:3072]
        GF1 = modB[:, 3072:4096]
        ident = alloc(128)
        identb = alloc(128, BF16)
        SHA = alloc(8)
        SCA1 = alloc(8)
        WI = alloc(NT * 4)
        GATES = alloc(NT * 4)
        IDXT = alloc(NT * 4, I32)
        carry = alloc(NE)
        ZT = alloc(4 * 1024, BF16)
        persist_small = off[0]
        KT = alloc(L, BF16)
        KIT = alloc(L, BF16)
        V1 = alloc(NT * 2 * 65 + 1, BF16)[:, 0:NT * 2 * 65]
        V1v = V1.rearrange("p (n g d) -> p n g d", g=2, d=65)
        wout = alloc(8 * 1024, BF16)
        woutv = wout.rearrange("p (kc n) -> p kc n", n=1024)
        wrb = alloc(8 * NE, BF16)
        wrv = wrb.rearrange("p (kc n) -> p kc n", n=NE)
        UT = alloc(128, BF16)
        persist_off = off[0]

        dma('sp', ident, id_d[:, :], [], ['ident'])
        cp('dve', identb, ident, ['ident'], ['identb'])
        memset('pool', V1, 1.0, [], ['V1'])
        memset('pool', carry, 0.0, [], ['carry'])

        c8 = alloc(8)
        csil = alloc(8)
        ones = alloc(128)
        cB = alloc(8 * 128)
        modA = alloc(2048)
        wab = [alloc(8 * 512), alloc(8 * 512)]
        badab = [alloc(512), alloc(512)]
        dma('sp', c8, c8_d[:, :], [], ['c8'])
        act(csil, c8, AF.Silu, ['c8'], ['csil'])
        memset('dve', ones, 1.0, [], ['ones'])
        for kc in range(8):
            ts('dve', cB[:, kc * 128:(kc + 1) * 128], ones, csil[:, kc:kc + 1], ALU.mult, ['ones', 'csil'], ['cB'])
        wada_v = wada_d.rearrange("(kc p) n -> p kc n", p=128)
        for ch in range(12):
            wb = wab[ch % 2]
            bb = badab[ch % 2]
            wbv = wb.rearrange("p (kc n) -> p kc n", n=512)
            dma('sp', wbv, wada_v[:, :, ch * 512:(ch + 1) * 512], [], [f'wab{ch % 2}'])
            dma('sp', bb, bada_d[:, ch * 512:(ch + 1) * 512].partition_broadcast(128), [], [f'badab{ch % 2}'])
            b = ch % 2
            for kc in range(8):
                mm(bank(b), cB[:, kc * 128:(kc + 1) * 128], wbv[:, kc, :], kc == 0, kc == 7,
                   ['cB', f'wab{ch % 2}'], [pk(b)])
            if ch < 4:
                dst = modA[:, ch * 512:(ch + 1) * 512]
                dkey = 'modA'
            else:
                dst = modB[:, (ch - 4) * 512:(ch - 3) * 512]
                dkey = 'modB'
            tt('dve', dst, bank(b), bb, ALU.add, [pk(b), f'badab{ch % 2}'], [dkey])
        ts('dve', modA[:, 1024:2048], modA[:, 1024:2048], 1.0, ALU.add, ['modA'], ['modA'])
        ts('dve', GA1, GA1, 1.0, ALU.add, ['modB'], ['modB'])
        ts('dve', SCF1, SCF1, 1.0, ALU.add, ['modB'], ['modB'])
        ts('dve', GF1, GF1, 1.0, ALU.add, ['modB'], ['modB'])
        for c in range(8):
            tp(bank(2 + (c % 2))[:, 0:128], modA[:, c * 128:(c + 1) * 128], ident, ['modA', 'ident'], [pk(2 + (c % 2))])
            cp('dve', SHA[:, c:c + 1], bank(2 + (c % 2))[:, 0:1], [pk(2 + (c % 2))], ['SHA'])
        for c in range(8):
            tp(bank(2 + (c % 2))[:, 0:128], modA[:, 1024 + c * 128:1024 + (c + 1) * 128], ident, ['modA', 'ident'], [pk(2 + (c % 2))])
            cp('dve', SCA1[:, c:c + 1], bank(2 + (c % 2))[:, 0:1], [pk(2 + (c % 2))], ['SCA1'])
        dbg_out("d_modB", modB, ['modB'])
        S.barrier(exclude=('zf',))
        off[0] = persist_off

        if stage == 'A':
            S.enabled = False
        win = alloc(8 * NCOL, BF16)
        winv = win.rearrange("p (kc n) -> p kc n", n=NCOL)
        xtb = [alloc(1024), alloc(1024)]
        csb = [alloc(64), alloc(64)]
        snb = [alloc(64), alloc(64)]
        hT = alloc(1024, BF16)
        tmA = alloc(512, BF16)
        tmB = alloc(512, BF16)
        QS = alloc(768, BF16)
        t1 = alloc(256)
        t2 = alloc(256)
        t3 = alloc(256)
        t4 = alloc(256)
        qkrot_b = [alloc(1024, BF16), alloc(1024, BF16)]
        qkT = alloc(1024, BF16)
        qdT = alloc(512, BF16)
        kdk = alloc(512, BF16)
        ATb = alloc(512, BF16)
        VR_b = [alloc(512, BF16), alloc(512, BF16)]
        SG_b = [alloc(512), alloc(512)]
        S32 = alloc(512)
        S16 = alloc(512, BF16)
        DTt = alloc(512)
        QDt = alloc(512)
        CHt = alloc(512)
        KDt = alloc(4)
        RNG = alloc(512)
        rn = alloc(512)
        retb = alloc(512, BF16)
        bst = alloc(24)
        bmv = alloc(8)
        sd4 = alloc(4)
        rs4 = alloc(4)

        win_v = win_d.rearrange("(kc p) n -> p kc n", p=128)
        for kc in range(8):
            dma('pool', winv[:, kc, :], win_v[:, kc, :], [], [f'win{kc}'])
        for kc in range(8):
            dma('pool', woutv[:, kc, :], wout_d[kc * 128:(kc + 1) * 128, :], [], [f'wout{kc}'])
        dma('pool', wrv, wr_d.rearrange("(kc p) n -> p kc n", p=128), [], ['wrb'])
        dma('pool', UT, ut_d[:, :], [], ['UT'])
        memset('dve', ZT, 0.0, [], ['ZT'])
        ZKEYS = []
        for zi in range(NE * CAP // 512):
            ZKEYS.append(f'xgz{zi}')
            S.add('pool', (lambda z: (lambda e: e.dma_start(out=xg_d[z * 512:(z + 1) * 512, :].rearrange("(a p) f -> p a f", p=128),
                                                            in_=ZT.rearrange("p (a f) -> p a f", f=1024))))(zi),
                  ['ZT'], [f'xgz{zi}'], dma=True, grp='zf')
        dma('sp', DTt, dt_d[:, :], [], ['DTt'])
        dma('sp', QDt, qd_d[:, :], [], ['QDt'])
        dma('sp', CHt, ch_d[:, :], [], ['CHt'])
        dma('sp', KDt, kd_d[:, :], [], ['KDt'])
        dma('sp', RNG, rng_d.partition_broadcast(128), [], ['RNG'])

        S.cut('pro')
        CH = [(0, 512), (512, 512), (1024, 132), (1156, 512), (1668, 512), (2180, 512), (2692, 512)]

        def proj_chunk(ci, b):
            c0, wd = CH[ci]
            for kc in range(8):
                mm(bank(b)[:, 0:wd], hT[:, kc * 128:(kc + 1) * 128], winv[:, kc, c0:c0 + wd], kc == 0, kc == 7,
                   ['hT', f'win{kc}'], [pk(b)])

        def b1_proj(n):
            xt = xtb[n % 2]
            xk = f'xt{n % 2}'
            qkrot, VR, SG = qkrot_b[n % 2], VR_b[n % 2], SG_b[n % 2]
            cs = csb[n % 2]
            sn = snb[n % 2]
            dma('sp', xt, x_d[n * 128:(n + 1) * 128, :], [], [xk])
            dma('sp', cs, cos_d[n * 128:(n + 1) * 128, :], [], [f'cs{n % 2}'])
            dma('sp', sn, sin_d[n * 128:(n + 1) * 128, :], [], [f'sn{n % 2}'])
            for c in range(8):
                b = 0 if c < 4 else 1
                tp(bank(b)[:, (c % 4) * 128:(c % 4 + 1) * 128], xt[:, c * 128:(c + 1) * 128], ident, [xk, 'ident'], [pk(b)])
            for c in range(8):
                b = 0 if c < 4 else 1
                ts('dve', hT[:, c * 128:(c + 1) * 128], bank(b)[:, (c % 4) * 128:(c % 4 + 1) * 128], SCA1[:, c:c + 1], ALU.mult,
                   [pk(b), 'SHA', 'SCA1'], ['hT'], s2=SHA[:, c:c + 1], op1=ALU.add)
            yield
            proj_chunk(0, 2)
            cp('dve', tmA, bank(2), [pk(2)], ['tmA'])
            yield
            proj_chunk(1, 3)
            cp('act', tmB, bank(3), [pk(3)], ['tmB'])
            yield
            for c in range(4):
                tp(bankb(4)[:, c * 128:(c + 1) * 128], tmA[:, c * 128:(c + 1) * 128], identb, ['tmA', 'identb'], [pk(4)])
            for c in range(4):
                tp(bankb(4)[:, 512 + c * 128:512 + (c + 1) * 128], tmB[:, c * 128:(c + 1) * 128], identb, ['tmB', 'identb'], [pk(4)])
            cp('dve', QS[:, 0:512], bankb(4)[:, 0:512], [pk(4)], ['QS'])
            cp('dve', QS[:, 512:768], bankb(4)[:, 640:896], [pk(4)], ['QS'])
            cp('dve', KT[:, n * 128:(n + 1) * 128], bankb(4)[:, 512:640], [pk(4)], [f'KT{n}'])
            cp('dve', KIT[:, n * 128:(n + 1) * 128], bankb(4)[:, 896:1024], [pk(4)], [f'KIT{n}'])
            dma('sp', qs_d[n, :, :], QS, ['QS'], [f'qsd{n}'])
            yield
            proj_chunk(2, 2)
            cp('dve', V1v[:, n, :, 0:64], bank(2)[:, 0:128].rearrange("p (g d) -> p g d", d=64), [pk(2), 'V1'], [f'V1_{n}'])
            ts('dve', WI[:, n * 4:(n + 1) * 4], bank(2)[:, 128:132], 0.0625, ALU.mult, [pk(2)], [f'WI{n}'])
            yield
            proj_chunk(3, 3)
            yield
            proj_chunk(4, 2)
            cosB = cs.unsqueeze(1).to_broadcast([128, 4, 64])
            sinB = sn.unsqueeze(1).to_broadcast([128, 4, 64])
            qkv = qkrot.rearrange("p (a two d) -> p a two d", two=2, d=64)
            for which, b in ((0, 3), (1, 2)):
                pv = bank(b).rearrange("p (h two d) -> p h two d", two=2, d=64)
                x1v = pv[:, :, 0, :]
                x2v = pv[:, :, 1, :]
                t1v = t1.rearrange("p (h d) -> p h d", d=64)
                t2v = t2.rearrange("p (h d) -> p h d", d=64)
                t3v = t3.rearrange("p (h d) -> p h d", d=64)
                t4v = t4.rearrange("p (h d) -> p h d", d=64)
                ck = [f'cs{n % 2}', f'sn{n % 2}']
                tt('dve', t1v, x1v, cosB, ALU.mult, [pk(b)] + ck, ['t1'])
                tt('dve', t2v, x2v, sinB, ALU.mult, [pk(b)] + ck, ['t2'])
                tt('dve', t3v, x1v, sinB, ALU.mult, [pk(b)] + ck, ['t3'])
                tt('dve', t4v, x2v, cosB, ALU.mult, [pk(b)] + ck, ['t4'])
                tt('dve', qkv[:, which * 4:(which + 1) * 4, 0, :], t1v, t2v, ALU.subtract, ['t1', 't2'], ['qkrot' + str(n % 2)])
                tt('dve', qkv[:, which * 4:(which + 1) * 4, 1, :], t3v, t4v, ALU.add, ['t3', 't4'], ['qkrot' + str(n % 2)])
            yield
            proj_chunk(5, 3)
            cp('act', VR, bank(3), [pk(3)], ['VR' + str(n % 2)])
            yield
            proj_chunk(6, 2)
            act(SG, bank(2), AF.Silu, [pk(2)], ['SG' + str(n % 2)])
            yield

        def b1_ret(n):
            qkrot, VR, SG = qkrot_b[n % 2], VR_b[n % 2], SG_b[n % 2]
            for a in range(8):
                tp(bankb(4)[:, a * 128:(a + 1) * 128], qkrot[:, a * 128:(a + 1) * 128], identb, ['qkrot' + str(n % 2), 'identb'], [pk(4)])
            cp('dve', qkT, bankb(4), [pk(4)], ['qkT'])
            tt('dve', qdT, bankb(4)[:, 0:512], QDt, ALU.mult, [pk(4), 'QDt'], ['qdT'])
            tt('dve', kdk.rearrange("p (h d) -> p h d", d=128), qkrot[:, 512:1024].rearrange("p (h d) -> p h d", d=128),
               KDt.unsqueeze(2).to_broadcast([128, 4, 128]), ALU.mult, ['qkrot' + str(n % 2), 'KDt'], ['kdk'])
            yield
            for h in range(4):
                mm(bank(5)[:, h * 128:(h + 1) * 128], qkT[:, 512 + h * 128:512 + (h + 1) * 128], qkT[:, h * 128:(h + 1) * 128],
                   True, True, ['qkT'], [pk(5)])
            tt('dve', ATb, bank(5), DTt, ALU.mult, [pk(5), 'DTt'], ['ATb'])
            for h in range(4):
                hs = slice(h * 128, (h + 1) * 128)
                mm(bank(6)[:, hs], ATb[:, hs], VR[:, hs], True, n == 0, ['ATb', 'VR' + str(n % 2)], [pk(6)])
                if n > 0:
                    mm(bank(6)[:, hs], qdT[:, hs], S16[:, hs], False, True, ['qdT', 'S16'], [pk(6)])
            yield
            yield
            for h in range(4):
                hs = slice(h * 128, (h + 1) * 128)
                mm(bank(7)[:, hs], kdk[:, hs], VR[:, hs], True, True, ['kdk', 'VR' + str(n % 2)], [pk(7)])
            if n == 0:
                cp('dve', S32, bank(7), [pk(7)], ['S32'])
            else:
                tt('dve', S32, S32, CHt, ALU.mult, ['S32', 'CHt'], ['S32'])
                tt('dve', S32, S32, bank(7), ALU.add, ['S32', pk(7)], ['S32'])
            if n < NT - 1:
                cp('act', S16, S32, ['S32'], ['S16'])
            yield
            for h in range(4):
                op('dve', 'bn_stats', [pk(6)], ['bst'], out=bst[:, h * 6:(h + 1) * 6], in_=bank(6)[:, h * 128:(h + 1) * 128])
            for h in range(4):
                op('dve', 'bn_aggr', ['bst'], ['bmv'], out=bmv[:, h * 2:(h + 1) * 2], in_=bst[:, h * 6:(h + 1) * 6])
            yield
            bmvv = bmv.rearrange("p (h two) -> p h two", two=2)
            ts('dve', sd4, bmvv[:, :, 1], EPS, ALU.add, ['bmv'], ['sd4'])
            act(sd4, sd4, AF.Sqrt, ['sd4'], ['sd4'])
            op('dve', 'reciprocal', ['sd4'], ['rs4'], out=rs4, in_=sd4)
            for h in range(4):
                hs = slice(h * 128, (h + 1) * 128)
                ts('dve', rn[:, hs], bank(6)[:, hs], bmv[:, 2 * h:2 * h + 1], ALU.subtract, [pk(6), 'bmv', 'rs4'], ['rn'],
                   s2=rs4[:, h:h + 1], op1=ALU.mult)
            yield
            tt('dve', rn, rn, RNG, ALU.mult, ['rn', 'RNG'], ['rn'])
            tt('dve', retb, rn, SG, ALU.mult, ['rn', 'SG' + str(n % 2)], ['retb'])
            dma('sp', ret_d[n * 128:(n + 1) * 128, :], retb, ['retb'], [f'retd{n}'])
            yield

        def run_gen(g):
            for _ in g:
                pass

        def interleave2(ga, gb):
            da = db = False
            while not (da and db):
                if not da:
                    try:
                        next(ga)
                    except StopIteration:
                        da = True
                if not db:
                    try:
                        next(gb)
                    except StopIteration:
                        db = True

        run_gen(b1_proj(0))
        for n in range(NT):
            interleave2(b1_ret(n), b1_proj(n + 1) if n + 1 < NT else iter(()))
        S.barrier(exclude=('zf',))
        off[0] = persist_off

        if stage == 'B1':
            S.enabled = False
        scoreb = [alloc(L), alloc(L)]
        penb = [alloc(L, BF16), alloc(L, BF16), alloc(L, BF16)]
        junk_b = [alloc(L, BF16), alloc(L, BF16)]
        xtb = [alloc(1024), alloc(1024)]
        QSb = [alloc(768, BF16), alloc(768, BF16), alloc(768, BF16)]
        rb = [alloc(512), alloc(512)]
        pTb = [alloc(1024, BF16), alloc(1024, BF16)]
        BT8 = alloc(2048, BF16)
        IREP = alloc(512, BF16)
        nrmin_b = [alloc(1), alloc(1)]
        hw__b = [alloc(1), alloc(1)]
        C31 = alloc(8)
        NEGD = alloc(128)
        POW = alloc(NIT + 2)
        W2_b = [alloc(NIT + 2), alloc(NIT + 2)]
        W2h_b = [alloc(NIT + 2), alloc(NIT + 2)]
        mid_b = [alloc(NIT + 2), alloc(NIT + 2)]
        cnt_b = [alloc(NIT + 2), alloc(NIT + 2)]
        uu_b = [alloc(NIT + 2), alloc(NIT + 2)]
        rmax_b = [alloc(1), alloc(1)]
        rmin_b = [alloc(1), alloc(1)]
        rrng_b = [alloc(1), alloc(1)]
        thr_b = [alloc(1), alloc(1)]
        rec8 = alloc(8)
        cat = alloc(1024, BF16)
        catT = alloc(1024, BF16)
        tmp_off = off[0]
        tmp = alloc(1024)
        yv = alloc(1024)
        BTt = arena[:, tmp_off:tmp_off + 2048]
        x1 = alloc(1024)
        LN1G = alloc(1024)
        LN1B = alloc(1024)
        h2 = alloc(1024, BF16)
        h2T = alloc(1024, BF16)
        BR = alloc(NE)
        ONEb = alloc(128, BF16)
        EC = alloc(NE)
        lg = alloc(NE)
        mx8 = alloc(8)
        nm = alloc(1)
        ex4 = alloc(4)
        sm = alloc(1)
        rsm = alloc(1)
        selb = alloc(NE, BF16)
        pos = alloc(NE)
        flat = alloc(NE)
        ohj = alloc(NE)
        idxf = alloc(4)
        lst = alloc(12)
        lmv = alloc(2)
        lsd = alloc(1)
        lrs = alloc(1)

        memset('dve', ONEb, 1.0, [], ['ONEb'])
        dma('sp', BTt, bt_d[:, :], [], ['BTt', 'tmp', 'yv'])
        dma('sp', C31, c31_d[:, :], [], ['C31'])
        dma('sp', NEGD, negd_d[:, :], [], ['NEGD'])
        dma('sp', POW, pow_d[:, :], [], ['POW'])
        dma('sp', EC, ec_d[:, :], [], ['EC'])
        dma('sp', LN1G, ln1g_d.partition_broadcast(128), [], ['LN1G'])
        dma('sp', LN1B, ln1b_d.partition_broadcast(128), [], ['LN1B'])
        dma('sp', BR, br_d.partition_broadcast(128), [], ['BR'])
        BTv = BTt.rearrange("p (k h t) -> p k h t", k=2, h=8)
        for k in range(2):
            tt('dve', BTv[:, k, :, :], BTv[:, k, :, :], C31.unsqueeze(2).to_broadcast([128, 8, 128]), ALU.subtract,
               ['BTt', 'C31'], ['BTt'])

        ts('dve', BT8, BTt, 8.0, ALU.mult, ['BTt', 'tmp', 'yv'], ['BT8'])
        for c in range(4):
            cp('dve', IREP[:, c * 128:(c + 1) * 128], identb, ['identb'], ['IREP'])

        def layer_norm(eng_src, src, gtab, btab, dst, rkeys, wkey, gk, bk):
            op('dve', 'bn_stats', rkeys, ['lst'], out=lst[:, 0:6], in_=src[:, 0:512])
            op('dve', 'bn_stats', rkeys, ['lst'], out=lst[:, 6:12], in_=src[:, 512:1024])
            op('dve', 'bn_aggr', ['lst'], ['lmv'], out=lmv, in_=lst.rearrange("p (a s) -> p a s", s=6))
            ts('dve', lsd, lmv[:, 1:2], EPS, ALU.add, ['lmv'], ['lsd'])
            act(lsd, lsd, AF.Sqrt, ['lsd'], ['lsd'])
            op('dve', 'reciprocal', ['lsd'], ['lrs'], out=lrs, in_=lsd)
            ts('dve', src, src, lmv[:, 0:1], ALU.subtract, rkeys + ['lmv', 'lrs'], rkeys, s2=lrs[:, 0:1], op1=ALU.mult)
            tt('dve', src, src, gtab, ALU.mult, rkeys + [gk], rkeys)
            tt('dve', dst, src, btab, ALU.add, rkeys + [bk], [wkey])

        def stage_scores(n):
            Sn = (n + 1) * 128
            QSn = QSb[n % 3]
            qk = f'QSb{n % 3}'
            pen = penb[n % 3]
            pnk = f'pen{n % 3}'
            q2 = n % 2
            score = scoreb[q2]
            junk = junk_b[q2]
            W2, mid, cnt, uu = W2_b[q2], mid_b[q2], cnt_b[q2], uu_b[q2]
            rmax, rmin, rrng, thr, nrmin, hw_ = rmax_b[q2], rmin_b[q2], rrng_b[q2], thr_b[q2], nrmin_b[q2], hw__b[q2]
            dma('sp', QSn, qs_d[n, :, :], [f'qsd{n}'], [qk])
            QITc = QSn[:, 512:768]
            kit_keys = [f'KIT{j}' for j in range(n + 1)]
            nchunk = (Sn + 511) // 512
            it = 0
            for c in range(nchunk):
                wd = min(512, Sn - c * 512)
                for h in range(4):
                    b = it % 2
                    it += 1
                    half = slice((h % 2) * 64, (h % 2) * 64 + 64)
                    mm(bank(b)[:, 0:wd], QITc[half, (h // 2) * 128:(h // 2 + 1) * 128], KIT[half, c * 512:c * 512 + wd], True, True,
                       [qk] + kit_keys[c * 4:c * 4 + 4], [pk(b)])
                    act(rb[b][:, 0:wd], bank(b)[:, 0:wd], AF.Relu, [pk(b)], [f'rb{b}'])
                    if h == 0:
                        ts('dve', score[:, c * 512:c * 512 + wd], rb[b][:, 0:wd], WI[:, n * 4:n * 4 + 1], ALU.mult,
                           [f'rb{b}', f'WI{n}'], ['score' + str(q2)])
                    else:
                        stt('dve', score[:, c * 512:c * 512 + wd], rb[b][:, 0:wd], WI[:, n * 4 + h:n * 4 + h + 1],
                            score[:, c * 512:c * 512 + wd], ALU.mult, ALU.add, [f'rb{b}', f'WI{n}', 'score' + str(q2)], ['score' + str(q2)])
                yield
            if Sn <= 256:
                tt('dve', score[:, n * 128:(n + 1) * 128], score[:, n * 128:(n + 1) * 128], NEGD, ALU.add, ['score' + str(q2), 'NEGD'], ['score' + str(q2)])
                memset('dve', thr, -1e29, [], ['thr' + str(q2)])
                yield
            else:
                op('dve', 'tensor_reduce', ['score' + str(q2)], ['rmax' + str(q2)], out=rmax, in_=score[:, 0:Sn], axis=AX.X, op=ALU.max)
                op('dve', 'tensor_reduce', ['score' + str(q2)], ['rmin' + str(q2)], out=rmin, in_=score[:, 0:Sn], axis=AX.X, op=ALU.min)
                tt('dve', score[:, n * 128:(n + 1) * 128], score[:, n * 128:(n + 1) * 128], NEGD, ALU.add, ['score' + str(q2), 'NEGD'], ['score' + str(q2)])
                on_dve = (n % 2 == 1)
                if on_dve:
                    tt('dve', rrng, rmax, rmin, ALU.subtract, ['rmax' + str(q2), 'rmin' + str(q2)], ['rrng' + str(q2)])
                    ts('dve', nrmin, rmin, 1.0, ALU.mult, ['rmin' + str(q2)], ['nrmin' + str(q2)])
                else:
                    tt('dve', rrng, rmin, rmax, ALU.subtract, ['rmax' + str(q2), 'rmin' + str(q2)], ['rrng' + str(q2)])
                    ts('dve', nrmin, rmin, -1.0, ALU.mult, ['rmin' + str(q2)], ['nrmin' + str(q2)])
                ts('dve', W2, POW, rrng[:, 0:1], ALU.mult, ['POW', 'rrng' + str(q2)], ['W2' + str(q2)])
                stt('dve', mid[:, 0:1], W2[:, 1:2], 1.0, nrmin, ALU.mult, ALU.add, ['W2' + str(q2), 'nrmin' + str(q2)], ['mid' + str(q2)])
                W2h = W2h_b[q2]
                ts('dve', W2h, W2, 0.5, ALU.mult, ['W2' + str(q2)], ['W2' + str(q2)])
                memset('dve', cnt, 0.0, [], ['cnt' + str(q2)])
                yield
                for k in range(NIT):
                    if on_dve:
                        ts('dve', junk[:, 0:Sn], score[:, 0:Sn], mid[:, k:k + 1], ALU.is_ge, ['score' + str(q2), 'mid' + str(q2), 'cnt' + str(q2)],
                           ['junk' + str(q2), 'cnt' + str(q2)], s2=0.0, op1=ALU.add, accum=cnt[:, k:k + 1])
                        ts('dve', uu[:, k:k + 1], cnt[:, k:k + 1], 255.5, ALU.is_ge, ['cnt' + str(q2)], ['uu' + str(q2)], s2=0.5, op1=ALU.subtract)
                    else:
                        act(junk[:, 0:Sn], score[:, 0:Sn], AF.Sign, ['score' + str(q2), 'mid' + str(q2), 'cnt' + str(q2)], ['junk' + str(q2), 'cnt' + str(q2)], bias=mid[:, k:k + 1], scale=1.0,
                            accum=cnt[:, k:k + 1])
                        act(uu[:, k:k + 1], cnt[:, k:k + 1], AF.Sign, ['cnt' + str(q2)], ['uu' + str(q2)], bias=float(Sn - 510.5), scale=1.0)
                        act(mid[:, k + 1:k + 2], uu[:, k:k + 1], AF.Identity, ['uu' + str(q2), 'W2' + str(q2), 'mid' + str(q2)], ['mid' + str(q2)],
                            bias=mid[:, k:k + 1], scale=W2h[:, k + 1:k + 2])
                        yield
                        continue
                    stt('dve', mid[:, k + 1:k + 2], uu[:, k:k + 1], W2[:, k + 1:k + 2], mid[:, k:k + 1], ALU.mult, ALU.add,
                        ['uu' + str(q2), 'W2' + str(q2), 'mid' + str(q2)], ['mid' + str(q2)])
                    yield
                ts('dve', hw_, W2[:, NIT:NIT + 1], 0.5, ALU.mult, ['W2' + str(q2)], ['hw_' + str(q2)])
                if on_dve:
                    tt('dve', thr, mid[:, NIT:NIT + 1], hw_, ALU.subtract, ['hw_' + str(q2), 'mid' + str(q2)], ['thr' + str(q2)])
                else:
                    stt('dve', thr, mid[:, NIT:NIT + 1], -1.0, hw_, ALU.mult, ALU.add, ['hw_' + str(q2), 'mid' + str(q2)], ['thr' + str(q2)])
            ts('dve', pen[:, 0:Sn], score[:, 0:Sn], thr[:, 0:1], ALU.is_lt, ['score' + str(q2), 'thr' + str(q2)], [pnk], s2=-240000.0, op1=ALU.mult)
            yield

        def stage_attn(n):
            xt = xtb[n % 2]
            xk = f'xt{n % 2}'
            QSn = QSb[n % 3]
            qk = f'QSb{n % 3}'
            pen = penb[n % 3]
            pnk = f'pen{n % 3}'
            QTc = QSn[:, 0:512]
            for j in range(n + 1):
                jb = j % 2
                Lp = PS[1 + jb][:, :]
                lk = [pk(2 + 2 * jb), pk(3 + 2 * jb)]
                near = j >= n - 1
                kind = 0 if j == n else 1
                for g in range(2):
                    hp = slice(g * 64, g * 64 + 64)
                    og = Lp[:, g * 512:(g + 1) * 512]
                    mm(og, KT[hp, j * 128:(j + 1) * 128], QTc[hp, :], True, False, [qk, f'KT{j}'], lk)
                    mm(og, pen[:, j * 128:(j + 1) * 128], IREP, False, not near, [pnk, 'IREP'], lk)
                    if near:
                        mm(og, identb, BT8[:, kind * 1024 + g * 512:kind * 1024 + (g + 1) * 512], False, True, ['identb', 'BT8'], lk)
                pT = pTb[jb]
                pk_ = f'pT{jb}'
                act(pT, Lp, AF.Exp, lk, [pk_], scale=0.125)
                for h in range(8):
                    g = h // 4
                    ob = 6 + g
                    mm(bank(ob)[:, (h % 4) * 65:(h % 4) * 65 + 65], pT[:, h * 128:(h + 1) * 128], V1v[:, j, g, :],
                       j == 0 and h % 4 == 0, j == n, [pk_, f'V1_{j}', 'V1'], [pk(ob)], skip=True)
                yield
            for g in range(2):
                ov = bank(6 + g)[:, 0:260].rearrange("p (h d) -> p h d", d=65)
                op('dve', 'reciprocal', [pk(6 + g)], ['rec8'], out=rec8[:, g * 4:(g + 1) * 4], in_=ov[:, :, 64])
                tt('dve', cat[:, g * 256:(g + 1) * 256].rearrange("p (h d) -> p h d", d=64), ov[:, :, 0:64],
                   rec8[:, g * 4:(g + 1) * 4].unsqueeze(2).to_broadcast([128, 4, 64]), ALU.mult, [pk(6 + g), 'rec8'], ['cat_a'])
            yield

        def stage_tail(n):
            xt = xtb[n % 2]
            xk = f'xt{n % 2}'
            dma('sp', xt, x_d[n * 128:(n + 1) * 128, :], [], [xk])
            dma('sp', cat[:, 512:1024], ret_d[n * 128:(n + 1) * 128, :], [f'retd{n}'], ['cat_r'])
            for c in range(8):
                tp(bankb(0)[:, c * 128:(c + 1) * 128], cat[:, c * 128:(c + 1) * 128], identb, ['cat_a', 'cat_r', 'identb'], [pk(0)])
            cp('dve', catT, bankb(0), [pk(0)], ['catT'])
            yield
            Mp = PS[0][:, :]
            mk = [pk(0), pk(1)]
            for half in range(2):
                for kc in range(8):
                    mm(Mp[:, half * 512:(half + 1) * 512], catT[:, kc * 128:(kc + 1) * 128], woutv[:, kc, half * 512:(half + 1) * 512],
                       kc == 0, kc == 7, ['catT', f'wout{kc}'], mk)
            tt('dve', tmp, Mp, GA1, ALU.mult, mk + ['modB'], ['tmp'])
            stt('dve', yv, xt, ALPHA, tmp, ALU.mult, ALU.add, [xk, 'tmp'], ['yv'])
            yield
            layer_norm('dve', yv, LN1G, LN1B, x1, ['yv'], 'x1', 'LN1G', 'LN1B')
            dma('sp', x1_d[n * 128:(n + 1) * 128, :], x1, ['x1'], [f'x1d{n}'])
            dbg_out("d_x1", x1, ['x1'], rows=(n * 128, (n + 1) * 128))
            yield
            tt('dve', tmp, x1, SCF1, ALU.mult, ['x1', 'modB'], ['tmp'])
            tt('dve', h2, tmp, SHF, ALU.add, ['tmp', 'modB'], ['h2'])
            for c in range(8):
                tp(bankb(1)[:, c * 128:(c + 1) * 128], h2[:, c * 128:(c + 1) * 128], identb, ['h2', 'identb'], [pk(1)])
            cp('dve', h2T, bankb(1), [pk(1)], ['h2T'])
            yield
            for kc in range(8):
                mm(bank(0)[:, 0:NE], h2T[:, kc * 128:(kc + 1) * 128], wrv[:, kc, :], kc == 0, kc == 7, ['h2T', 'wrb'], [pk(0)])
            tt('dve', lg, bank(0)[:, 0:NE], BR, ALU.add, [pk(0), 'BR'], ['lg'])
            op('dve', 'max', ['lg'], ['mx8'], out=mx8, in_=lg)
            ts('dve', nm, mx8[:, 0:1], -1.0, ALU.mult, ['mx8'], ['nm'])
            memset('dve', sm, 0.0, [], ['sm'])
            act(ex4, mx8[:, 0:4], AF.Exp, ['mx8', 'nm', 'sm'], ['ex4', 'sm'], bias=nm[:, 0:1], scale=1.0, accum=sm)
            op('dve', 'reciprocal', ['sm'], ['rsm'], out=rsm, in_=sm)
            ts('dve', GATES[:, n * 4:(n + 1) * 4], ex4, rsm[:, 0:1], ALU.mult, ['ex4', 'rsm'], [f'GATES{n}'])
            yield
            ts('dve', selb, lg, mx8[:, 3:4], ALU.is_ge, ['lg', 'mx8'], ['selb'])
            mm(bank(0)[:, 64:64 + NE], UT, selb, True, True, ['UT', 'selb'], [pk(0)])
            mm(bank(0)[:, 128:128 + NE], ONEb, selb, True, True, ['ONEb', 'selb'], [pk(0)])
            tt('dve', pos, bank(0)[:, 64:64 + NE], carry, ALU.add, [pk(0), 'carry'], ['pos'])
            tt('dve', carry, carry, bank(0)[:, 128:128 + NE], ALU.add, [pk(0), 'carry'], ['carry'])
            yield
            stt('dve', flat, pos, float(CAP - 1), EC, ALU.min, ALU.add, ['pos', 'EC'], ['flat'])
            memset('dve', idxf, 0.0, [], ['idxf'])
            for k in range(4):
                stt('dve', ohj, lg, mx8[:, k:k + 1], flat, ALU.is_equal, ALU.mult, ['lg', 'mx8', 'flat', 'idxf'], ['ohj', 'idxf'],
                    accum=idxf[:, k:k + 1])
            cp('dve', IDXT[:, n * 4:(n + 1) * 4], idxf, ['idxf'], [f'IDX{n}'])
            for k in range(4):
                op('pool', 'indirect_dma_start', ['h2', f'IDX{n}'] + ZKEYS, [f'xgd{n}_{k}'], dma=True,
                   out=xg_d[:, :], out_offset=bass.IndirectOffsetOnAxis(ap=IDXT[:, n * 4 + k:n * 4 + k + 1], axis=0),
                   in_=h2, in_offset=None)
            yield

        def run_all(g):
            for _ in g:
                pass

        def nsteps_scores(n):
            return ((n + 1) * 128 + 511) // 512 + NIT + 3

        def interleave(items):
            done = [0] * len(items)
            while True:
                best = None
                for i, (g, q) in enumerate(items):
                    if done[i] >= q:
                        continue
                    frac = done[i] / q
                    if best is None or frac < best[0]:
                        best = (frac, i)
                if best is None:
                    break
                i = best[1]
                try:
                    next(items[i][0])
                except StopIteration:
                    pass
                done[i] += 1

        gens = {0: stage_scores(0)}
        prog_ = {0: 0}
        run_all(gens[0])
        prog_[0] = nsteps_scores(0) + 1
        if NT > 1:
            gens[1] = stage_scores(1)
            prog_[1] = 0
        for n in range(NT + 1):
            items = []
            if n < NT:
                items.append([stage_attn(n), n + 3])
            if n >= 1:
                items.append([stage_tail(n - 1), 9])
            if n + 1 < NT:
                tot = nsteps_scores(n + 1) + 1
                items.append([gens[n + 1], tot - prog_[n + 1]])
                prog_[n + 1] = tot
            if n + 2 < NT:
                gens[n + 2] = stage_scores(n + 2)
                half = (nsteps_scores(n + 2) + 1) // 2
                items.append([gens[n + 2], half])
                prog_[n + 2] = half
            interleave(items)
        if "d_gates" in dbg_d:
            dma('sp', dbg_d["d_gates"][:, :], GATES, [f'GATES{n}' for n in range(NT)], [])
        if "d_idx" in dbg_d:
            cp('dve', tmp[:, 0:128], IDXT, [f'IDX{n}' for n in range(NT)], ['tmp'])
            dma('sp', dbg_d["d_idx"][:, :], tmp[:, 0:128], ['tmp'], [])
        S.barrier()
        off[0] = persist_small

        if stage == 'B2':
            S.enabled = False
        Wgu = [alloc(8 * 2048, BF16), alloc(8 * 2048, BF16)]
        Wdn = [alloc(8 * 1024, BF16)]
        XRb = [alloc(1024, BF16) for _ in range(NSLOT)]
        XT = alloc(8 * CAP, BF16)
        XTv = XT.rearrange("p (kc s) -> p kc s", s=CAP)
        Aact = alloc(8 * CAP, BF16)
        Av = Aact.rearrange("p (m s) -> p m s", s=CAP)
        BD = alloc(1024)
        BGU = alloc(NE * 16)
        gq = alloc(512)
        sgm = alloc(512)
        uq = alloc(512)
        tq = alloc(512)
        yo = [alloc(1024), alloc(1024)]
        dma('sp', BGU, bgu_d[:, :], [], ['BGU'])
        BGA = alloc(NE * 16)
        F7 = float(7.0 / (1.0 + np.exp(-1.702 * 7.0)))
        BGUv = BGU.rearrange("p (e c) -> p e c", c=16)
        BGAv = BGA.rearrange("p (e c) -> p e c", c=16)
        ts('dve', BGAv[:, :, 0:8], BGUv[:, :, 0:8], 1.702, ALU.mult, ['BGU'], ['BGA'])
        ts('dve', BGAv[:, :, 8:16], BGUv[:, :, 8:16], 1.0, ALU.add, ['BGU'], ['BGA'])

        STG = [alloc(2048) for _ in range(3)]
        chunks = []
        for kc_ in range(8):
            chunks.append(('g', 0, kc_))
        for q_ in range(4):
            chunks.append(('d', 0, q_))
        for ex_ in range(NE):
            for m_ in range(8):
                if ex_ >= 1 and m_ < 4:
                    chunks.append(('d', ex_, m_))
                if ex_ + 1 < NE:
                    chunks.append(('g', ex_ + 1, m_))
        issued = [0]

        def issue_next():
            i = issued[0]
            if i >= len(chunks):
                return
            issued[0] += 1
            kind, ex_, c_ = chunks[i]
            st = STG[i % 3]
            if kind == 'g':
                dma('act', st, wgu_d[ex_, c_ * 128:(c_ + 1) * 128, :], [], [f'stg{i % 3}'])
            else:
                dma('act', st.rearrange("p (m n) -> p m n", n=1024),
                    wd_d[ex_, 2 * c_ * 128:(2 * c_ + 2) * 128, :].rearrange("(m p) n -> p m n", p=128), [], [f'stg{i % 3}'])

        casted = [0]

        def cast_next():
            i = casted[0]
            casted[0] += 1
            kind, ex_, c_ = chunks[i]
            st = STG[i % 3]
            if kind == 'g':
                dst = Wgu[ex_ % 2].rearrange("p (kc n) -> p kc n", n=2048)[:, c_, :]
                cp('act', dst, st, [f'stg{i % 3}'], [f'Wgu{ex_ % 2}_{c_}'])
            else:
                dst = Wdn[0][:, 2 * c_ * 1024:(2 * c_ + 2) * 1024]
                cp('act', dst, st, [f'stg{i % 3}'], [f'Wdn_{2 * c_}', f'Wdn_{2 * c_ + 1}'])
            issue_next()

        for _ in range(3):
            issue_next()
        for _ in range(12):
            cast_next()
        HALVES = [(0, 512), (512, CAP - 512)] if CAP > 512 else [(0, CAP)]

        def load_xr(ex):
            for a in range(NSLOT):
                dma('sp', XRb[a], xg_d[ex * CAP + a * 128:ex * CAP + (a + 1) * 128, :], [], [f'XR{a}'])

        def prep_xt(ex):
            for a in range(NSLOT):
                b = a % 2
                XR = XRb[a]
                for kc in range(8):
                    tp(bankb(b)[:, kc * 128:(kc + 1) * 128], XR[:, kc * 128:(kc + 1) * 128], identb,
                       [f'XR{a}', 'identb'], [pk(b)])
                cp('dve', XTv[:, :, a * 128:(a + 1) * 128], bankb(b).rearrange("p (kc s) -> p kc s", s=128),
                   [pk(b)], ['XT'])

        load_xr(0)
        prep_xt(0)
        for ex in range(NE):
            eb = ex % 2
            Wg = Wgu[eb]
            Wgv = Wg.rearrange("p (kc n) -> p kc n", n=2048)
            Wd = Wdn[0]
            Wdv = Wd.rearrange("p (kc n) -> p kc n", n=1024)
            dma('sp', BD, bd_d[ex:ex + 1, :].partition_broadcast(128), [], ['BD'])
            for m in range(8):
                if ex >= 1 and m < 4:
                    cast_next()
                if ex + 1 < NE:
                    cast_next()
                bg = BGA[:, ex * 16 + m:ex * 16 + m + 1]
                bu = BGA[:, ex * 16 + 8 + m:ex * 16 + 8 + m + 1]
                for hh, (h0_, hw2) in enumerate(HALVES):
                    cs_ = slice(h0_, h0_ + hw2)
                    gb = 2 + hh % 2
                    ub = 4 + hh % 2
                    for (bb_, c0) in ((gb, m * 128), (ub, 1024 + m * 128)):
                        for kc in range(8):
                            mm(bank(bb_)[:, 0:hw2], Wgv[:, kc, c0:c0 + 128], XTv[:, kc, cs_], kc == 0, kc == 7,
                               [f'Wgu{eb}_{kc}', 'XT'], [pk(bb_)])
                    act(sgm[:, 0:hw2], bank(gb)[:, 0:hw2], AF.Silu, [pk(gb), 'BGA'], ['sgm'], bias=bg, scale=1.702)
                    ts('dve', uq[:, 0:hw2], bank(ub)[:, 0:hw2], bu, ALU.add, [pk(ub), 'BGA'], ['uq'], s2=8.0, op1=ALU.min)
                    ts('dve', tq[:, 0:hw2], sgm[:, 0:hw2], 1.0 / 1.702, ALU.mult, ['sgm'], ['tq'], s2=F7, op1=ALU.min)
                    stt('dve', Av[:, m, cs_], uq[:, 0:hw2], -6.0, tq[:, 0:hw2], ALU.max, ALU.mult, ['uq', 'tq'], ['Aact'])
                if m == 0 and ex + 1 < NE:
                    load_xr(ex + 1)
            if ex + 1 < NE:
                prep_xt(ex + 1)
            for a in range(NSLOT):
                Yp = PS[3 if a % 2 == 0 else 0][:, :]
                yk = [pk(6), pk(7)] if a % 2 == 0 else [pk(0), pk(1)]
                for half in range(2):
                    for m in range(8):
                        mm(Yp[:, half * 512:(half + 1) * 512], Av[:, m, a * 128:(a + 1) * 128], Wdv[:, m, half * 512:(half + 1) * 512],
                           m == 0, m == 7, ['Aact', f'Wdn_{m}'], yk)
                tt('dve', yo[a % 2], Yp, BD, ALU.add, yk + ['BD'], [f'yo{a % 2}'])
                dma('sp', y_d[ex * CAP + a * 128:ex * CAP + (a + 1) * 128, :], yo[a % 2], [f'yo{a % 2}'], [f'yd{ex}_{a}'])
        S.barrier()
        off[0] = persist_small

        if stage == 'C':
            S.enabled = False
        YG2 = [[alloc(1024) for _ in range(4)] for _ in range(2)]
        x1b = [alloc(1024), alloc(1024)]
        accb = alloc(1024)
        tmp = alloc(1024)
        ob = [alloc(1024), alloc(1024)]
        LN2G = alloc(1024)
        LN2B = alloc(1024)
        lst = alloc(12)
        lmv = alloc(2)
        lsd = alloc(1)
        lrs = alloc(1)
        dma('sp', LN2G, ln2g_d.partition_broadcast(128), [], ['LN2G'])
        dma('sp', LN2B, ln2b_d.partition_broadcast(128), [], ['LN2B'])
        for n in range(NT):
            x1t = x1b[n % 2]
            YG = YG2[n % 2]
            dma('sp', x1t, x1_d[n * 128:(n + 1) * 128, :], [f'x1d{n}'], [f'x1b{n % 2}'])
            for k in range(4):
                op('pool', 'indirect_dma_start', [f'IDX{n}'], [f'YG{n % 2}_{k}'], dma=True,
                   out=YG[k], out_offset=None, in_=y_d[:, :],
                   in_offset=bass.IndirectOffsetOnAxis(ap=IDXT[:, n * 4 + k:n * 4 + k + 1], axis=0))
            ts('dve', accb, YG[0], GATES[:, n * 4:n * 4 + 1], ALU.mult, [f'YG{n % 2}_0', f'GATES{n}'], ['accb'])
            for k in range(1, 4):
                stt('dve', accb, YG[k], GATES[:, n * 4 + k:n * 4 + k + 1], accb, ALU.mult, ALU.add, [f'YG{n % 2}_{k}', f'GATES{n}', 'accb'], ['accb'])
            if "d_ff" in dbg_d:
                dma('sp', dbg_d["d_ff"][n * 128:(n + 1) * 128, :], accb, ['accb'], [])
            tt('dve', tmp, accb, GF1, ALU.mult, ['accb', 'modB'], ['tmp'])
            stt('dve', tmp, x1t, ALPHA, tmp, ALU.mult, ALU.add, [f'x1b{n % 2}', 'tmp'], ['tmp'])
            layer_norm('dve', tmp, LN2G, LN2B, ob[n % 2], ['tmp'], f'ob{n % 2}', 'LN2G', 'LN2B')
            dma('sp', out_d[n * 128:(n + 1) * 128, :], ob[n % 2], [f'ob{n % 2}'], [f'outd{n}'])

        sems = {}
        for e in ENGS:
            sems[('e', e)] = es.enter_context(nc.semaphore(f"se_{e}"))
        for k in S.dma_cnt:
            sems[k] = es.enter_context(nc.semaphore(f"sd_{k[1]}_{k[2]}"))
        final = S.all_tokens()
        blk = es.enter_context(nc.Block())
        bmap = {'pe': blk.tensor, 'act': blk.scalar, 'dve': blk.vector, 'pool': blk.gpsimd, 'sp': blk.sync}

        def make(eng):
            def body(e):
                for waits, fn, tok, inc in S.prog[eng]:
                    for k, v in waits:
                        e.wait_ge(sems[k], v)
                    inst = fn(e)
                    inst.then_inc(sems[tok[0]], inc)
                if eng == 'sp':
                    for k, v in final.items():
                        e.wait_ge(sems[k], v)
            return body
        for eng in ENGS:
            bmap[eng](make(eng))
    return nc


def host_consts():
    f32 = np.float32
    s = np.arange(128)[:, None]
    t = np.arange(128)[None, :]
    gam = (1.0 - 2.0 ** (-5.0 - np.arange(4))).astype(np.float64)
    dtab = np.zeros((128, 4, 128), f32)
    qd = np.zeros((128, 4, 128), f32)
    chtab = np.zeros((128, 4, 128), f32)
    kd = np.zeros((128, 4), f32)
    for h in range(4):
        diff = (t - s)
        dtab[:, h, :] = np.where(diff >= 0, gam[h] ** np.maximum(diff, 0), 0.0) / np.sqrt(128.0)
        qd[:, h, :] = (gam[h] ** (np.arange(128) + 1.0))[None, :]
        chtab[:, h, :] = gam[h] ** 128.0
        kd[:, h] = gam[h] ** (127.0 - np.arange(128)) / np.sqrt(128.0)
    inv = 1.0 / (10000.0 ** (np.arange(0, 128, 2, dtype=np.float32) / 128.0))
    ang = np.arange(L, dtype=np.float32)[:, None] * inv[None, :].astype(np.float32)
    cos = np.cos(ang).astype(f32)
    sin = np.sin(ang).astype(f32)
    negd = np.where(t > s, -1e30, 0.0).astype(f32)
    negd = np.where(np.arange(128)[None, :] > np.arange(128)[:, None], -1e30, 0.0).astype(f32)
    ut = (np.arange(128)[:, None] < np.arange(128)[None, :]).astype(f32)
    pow2 = np.tile((2.0 ** -np.arange(NIT + 2)).astype(f32)[None, :], (128, 1))
    ec = np.tile((np.arange(NE) * CAP).astype(f32)[None, :], (128, 1))
    ident = np.eye(128, dtype=f32)
    dist0 = t - s
    dist1 = t - s + 128
    b0 = t5_bucket_np(dist0)
    b1 = t5_bucket_np(dist1)
    return dict(dtab=dtab.reshape(128, 512), qdtab=qd.reshape(128, 512), chtab=chtab.reshape(128, 512), kdtab=kd,
                cos=cos, sin=sin, negd=negd, ut=ut, pow2=pow2, ec=ec, ident=ident), b0, b1, (dist0 >= 0)


_PERM = None


def _perm():
    idx = []
    for hh in [0, 4, 1, 5, 2, 6, 3, 7]:
        idx += list(range(hh * 64, hh * 64 + 64))
    idx += list(range(512, 640))
    idx += list(range(768, 1024))
    idx += list(range(1024, 1088)) * 2
    idx += list(range(640, 768))
    idx += list(range(1088, 1092))
    idx += list(range(1092, 2116))
    idx += list(range(2116, 2628))
    idx += list(range(2628, 3140))
    assert len(idx) == NCOL
    return np.array(idx)


def make_in_maps(inputs, cores):
    f32 = np.float32
    consts, b0, b1, causal = host_consts()
    rel_bias = np.asarray(inputs["rel_bias"], f32)
    bt = np.zeros((128, 2, 8, 128), f32)
    g0 = rel_bias[b0]
    g1 = rel_bias[b1]
    bt[:, 0] = np.where(causal[:, None, :], np.transpose(g0, (0, 2, 1)), 0.0)
    bt[:, 1] = np.transpose(g1, (0, 2, 1))
    c31 = np.tile(rel_bias[31][None, :], (128, 1)).astype(f32)
    w_in = np.ascontiguousarray(np.asarray(inputs["w_in"][0], f32)[:, _perm()])
    bgu = np.ascontiguousarray(np.asarray(inputs["b_gate_up"][0], f32).reshape(NE, 16, 128).transpose(2, 0, 1)).reshape(128, NE * 16)
    shared = dict(
        w_ada=np.ascontiguousarray(inputs["w_ada"][0], dtype=f32), b_ada=np.ascontiguousarray(inputs["b_ada"], dtype=f32).reshape(1, -1),
        w_in=w_in, w_out=np.ascontiguousarray(inputs["w_out"][0], dtype=f32),
        bt=bt.reshape(128, 2048), c31=c31,
        rng=np.asarray(inputs["ret_norm_g"], f32).reshape(1, 512),
        ln1g=np.asarray(inputs["ln1_g"], f32).reshape(1, D), ln1b=np.asarray(inputs["ln1_b"], f32).reshape(1, D),
        ln2g=np.asarray(inputs["ln2_g"], f32).reshape(1, D), ln2b=np.asarray(inputs["ln2_b"], f32).reshape(1, D),
        wr=np.ascontiguousarray(inputs["w_router"][0], dtype=f32), br=np.asarray(inputs["b_router"], f32).reshape(1, NE),
        wgu=np.ascontiguousarray(inputs["w_gate_up"][0], dtype=f32), bgu=bgu,
        wd=np.ascontiguousarray(inputs["w_down"][0], dtype=f32), bd=np.ascontiguousarray(inputs["b_down"][0], dtype=f32),
    )
    shared.update(consts)
    maps = []
    for b in cores:
        m = dict(shared)
        m["x"] = np.ascontiguousarray(inputs["x"][b], dtype=f32)
        m["c8"] = np.ascontiguousarray(np.asarray(inputs["c"][b], f32).reshape(8, 128).T)
        maps.append(m)
    return maps


def kernel(**inputs):
    nc = build_nc()
    maps = make_in_maps(inputs, list(range(8)))
    res = run_bass_kernel_spmd(nc, maps, core_ids=list(range(8)))
    out = np.stack([np.asarray(r["out"], np.float32) for r in res.results], axis=0)
    return out
```

```python
import os
import contextlib
import numpy as np
import ml_dtypes
import concourse.bass as bass
import concourse.mybir as mybir
from concourse.bass_utils import run_bass_kernel_spmd

F32 = mybir.dt.float32
BF16 = mybir.dt.bfloat16
I32 = mybir.dt.int32
AF = mybir.ActivationFunctionType
ALU = mybir.AluOpType
AX = mybir.AxisListType

L = 4096
D = 1024
NT = 32
NCOL = 3204
NE = 32
CAP = 896
NSLOT = CAP // 128
NIT = 18
ALPHA = float(2.0 ** 0.25)
EPS = 1e-5
ENGS = ['pe', 'act', 'dve', 'pool', 'sp']
NDMA = 8


class Sched:
    def __init__(self):
        self.prog = {e: [] for e in ENGS}
        self.seq = {e: 0 for e in ENGS}
        self.res = {}
        self.waited = {e: {} for e in ENGS}
        self.dma_rr = {e: 0 for e in ENGS}
        self.dma_cnt = {}
        self.pending = {e: {} for e in ENGS}
        self.enabled = True

    def add(self, eng, fn, r=(), w=(), dma=False, grp=None):
        if not self.enabled:
            return None
        needs = dict(self.pending[eng])
        self.pending[eng] = {}

        def need(tok):
            if tok is None:
                return
            k, v = tok
            if needs.get(k, 0) < v:
                needs[k] = v
        for key in r:
            st = self.res.get(key)
            if st:
                need(st[0])
        for key in w:
            st = self.res.get(key)
            if st:
                need(st[0])
                for k, v in st[1].items():
                    need((k, v))
        if dma:
            gname = grp or eng
            j = self.dma_rr.get(gname, 0)
            self.dma_rr[gname] = (j + 1) % NDMA
            semk = ('d', gname, j)
            prev = self.dma_cnt.get(semk, 0)
            if prev:
                need((semk, 16 * prev))
            self.dma_cnt[semk] = prev + 1
            tok = (semk, 16 * (prev + 1))
            inc = 16
        else:
            self.seq[eng] += 1
            tok = (('e', eng), self.seq[eng])
            inc = 1
        waits = []
        for k, v in needs.items():
            if k == ('e', 'pe') and eng == 'pe' and not dma:
                continue
            if self.waited[eng].get(k, 0) >= v:
                continue
            self.waited[eng][k] = v
            waits.append((k, v))
        self.prog[eng].append((waits, fn, tok, inc))
        for key in w:
            self.res[key] = [tok, {}]
        for key in r:
            if key in w:
                continue
            st = self.res.setdefault(key, [None, {}])
            if st[1].get(tok[0], 0) < tok[1]:
                st[1][tok[0]] = tok[1]
        return tok

    def cut(self, name):
        if os.environ.get('KCUT', '') == name:
            self.enabled = False

    def all_tokens(self):
        toks = {}
        for e in ENGS:
            if self.seq[e]:
                toks[('e', e)] = self.seq[e]
        for k, c in self.dma_cnt.items():
            toks[k] = 16 * c
        return toks

    def barrier(self, exclude=()):
        toks = self.all_tokens()
        for e in ENGS:
            for k, v in toks.items():
                if k[0] == 'd' and k[1] in exclude:
                    continue
                if self.pending[e].get(k, 0) < v:
                    self.pending[e][k] = v


def t5_bucket_np(n):
    n = np.maximum(n, 0)
    nf = np.maximum(n, 1).astype(np.float32)
    large = 16 + (np.log(nf / np.float32(16)) / np.float32(np.log(128 / 16)) * np.float32(16)).astype(np.int32)
    large = np.minimum(large, 31)
    return np.where(n < 16, n, large)


def build_nc(dbg=(), stage='D'):
    nc = bass.Bass("TRN2", target_bir_lowering=False)
    S = Sched()

    def din(name, shape, dt=F32):
        return nc.dram_tensor(name, list(shape), dt, kind="ExternalInput").ap()

    x_d = din("x", [L, D])
    c8_d = din("c8", [128, 8])
    wada_d = din("w_ada", [D, 6 * D])
    bada_d = din("b_ada", [1, 6 * D])
    win_d = din("w_in", [D, NCOL])
    wout_d = din("w_out", [D, D])
    bt_d = din("bt", [128, 2048])
    c31_d = din("c31", [128, 8])
    cos_d = din("cos", [L, 64])
    sin_d = din("sin", [L, 64])
    dt_d = din("dtab", [128, 512])
    qd_d = din("qdtab", [128, 512])
    ch_d = din("chtab", [128, 512])
    kd_d = din("kdtab", [128, 4])
    rng_d = din("rng", [1, 512])
    ln1g_d = din("ln1g", [1, D])
    ln1b_d = din("ln1b", [1, D])
    ln2g_d = din("ln2g", [1, D])
    ln2b_d = din("ln2b", [1, D])
    wr_d = din("wr", [D, NE])
    br_d = din("br", [1, NE])
    wgu_d = din("wgu", [NE, D, 2 * D])
    bgu_d = din("bgu", [128, NE * 16])
    wd_d = din("wd", [NE, D, D])
    bd_d = din("bd", [NE, D])
    negd_d = din("negd", [128, 128])
    ut_d = din("ut", [128, 128])
    pow_d = din("pow2", [128, NIT + 2])
    ec_d = din("ec", [128, NE])
    id_d = din("ident", [128, 128])
    out_d = nc.dram_tensor("out", [L, D], F32, kind="ExternalOutput").ap()
    dbg_d = {}
    for name, shape in dbg:
        dbg_d[name] = nc.dram_tensor(name, list(shape), F32, kind="ExternalOutput").ap()

    qs_d = nc.dram_tensor("qs_scr", [NT, 128, 768], BF16, kind="Internal").ap()
    ret_d = nc.dram_tensor("ret_scr", [L, 512], BF16, kind="Internal").ap()
    x1_d = nc.dram_tensor("x1_scr", [L, D], F32, kind="Internal").ap()
    xg_d = nc.dram_tensor("xg_scr", [NE * CAP, D], BF16, kind="Internal").ap()
    y_d = nc.dram_tensor("y_scr", [NE * CAP, D], F32, kind="Internal").ap()

    with contextlib.ExitStack() as es:
        ARENA_W = 53000
        arena = es.enter_context(nc.sbuf_tensor("arena", [128, ARENA_W], F32))
        PS = [es.enter_context(nc.psum_tensor(f"ps{i}", [128, 1024], F32)) for i in range(4)]
        off = [0]

        def alloc(n, dt=F32):
            n32 = n if dt in (F32, I32) else (n + 1) // 2
            assert off[0] + n32 <= ARENA_W, (off[0], n32)
            a = arena[:, off[0]:off[0] + n32]
            off[0] += n32
            if dt == F32:
                return a
            return a.bitcast(dt)

        def bank(b):
            return PS[b // 2][:, (b % 2) * 512:(b % 2) * 512 + 512]

        def bankb(b):
            return bank(b).bitcast(BF16)

        def pk(b):
            return f"ps{b}"

        def mm(out, lhsT, rhs, start, stop, r, w, skip=False):
            if skip:
                S.add('pe', lambda e: e.matmul(out, lhsT=lhsT, rhs=rhs, start=start, stop=stop, skip_group_check=True), r, w)
            else:
                S.add('pe', lambda e: e.matmul(out, lhsT=lhsT, rhs=rhs, start=start, stop=stop), r, w)

        def tp(out, in_, idn, r, w):
            S.add('pe', lambda e: e.transpose(out=out, in_=in_, identity=idn), r, w)

        def dma(eng, out, in_, r, w):
            return S.add(eng, lambda e: e.dma_start(out=out, in_=in_), r, w, dma=True)

        def act(out, in_, func, r, w, bias=0.0, scale=1.0, accum=None):
            if accum is None:
                S.add('act', lambda e: e.activation(out=out, in_=in_, func=func, bias=bias, scale=scale), r, w)
            else:
                S.add('act', lambda e: e.activation(out=out, in_=in_, func=func, bias=bias, scale=scale, accum_out=accum), r, w)

        def cp(eng, out, in_, r, w):
            if eng == 'act':
                S.add('act', lambda e: e.copy(out=out, in_=in_), r, w)
            else:
                S.add(eng, lambda e: e.tensor_copy(out=out, in_=in_), r, w)

        def tt(eng, out, in0, in1, op, r, w):
            S.add(eng, lambda e: e.tensor_tensor(out=out, in0=in0, in1=in1, op=op), r, w)

        def ts(eng, out, in0, s1, op0, r, w, s2=None, op1=None, accum=None):
            if op1 is None:
                S.add(eng, lambda e: e.tensor_scalar(out=out, in0=in0, scalar1=s1, scalar2=None, op0=op0), r, w)
            elif accum is None:
                S.add(eng, lambda e: e.tensor_scalar(out=out, in0=in0, scalar1=s1, scalar2=s2, op0=op0, op1=op1), r, w)
            else:
                S.add(eng, lambda e: e.tensor_scalar(out=out, in0=in0, scalar1=s1, scalar2=s2, op0=op0, op1=op1, accum_out=accum), r, w)

        def stt(eng, out, in0, scalar, in1, op0, op1, r, w, accum=None):
            if accum is None:
                S.add(eng, lambda e: e.scalar_tensor_tensor(out=out, in0=in0, scalar=scalar, in1=in1, op0=op0, op1=op1), r, w)
            else:
                S.add(eng, lambda e: e.scalar_tensor_tensor(out=out, in0=in0, scalar=scalar, in1=in1, op0=op0, op1=op1, accum_out=accum), r, w)

        def op(eng, name, r, w, dma=False, **kw):
            return S.add(eng, lambda e: getattr(e, name)(**kw), r, w, dma=dma)

        def memset(eng, out, val, r, w):
            S.add(eng, lambda e: e.memset(out, val), r, w)

        def dbg_out(name, src, r, rows=None):
            if name in dbg_d:
                dst = dbg_d[name] if rows is None else dbg_d[name][rows[0]:rows[1], :]
                dma('sp', dst, src, r, [])

        modB = alloc(4096)
        GA1 = modB[:, 0:1024]
        SHF = modB[:, 1024:2048]
        SCF1 = modB[:, 2048:3072]
        GF1 = modB[:, 3072:4096]
        ident = alloc(128)
        identb = alloc(128, BF16)
        SHA = alloc(8)
        SCA1 = alloc(8)
        WI = alloc(NT * 4)
        GATES = alloc(NT * 4)
        IDXT = alloc(NT * 4, I32)
        carry = alloc(NE)
        ZT = alloc(4 * 1024, BF16)
        persist_small = off[0]
        KT = alloc(L, BF16)
        KIT = alloc(L, BF16)
        V1 = alloc(NT * 2 * 65 + 1, BF16)[:, 0:NT * 2 * 65]
        V1v = V1.rearrange("p (n g d) -> p n g d", g=2, d=65)
        wout = alloc(8 * 1024, BF16)
        woutv = wout.rearrange("p (kc n) -> p kc n", n=1024)
        wrb = alloc(8 * NE, BF16)
        wrv = wrb.rearrange("p (kc n) -> p kc n", n=NE)
        UT = alloc(128, BF16)
        persist_off = off[0]

        dma('sp', ident, id_d[:, :], [], ['ident'])
        cp('dve', identb, ident, ['ident'], ['identb'])
        memset('pool', V1, 1.0, [], ['V1'])
        memset('pool', carry, 0.0, [], ['carry'])

        c8 = alloc(8)
        csil = alloc(8)
        ones = alloc(128)
        cB = alloc(8 * 128)
        modA = alloc(2048)
        wab = [alloc(8 * 512), alloc(8 * 512)]
        badab = [alloc(512), alloc(512)]
        dma('sp', c8, c8_d[:, :], [], ['c8'])
        act(csil, c8, AF.Silu, ['c8'], ['csil'])
        memset('dve', ones, 1.0, [], ['ones'])
        for kc in range(8):
            ts('dve', cB[:, kc * 128:(kc + 1) * 128], ones, csil[:, kc:kc + 1], ALU.mult, ['ones', 'csil'], ['cB'])
        wada_v = wada_d.rearrange("(kc p) n -> p kc n", p=128)
        for ch in range(12):
            wb = wab[ch % 2]
            bb = badab[ch % 2]
            wbv = wb.rearrange("p (kc n) -> p kc n", n=512)
            dma('sp', wbv, wada_v[:, :, ch * 512:(ch + 1) * 512], [], [f'wab{ch % 2}'])
            dma('sp', bb, bada_d[:, ch * 512:(ch + 1) * 512].partition_broadcast(128), [], [f'badab{ch % 2}'])
            b = ch % 2
            for kc in range(8):
                mm(bank(b), cB[:, kc * 128:(kc + 1) * 128], wbv[:, kc, :], kc == 0, kc == 7,
                   ['cB', f'wab{ch % 2}'], [pk(b)])
            if ch < 4:
                dst = modA[:, ch * 512:(ch + 1) * 512]
                dkey = 'modA'
            else:
                dst = modB[:, (ch - 4) * 512:(ch - 3) * 512]
                dkey = 'modB'
            tt('dve', dst, bank(b), bb, ALU.add, [pk(b), f'badab{ch % 2}'], [dkey])
        ts('dve', modA[:, 1024:2048], modA[:, 1024:2048], 1.0, ALU.add, ['modA'], ['modA'])
        ts('dve', GA1, GA1, 1.0, ALU.add, ['modB'], ['modB'])
        ts('dve', SCF1, SCF1, 1.0, ALU.add, ['modB'], ['modB'])
        ts('dve', GF1, GF1, 1.0, ALU.add, ['modB'], ['modB'])
        for c in range(8):
            tp(bank(2 + (c % 2))[:, 0:128], modA[:, c * 128:(c + 1) * 128], ident, ['modA', 'ident'], [pk(2 + (c % 2))])
            cp('dve', SHA[:, c:c + 1], bank(2 + (c % 2))[:, 0:1], [pk(2 + (c % 2))], ['SHA'])
        for c in range(8):
            tp(bank(2 + (c % 2))[:, 0:128], modA[:, 1024 + c * 128:1024 + (c + 1) * 128], ident, ['modA', 'ident'], [pk(2 + (c % 2))])
            cp('dve', SCA1[:, c:c + 1], bank(2 + (c % 2))[:, 0:1], [pk(2 + (c % 2))], ['SCA1'])
        dbg_out("d_modB", modB, ['modB'])
        S.barrier(exclude=('zf',))
        off[0] = persist_off

        if stage == 'A':
            S.enabled = False
        win = alloc(8 * NCOL, BF16)
        winv = win.rearrange("p (kc n) -> p kc n", n=NCOL)
        xtb = [alloc(1024), alloc(1024)]
        csb = [alloc(64), alloc(64)]
        snb = [alloc(64), alloc(64)]
        hT = alloc(1024, BF16)
        tmA = alloc(512, BF16)
        tmB = alloc(512, BF16)
        QS = alloc(768, BF16)
        t1 = alloc(256)
        t2 = alloc(256)
        t3 = alloc(256)
        t4 = alloc(256)
        qkrot_b = [alloc(1024, BF16), alloc(1024, BF16)]
        qkT = alloc(1024, BF16)
        qdT = alloc(512, BF16)
        kdk = alloc(512, BF16)
        ATb = alloc(512, BF16)
        VR_b = [alloc(512, BF16), alloc(512, BF16)]
        SG_b = [alloc(512), alloc(512)]
        S32 = alloc(512)
        S16 = alloc(512, BF16)
        DTt = alloc(512)
        QDt = alloc(512)
        CHt = alloc(512)
        KDt = alloc(4)
        RNG = alloc(512)
        rn = alloc(512)
        retb = alloc(512, BF16)
        bst = alloc(24)
        bmv = alloc(8)
        sd4 = alloc(4)
        rs4 = alloc(4)

        win_v = win_d.rearrange("(kc p) n -> p kc n", p=128)
        for kc in range(8):
            dma('pool', winv[:, kc, :], win_v[:, kc, :], [], [f'win{kc}'])
        for kc in range(8):
            dma('pool', woutv[:, kc, :], wout_d[kc * 128:(kc + 1) * 128, :], [], [f'wout{kc}'])
        dma('pool', wrv, wr_d.rearrange("(kc p) n -> p kc n", p=128), [], ['wrb'])
        dma('pool', UT, ut_d[:, :], [], ['UT'])
        memset('dve', ZT, 0.0, [], ['ZT'])
        ZKEYS = []
        for zi in range(NE * CAP // 512):
            ZKEYS.append(f'xgz{zi}')
            S.add('pool', (lambda z: (lambda e: e.dma_start(out=xg_d[z * 512:(z + 1) * 512, :].rearrange("(a p) f -> p a f", p=128),
                                                            in_=ZT.rearrange("p (a f) -> p a f", f=1024))))(zi),
                  ['ZT'], [f'xgz{zi}'], dma=True, grp='zf')
        dma('sp', DTt, dt_d[:, :], [], ['DTt'])
        dma('sp', QDt, qd_d[:, :], [], ['QDt'])
        dma('sp', CHt, ch_d[:, :], [], ['CHt'])
        dma('sp', KDt, kd_d[:, :], [], ['KDt'])
        dma('sp', RNG, rng_d.partition_broadcast(128), [], ['RNG'])

        S.cut('pro')
        CH = [(0, 512), (512, 512), (1024, 132), (1156, 512), (1668, 512), (2180, 512), (2692, 512)]

        def proj_chunk(ci, b):
            c0, wd = CH[ci]
            for kc in range(8):
                mm(bank(b)[:, 0:wd], hT[:, kc * 128:(kc + 1) * 128], winv[:, kc, c0:c0 + wd], kc == 0, kc == 7,
                   ['hT', f'win{kc}'], [pk(b)])

        def b1_proj(n):
            xt = xtb[n % 2]
            xk = f'xt{n % 2}'
            qkrot, VR, SG = qkrot_b[n % 2], VR_b[n % 2], SG_b[n % 2]
            cs = csb[n % 2]
            sn = snb[n % 2]
            dma('sp', xt, x_d[n * 128:(n + 1) * 128, :], [], [xk])
            dma('sp', cs, cos_d[n * 128:(n + 1) * 128, :], [], [f'cs{n % 2}'])
            dma('sp', sn, sin_d[n * 128:(n + 1) * 128, :], [], [f'sn{n % 2}'])
            for c in range(8):
                b = 0 if c < 4 else 1
                tp(bank(b)[:, (c % 4) * 128:(c % 4 + 1) * 128], xt[:, c * 128:(c + 1) * 128], ident, [xk, 'ident'], [pk(b)])
            for c in range(8):
                b = 0 if c < 4 else 1
                ts('dve', hT[:, c * 128:(c + 1) * 128], bank(b)[:, (c % 4) * 128:(c % 4 + 1) * 128], SCA1[:, c:c + 1], ALU.mult,
                   [pk(b), 'SHA', 'SCA1'], ['hT'], s2=SHA[:, c:c + 1], op1=ALU.add)
            yield
            proj_chunk(0, 2)
            cp('dve', tmA, bank(2), [pk(2)], ['tmA'])
            yield
            proj_chunk(1, 3)
            cp('act', tmB, bank(3), [pk(3)], ['tmB'])
            yield
            for c in range(4):
                tp(bankb(4)[:, c * 128:(c + 1) * 128], tmA[:, c * 128:(c + 1) * 128], identb, ['tmA', 'identb'], [pk(4)])
            for c in range(4):
                tp(bankb(4)[:, 512 + c * 128:512 + (c + 1) * 128], tmB[:, c * 128:(c + 1) * 128], identb, ['tmB', 'identb'], [pk(4)])
            cp('dve', QS[:, 0:512], bankb(4)[:, 0:512], [pk(4)], ['QS'])
            cp('dve', QS[:, 512:768], bankb(4)[:, 640:896], [pk(4)], ['QS'])
            cp('dve', KT[:, n * 128:(n + 1) * 128], bankb(4)[:, 512:640], [pk(4)], [f'KT{n}'])
            cp('dve', KIT[:, n * 128:(n + 1) * 128], bankb(4)[:, 896:1024], [pk(4)], [f'KIT{n}'])
            dma('sp', qs_d[n, :, :], QS, ['QS'], [f'qsd{n}'])
            yield
            proj_chunk(2, 2)
            cp('dve', V1v[:, n, :, 0:64], bank(2)[:, 0:128].rearrange("p (g d) -> p g d", d=64), [pk(2), 'V1'], [f'V1_{n}'])
            ts('dve', WI[:, n * 4:(n + 1) * 4], bank(2)[:, 128:132], 0.0625, ALU.mult, [pk(2)], [f'WI{n}'])
            yield
            proj_chunk(3, 3)
            yield
            proj_chunk(4, 2)
            cosB = cs.unsqueeze(1).to_broadcast([128, 4, 64])
            sinB = sn.unsqueeze(1).to_broadcast([128, 4, 64])
            qkv = qkrot.rearrange("p (a two d) -> p a two d", two=2, d=64)
            for which, b in ((0, 3), (1, 2)):
                pv = bank(b).rearrange("p (h two d) -> p h two d", two=2, d=64)
                x1v = pv[:, :, 0, :]
                x2v = pv[:, :, 1, :]
                t1v = t1.rearrange("p (h d) -> p h d", d=64)
                t2v = t2.rearrange("p (h d) -> p h d", d=64)
                t3v = t3.rearrange("p (h d) -> p h d", d=64)
                t4v = t4.rearrange("p (h d) -> p h d", d=64)
                ck = [f'cs{n % 2}', f'sn{n % 2}']
                tt('dve', t1v, x1v, cosB, ALU.mult, [pk(b)] + ck, ['t1'])
                tt('dve', t2v, x2v, sinB, ALU.mult, [pk(b)] + ck, ['t2'])
                tt('dve', t3v, x1v, sinB, ALU.mult, [pk(b)] + ck, ['t3'])
                tt('dve', t4v, x2v, cosB, ALU.mult, [pk(b)] + ck, ['t4'])
                tt('dve', qkv[:, which * 4:(which + 1) * 4, 0, :], t1v, t2v, ALU.subtract, ['t1', 't2'], ['qkrot' + str(n % 2)])
                tt('dve', qkv[:, which * 4:(which + 1) * 4, 1, :], t3v, t4v, ALU.add, ['t3', 't4'], ['qkrot' + str(n % 2)])
            yield
            proj_chunk(5, 3)
            cp('act', VR, bank(3), [pk(3)], ['VR' + str(n % 2)])
            yield
            proj_chunk(6, 2)
            act(SG, bank(2), AF.Silu, [pk(2)], ['SG' + str(n % 2)])
            yield

        def b1_ret(n):
            qkrot, VR, SG = qkrot_b[n % 2], VR_b[n % 2], SG_b[n % 2]
            for a in range(8):
                tp(bankb(4)[:, a * 128:(a + 1) * 128], qkrot[:, a * 128:(a + 1) * 128], identb, ['qkrot' + str(n % 2), 'identb'], [pk(4)])
            cp('dve', qkT, bankb(4), [pk(4)], ['qkT'])
            tt('dve', qdT, bankb(4)[:, 0:512], QDt, ALU.mult, [pk(4), 'QDt'], ['qdT'])
            tt('dve', kdk.rearrange("p (h d) -> p h d", d=128), qkrot[:, 512:1024].rearrange("p (h d) -> p h d", d=128),
               KDt.unsqueeze(2).to_broadcast([128, 4, 128]), ALU.mult, ['qkrot' + str(n % 2), 'KDt'], ['kdk'])
            yield
            for h in range(4):
                mm(bank(5)[:, h * 128:(h + 1) * 128], qkT[:, 512 + h * 128:512 + (h + 1) * 128], qkT[:, h * 128:(h + 1) * 128],
                   True, True, ['qkT'], [pk(5)])
            tt('dve', ATb, bank(5), DTt, ALU.mult, [pk(5), 'DTt'], ['ATb'])
            for h in range(4):
                hs = slice(h * 128, (h + 1) * 128)
                mm(bank(6)[:, hs], ATb[:, hs], VR[:, hs], True, n == 0, ['ATb', 'VR' + str(n % 2)], [pk(6)])
                if n > 0:
                    mm(bank(6)[:, hs], qdT[:, hs], S16[:, hs], False, True, ['qdT', 'S16'], [pk(6)])
            yield
            yield
            for h in range(4):
                hs = slice(h * 128, (h + 1) * 128)
                mm(bank(7)[:, hs], kdk[:, hs], VR[:, hs], True, True, ['kdk', 'VR' + str(n % 2)], [pk(7)])
            if n == 0:
                cp('dve', S32, bank(7), [pk(7)], ['S32'])
            else:
                tt('dve', S32, S32, CHt, ALU.mult, ['S32', 'CHt'], ['S32'])
                tt('dve', S32, S32, bank(7), ALU.add, ['S32', pk(7)], ['S32'])
            if n < NT - 1:
                cp('act', S16, S32, ['S32'], ['S16'])
            yield
            for h in range(4):
                op('dve', 'bn_stats', [pk(6)], ['bst'], out=bst[:, h * 6:(h + 1) * 6], in_=bank(6)[:, h * 128:(h + 1) * 128])
            for h in range(4):
                op('dve', 'bn_aggr', ['bst'], ['bmv'], out=bmv[:, h * 2:(h + 1) * 2], in_=bst[:, h * 6:(h + 1) * 6])
            yield
            bmvv = bmv.rearrange("p (h two) -> p h two", two=2)
            ts('dve', sd4, bmvv[:, :, 1], EPS, ALU.add, ['bmv'], ['sd4'])
            act(sd4, sd4, AF.Sqrt, ['sd4'], ['sd4'])
            op('dve', 'reciprocal', ['sd4'], ['rs4'], out=rs4, in_=sd4)
            for h in range(4):
                hs = slice(h * 128, (h + 1) * 128)
                ts('dve', rn[:, hs], bank(6)[:, hs], bmv[:, 2 * h:2 * h + 1], ALU.subtract, [pk(6), 'bmv', 'rs4'], ['rn'],
                   s2=rs4[:, h:h + 1], op1=ALU.mult)
            yield
            tt('dve', rn, rn, RNG, ALU.mult, ['rn', 'RNG'], ['rn'])
            tt('dve', retb, rn, SG, ALU.mult, ['rn', 'SG' + str(n % 2)], ['retb'])
            dma('sp', ret_d[n * 128:(n + 1) * 128, :], retb, ['retb'], [f'retd{n}'])
            yield

        def run_gen(g):
            for _ in g:
                pass

        def interleave2(ga, gb):
            da = db = False
            while not (da and db):
                if not da:
                    try:
                        next(ga)
                    except StopIteration:
                        da = True
                if not db:
                    try:
                        next(gb)
                    except StopIteration:
                        db = True

        run_gen(b1_proj(0))
        for n in range(NT):
            interleave2(b1_ret(n), b1_proj(n + 1) if n + 1 < NT else iter(()))
        S.barrier(exclude=('zf',))
        off[0] = persist_off

        if stage == 'B1':
            S.enabled = False
        scoreb = [alloc(L), alloc(L)]
        penb = [alloc(L, BF16), alloc(L, BF16), alloc(L, BF16)]
        junk_b = [alloc(L, BF16), alloc(L, BF16)]
        xtb = [alloc(1024), alloc(1024)]
        QSb = [alloc(768, BF16), alloc(768, BF16), alloc(768, BF16)]
        rb = [alloc(512), alloc(512)]
        pTb = [alloc(1024, BF16), alloc(1024, BF16)]
        BT8 = alloc(2048, BF16)
        IREP = alloc(512, BF16)
        nrmin_b = [alloc(1), alloc(1)]
        hw__b = [alloc(1), alloc(1)]
        C31 = alloc(8)
        NEGD = alloc(128)
        POW = alloc(NIT + 2)
        W2_b = [alloc(NIT + 2), alloc(NIT + 2)]
        W2h_b = [alloc(NIT + 2), alloc(NIT + 2)]
        mid_b = [alloc(NIT + 2), alloc(NIT + 2)]
        cnt_b = [alloc(NIT + 2), alloc(NIT + 2)]
        uu_b = [alloc(NIT + 2), alloc(NIT + 2)]
        rmax_b = [alloc(1), alloc(1)]
        rmin_b = [alloc(1), alloc(1)]
        rrng_b = [alloc(1), alloc(1)]
        thr_b = [alloc(1), alloc(1)]
        rec8 = alloc(8)
        cat = alloc(1024, BF16)
        catT = alloc(1024, BF16)
        tmp_off = off[0]
        tmp = alloc(1024)
        yv = alloc(1024)
        BTt = arena[:, tmp_off:tmp_off + 2048]
        x1 = alloc(1024)
        LN1G = alloc(1024)
        LN1B = alloc(1024)
        h2 = alloc(1024, BF16)
        h2T = alloc(1024, BF16)
        BR = alloc(NE)
        ONEb = alloc(128, BF16)
        EC = alloc(NE)
        lg = alloc(NE)
        mx8 = alloc(8)
        nm = alloc(1)
        ex4 = alloc(4)
        sm = alloc(1)
        rsm = alloc(1)
        selb = alloc(NE, BF16)
        pos = alloc(NE)
        flat = alloc(NE)
        ohj = alloc(NE)
        idxf = alloc(4)
        lst = alloc(12)
        lmv = alloc(2)
        lsd = alloc(1)
        lrs = alloc(1)

        memset('dve', ONEb, 1.0, [], ['ONEb'])
        dma('sp', BTt, bt_d[:, :], [], ['BTt', 'tmp', 'yv'])
        dma('sp', C31, c31_d[:, :], [], ['C31'])
        dma('sp', NEGD, negd_d[:, :], [], ['NEGD'])
        dma('sp', POW, pow_d[:, :], [], ['POW'])
        dma('sp', EC, ec_d[:, :], [], ['EC'])
        dma('sp', LN1G, ln1g_d.partition_broadcast(128), [], ['LN1G'])
        dma('sp', LN1B, ln1b_d.partition_broadcast(128), [], ['LN1B'])
        dma('sp', BR, br_d.partition_broadcast(128), [], ['BR'])
        BTv = BTt.rearrange("p (k h t) -> p k h t", k=2, h=8)
        for k in range(2):
            tt('dve', BTv[:, k, :, :], BTv[:, k, :, :], C31.unsqueeze(2).to_broadcast([128, 8, 128]), ALU.subtract,
               ['BTt', 'C31'], ['BTt'])

        ts('dve', BT8, BTt, 8.0, ALU.mult, ['BTt', 'tmp', 'yv'], ['BT8'])
        for c in range(4):
            cp('dve', IREP[:, c * 128:(c + 1) * 128], identb, ['identb'], ['IREP'])

        def layer_norm(eng_src, src, gtab, btab, dst, rkeys, wkey, gk, bk):
            op('dve', 'bn_stats', rkeys, ['lst'], out=lst[:, 0:6], in_=src[:, 0:512])
            op('dve', 'bn_stats', rkeys, ['lst'], out=lst[:, 6:12], in_=src[:, 512:1024])
            op('dve', 'bn_aggr', ['lst'], ['lmv'], out=lmv, in_=lst.rearrange("p (a s) -> p a s", s=6))
            ts('dve', lsd, lmv[:, 1:2], EPS, ALU.add, ['lmv'], ['lsd'])
            act(lsd, lsd, AF.Sqrt, ['lsd'], ['lsd'])
            op('dve', 'reciprocal', ['lsd'], ['lrs'], out=lrs, in_=lsd)
            stt('dve', lsd, lmv[:, 0:1], -1.0, lrs, ALU.mult, ALU.mult, ['lmv', 'lrs'], ['lsd'])
            act(src, src, AF.Identity, rkeys + ['lsd', 'lrs'], rkeys, bias=lsd[:, 0:1], scale=lrs[:, 0:1])
            tt('dve', src, src, gtab, ALU.mult, rkeys + [gk], rkeys)
            tt('dve', dst, src, btab, ALU.add, rkeys + [bk], [wkey])

        def stage_scores(n):
            Sn = (n + 1) * 128
            QSn = QSb[n % 3]
            qk = f'QSb{n % 3}'
            pen = penb[n % 3]
            pnk = f'pen{n % 3}'
            q2 = n % 2
            score = scoreb[q2]
            junk = junk_b[q2]
            W2, mid, cnt, uu = W2_b[q2], mid_b[q2], cnt_b[q2], uu_b[q2]
            rmax, rmin, rrng, thr, nrmin, hw_ = rmax_b[q2], rmin_b[q2], rrng_b[q2], thr_b[q2], nrmin_b[q2], hw__b[q2]
            dma('sp', QSn, qs_d[n, :, :], [f'qsd{n}'], [qk])
            QITc = QSn[:, 512:768]
            kit_keys = [f'KIT{j}' for j in range(n + 1)]
            nchunk = (Sn + 511) // 512
            it = 0
            for c in range(nchunk):
                wd = min(512, Sn - c * 512)
                for h in range(4):
                    b = it % 2
                    it += 1
                    half = slice((h % 2) * 64, (h % 2) * 64 + 64)
                    mm(bank(b)[:, 0:wd], QITc[half, (h // 2) * 128:(h // 2 + 1) * 128], KIT[half, c * 512:c * 512 + wd], True, True,
                       [qk] + kit_keys[c * 4:c * 4 + 4], [pk(b)])
                    act(rb[b][:, 0:wd], bank(b)[:, 0:wd], AF.Relu, [pk(b)], [f'rb{b}'])
                    if h == 0:
                        ts('dve', score[:, c * 512:c * 512 + wd], rb[b][:, 0:wd], WI[:, n * 4:n * 4 + 1], ALU.mult,
                           [f'rb{b}', f'WI{n}'], ['score' + str(q2)])
                    else:
                        stt('dve', score[:, c * 512:c * 512 + wd], rb[b][:, 0:wd], WI[:, n * 4 + h:n * 4 + h + 1],
                            score[:, c * 512:c * 512 + wd], ALU.mult, ALU.add, [f'rb{b}', f'WI{n}', 'score' + str(q2)], ['score' + str(q2)])
                yield
            if Sn <= 256:
                tt('dve', score[:, n * 128:(n + 1) * 128], score[:, n * 128:(n + 1) * 128], NEGD, ALU.add, ['score' + str(q2), 'NEGD'], ['score' + str(q2)])
                memset('dve', thr, -1e29, [], ['thr' + str(q2)])
                yield
            else:
                op('dve', 'tensor_reduce', ['score' + str(q2)], ['rmax' + str(q2)], out=rmax, in_=score[:, 0:Sn], axis=AX.X, op=ALU.max)
                op('dve', 'tensor_reduce', ['score' + str(q2)], ['rmin' + str(q2)], out=rmin, in_=score[:, 0:Sn], axis=AX.X, op=ALU.min)
                tt('dve', score[:, n * 128:(n + 1) * 128], score[:, n * 128:(n + 1) * 128], NEGD, ALU.add, ['score' + str(q2), 'NEGD'], ['score' + str(q2)])
                on_dve = (n % 2 == 1)
                if on_dve:
                    tt('dve', rrng, rmax, rmin, ALU.subtract, ['rmax' + str(q2), 'rmin' + str(q2)], ['rrng' + str(q2)])
                    ts('dve', nrmin, rmin, 1.0, ALU.mult, ['rmin' + str(q2)], ['nrmin' + str(q2)])
                else:
                    tt('dve', rrng, rmin, rmax, ALU.subtract, ['rmax' + str(q2), 'rmin' + str(q2)], ['rrng' + str(q2)])
                    ts('dve', nrmin, rmin, -1.0, ALU.mult, ['rmin' + str(q2)], ['nrmin' + str(q2)])
                ts('dve', W2, POW, rrng[:, 0:1], ALU.mult, ['POW', 'rrng' + str(q2)], ['W2' + str(q2)])
                stt('dve', mid[:, 0:1], W2[:, 1:2], 1.0, nrmin, ALU.mult, ALU.add, ['W2' + str(q2), 'nrmin' + str(q2)], ['mid' + str(q2)])
                W2h = W2h_b[q2]
                ts('dve', W2h, W2, 0.5, ALU.mult, ['W2' + str(q2)], ['W2' + str(q2)])
                memset('dve', cnt, 0.0, [], ['cnt' + str(q2)])
                yield
                for k in range(NIT):
                    if on_dve:
                        ts('dve', junk[:, 0:Sn], score[:, 0:Sn], mid[:, k:k + 1], ALU.is_ge, ['score' + str(q2), 'mid' + str(q2), 'cnt' + str(q2)],
                           ['junk' + str(q2), 'cnt' + str(q2)], s2=0.0, op1=ALU.add, accum=cnt[:, k:k + 1])
                        ts('dve', uu[:, k:k + 1], cnt[:, k:k + 1], 255.5, ALU.is_ge, ['cnt' + str(q2)], ['uu' + str(q2)], s2=0.5, op1=ALU.subtract)
                    else:
                        act(junk[:, 0:Sn], score[:, 0:Sn], AF.Sign, ['score' + str(q2), 'mid' + str(q2), 'cnt' + str(q2)], ['junk' + str(q2), 'cnt' + str(q2)], bias=mid[:, k:k + 1], scale=1.0,
                            accum=cnt[:, k:k + 1])
                        act(uu[:, k:k + 1], cnt[:, k:k + 1], AF.Sign, ['cnt' + str(q2)], ['uu' + str(q2)], bias=float(Sn - 510.5), scale=1.0)
                        act(mid[:, k + 1:k + 2], uu[:, k:k + 1], AF.Identity, ['uu' + str(q2), 'W2' + str(q2), 'mid' + str(q2)], ['mid' + str(q2)],
                            bias=mid[:, k:k + 1], scale=W2h[:, k + 1:k + 2])
                        yield
                        continue
                    stt('dve', mid[:, k + 1:k + 2], uu[:, k:k + 1], W2[:, k + 1:k + 2], mid[:, k:k + 1], ALU.mult, ALU.add,
                        ['uu' + str(q2), 'W2' + str(q2), 'mid' + str(q2)], ['mid' + str(q2)])
                    yield
                ts('dve', hw_, W2[:, NIT:NIT + 1], 0.5, ALU.mult, ['W2' + str(q2)], ['hw_' + str(q2)])
                if on_dve:
                    tt('dve', thr, mid[:, NIT:NIT + 1], hw_, ALU.subtract, ['hw_' + str(q2), 'mid' + str(q2)], ['thr' + str(q2)])
                else:
                    stt('dve', thr, mid[:, NIT:NIT + 1], -1.0, hw_, ALU.mult, ALU.add, ['hw_' + str(q2), 'mid' + str(q2)], ['thr' + str(q2)])
            ts('dve', pen[:, 0:Sn], score[:, 0:Sn], thr[:, 0:1], ALU.is_lt, ['score' + str(q2), 'thr' + str(q2)], [pnk], s2=-240000.0, op1=ALU.mult)
            yield

        def stage_attn(n):
            xt = xtb[n % 2]
            xk = f'xt{n % 2}'
            QSn = QSb[n % 3]
            qk = f'QSb{n % 3}'
            pen = penb[n % 3]
            pnk = f'pen{n % 3}'
            QTc = QSn[:, 0:512]
            for j in range(n + 1):
                jb = j % 2
                Lp = PS[1 + jb][:, :]
                lk = [pk(2 + 2 * jb), pk(3 + 2 * jb)]
                near = j >= n - 1
                kind = 0 if j == n else 1
                for g in range(2):
                    hp = slice(g * 64, g * 64 + 64)
                    og = Lp[:, g * 512:(g + 1) * 512]
                    mm(og, KT[hp, j * 128:(j + 1) * 128], QTc[hp, :], True, False, [qk, f'KT{j}'], lk)
                    mm(og, pen[:, j * 128:(j + 1) * 128], IREP, False, not near, [pnk, 'IREP'], lk)
                    if near:
                        mm(og, identb, BT8[:, kind * 1024 + g * 512:kind * 1024 + (g + 1) * 512], False, True, ['identb', 'BT8'], lk)
                pT = pTb[jb]
                pk_ = f'pT{jb}'
                act(pT, Lp, AF.Exp, lk, [pk_], scale=0.125)
                for h in range(8):
                    g = h // 4
                    ob = 6 + g
                    mm(bank(ob)[:, (h % 4) * 65:(h % 4) * 65 + 65], pT[:, h * 128:(h + 1) * 128], V1v[:, j, g, :],
                       j == 0 and h % 4 == 0, j == n, [pk_, f'V1_{j}', 'V1'], [pk(ob)], skip=True)
                yield
            for g in range(2):
                ov = bank(6 + g)[:, 0:260].rearrange("p (h d) -> p h d", d=65)
                op('dve', 'reciprocal', [pk(6 + g)], ['rec8'], out=rec8[:, g * 4:(g + 1) * 4], in_=ov[:, :, 64])
                tt('dve', cat[:, g * 256:(g + 1) * 256].rearrange("p (h d) -> p h d", d=64), ov[:, :, 0:64],
                   rec8[:, g * 4:(g + 1) * 4].unsqueeze(2).to_broadcast([128, 4, 64]), ALU.mult, [pk(6 + g), 'rec8'], ['cat_a'])
            yield

        def stage_tail(n):
            xt = xtb[n % 2]
            xk = f'xt{n % 2}'
            dma('sp', xt, x_d[n * 128:(n + 1) * 128, :], [], [xk])
            dma('sp', cat[:, 512:1024], ret_d[n * 128:(n + 1) * 128, :], [f'retd{n}'], ['cat_r'])
            for c in range(8):
                tp(bankb(0)[:, c * 128:(c + 1) * 128], cat[:, c * 128:(c + 1) * 128], identb, ['cat_a', 'cat_r', 'identb'], [pk(0)])
            cp('dve', catT, bankb(0), [pk(0)], ['catT'])
            yield
            Mp = PS[0][:, :]
            mk = [pk(0), pk(1)]
            for half in range(2):
                for kc in range(8):
                    mm(Mp[:, half * 512:(half + 1) * 512], catT[:, kc * 128:(kc + 1) * 128], woutv[:, kc, half * 512:(half + 1) * 512],
                       kc == 0, kc == 7, ['catT', f'wout{kc}'], mk)
            tt('dve', tmp, Mp, GA1, ALU.mult, mk + ['modB'], ['tmp'])
            stt('dve', yv, xt, ALPHA, tmp, ALU.mult, ALU.add, [xk, 'tmp'], ['yv'])
            yield
            layer_norm('dve', yv, LN1G, LN1B, x1, ['yv'], 'x1', 'LN1G', 'LN1B')
            dma('sp', x1_d[n * 128:(n + 1) * 128, :], x1, ['x1'], [f'x1d{n}'])
            dbg_out("d_x1", x1, ['x1'], rows=(n * 128, (n + 1) * 128))
            yield
            tt('dve', tmp, x1, SCF1, ALU.mult, ['x1', 'modB'], ['tmp'])
            tt('dve', h2, tmp, SHF, ALU.add, ['tmp', 'modB'], ['h2'])
            for c in range(8):
                tp(bankb(1)[:, c * 128:(c + 1) * 128], h2[:, c * 128:(c + 1) * 128], identb, ['h2', 'identb'], [pk(1)])
            cp('dve', h2T, bankb(1), [pk(1)], ['h2T'])
            yield
            for kc in range(8):
                mm(bank(0)[:, 0:NE], h2T[:, kc * 128:(kc + 1) * 128], wrv[:, kc, :], kc == 0, kc == 7, ['h2T', 'wrb'], [pk(0)])
            tt('dve', lg, bank(0)[:, 0:NE], BR, ALU.add, [pk(0), 'BR'], ['lg'])
            op('dve', 'max', ['lg'], ['mx8'], out=mx8, in_=lg)
            ts('dve', nm, mx8[:, 0:1], -1.0, ALU.mult, ['mx8'], ['nm'])
            memset('dve', sm, 0.0, [], ['sm'])
            act(ex4, mx8[:, 0:4], AF.Exp, ['mx8', 'nm', 'sm'], ['ex4', 'sm'], bias=nm[:, 0:1], scale=1.0, accum=sm)
            op('dve', 'reciprocal', ['sm'], ['rsm'], out=rsm, in_=sm)
            ts('dve', GATES[:, n * 4:(n + 1) * 4], ex4, rsm[:, 0:1], ALU.mult, ['ex4', 'rsm'], [f'GATES{n}'])
            yield
            ts('dve', selb, lg, mx8[:, 3:4], ALU.is_ge, ['lg', 'mx8'], ['selb'])
            mm(bank(0)[:, 64:64 + NE], UT, selb, True, True, ['UT', 'selb'], [pk(0)])
            mm(bank(0)[:, 128:128 + NE], ONEb, selb, True, True, ['ONEb', 'selb'], [pk(0)])
            tt('dve', pos, bank(0)[:, 64:64 + NE], carry, ALU.add, [pk(0), 'carry'], ['pos'])
            tt('dve', carry, carry, bank(0)[:, 128:128 + NE], ALU.add, [pk(0), 'carry'], ['carry'])
            yield
            stt('dve', flat, pos, float(CAP - 1), EC, ALU.min, ALU.add, ['pos', 'EC'], ['flat'])
            memset('dve', idxf, 0.0, [], ['idxf'])
            for k in range(4):
                stt('dve', ohj, lg, mx8[:, k:k + 1], flat, ALU.is_equal, ALU.mult, ['lg', 'mx8', 'flat', 'idxf'], ['ohj', 'idxf'],
                    accum=idxf[:, k:k + 1])
            cp('dve', IDXT[:, n * 4:(n + 1) * 4], idxf, ['idxf'], [f'IDX{n}'])
            for k in range(4):
                op('pool', 'indirect_dma_start', ['h2', f'IDX{n}'] + ZKEYS, [f'xgd{n}_{k}'], dma=True,
                   out=xg_d[:, :], out_offset=bass.IndirectOffsetOnAxis(ap=IDXT[:, n * 4 + k:n * 4 + k + 1], axis=0),
                   in_=h2, in_offset=None)
            yield

        def run_all(g):
            for _ in g:
                pass

        def nsteps_scores(n):
            return ((n + 1) * 128 + 511) // 512 + NIT + 3

        def interleave(items):
            done = [0] * len(items)
            while True:
                best = None
                for i, (g, q) in enumerate(items):
                    if done[i] >= q:
                        continue
                    frac = done[i] / q
                    if best is None or frac < best[0]:
                        best = (frac, i)
                if best is None:
                    break
                i = best[1]
                try:
                    next(items[i][0])
                except StopIteration:
                    pass
                done[i] += 1

        gens = {0: stage_scores(0)}
        prog_ = {0: 0}
        run_all(gens[0])
        prog_[0] = nsteps_scores(0) + 1
        if NT > 1:
            gens[1] = stage_scores(1)
            prog_[1] = 0
        for n in range(NT + 1):
            items = []
            if n < NT:
                items.append([stage_attn(n), n + 3])
            if n >= 1:
                items.append([stage_tail(n - 1), 9])
            if n + 1 < NT:
                tot = nsteps_scores(n + 1) + 1
                items.append([gens[n + 1], tot - prog_[n + 1]])
                prog_[n + 1] = tot
            if n + 2 < NT:
                gens[n + 2] = stage_scores(n + 2)
                half = max(1, int((nsteps_scores(n + 2) + 1) * float(os.environ.get('KQ', '0.33'))))
                items.append([gens[n + 2], half])
                prog_[n + 2] = half
            interleave(items)
        if "d_gates" in dbg_d:
            dma('sp', dbg_d["d_gates"][:, :], GATES, [f'GATES{n}' for n in range(NT)], [])
        if "d_idx" in dbg_d:
            cp('dve', tmp[:, 0:128], IDXT, [f'IDX{n}' for n in range(NT)], ['tmp'])
            dma('sp', dbg_d["d_idx"][:, :], tmp[:, 0:128], ['tmp'], [])
        S.barrier()
        off[0] = persist_small

        if stage == 'B2':
            S.enabled = False
        Wgu = [alloc(8 * 2048, BF16), alloc(8 * 2048, BF16)]
        Wdn = [alloc(8 * 1024, BF16)]
        XRb = [alloc(1024, BF16) for _ in range(NSLOT)]
        XT = alloc(8 * CAP, BF16)
        XTv = XT.rearrange("p (kc s) -> p kc s", s=CAP)
        Aact = alloc(8 * CAP, BF16)
        Av = Aact.rearrange("p (m s) -> p m s", s=CAP)
        BD = alloc(1024)
        BGU = alloc(NE * 16)
        gq = alloc(512)
        sgm = alloc(512)
        uq = alloc(512)
        tq = alloc(512)
        yo = [alloc(1024), alloc(1024)]
        dma('sp', BGU, bgu_d[:, :], [], ['BGU'])
        BGA = alloc(NE * 16)
        F7 = float(7.0 / (1.0 + np.exp(-1.702 * 7.0)))
        BGUv = BGU.rearrange("p (e c) -> p e c", c=16)
        BGAv = BGA.rearrange("p (e c) -> p e c", c=16)
        ts('dve', BGAv[:, :, 0:8], BGUv[:, :, 0:8], 1.702, ALU.mult, ['BGU'], ['BGA'])
        ts('dve', BGAv[:, :, 8:16], BGUv[:, :, 8:16], 1.0, ALU.add, ['BGU'], ['BGA'])

        STG = [alloc(2048) for _ in range(3)]
        chunks = []
        for kc_ in range(8):
            chunks.append(('g', 0, kc_))
        for q_ in range(4):
            chunks.append(('d', 0, q_))
        for ex_ in range(NE):
            for m_ in range(8):
                if ex_ >= 1 and m_ < 4:
                    chunks.append(('d', ex_, m_))
                if ex_ + 1 < NE:
                    chunks.append(('g', ex_ + 1, m_))
        issued = [0]

        def issue_next():
            i = issued[0]
            if i >= len(chunks):
                return
            issued[0] += 1
            kind, ex_, c_ = chunks[i]
            st = STG[i % 3]
            if kind == 'g':
                dma('act', st, wgu_d[ex_, c_ * 128:(c_ + 1) * 128, :], [], [f'stg{i % 3}'])
            else:
                dma('act', st.rearrange("p (m n) -> p m n", n=1024),
                    wd_d[ex_, 2 * c_ * 128:(2 * c_ + 2) * 128, :].rearrange("(m p) n -> p m n", p=128), [], [f'stg{i % 3}'])

        casted = [0]

        def cast_next():
            i = casted[0]
            casted[0] += 1
            kind, ex_, c_ = chunks[i]
            st = STG[i % 3]
            if kind == 'g':
                dst = Wgu[ex_ % 2].rearrange("p (kc n) -> p kc n", n=2048)[:, c_, :]
                cp('act', dst, st, [f'stg{i % 3}'], [f'Wgu{ex_ % 2}_{c_}'])
            else:
                dst = Wdn[0][:, 2 * c_ * 1024:(2 * c_ + 2) * 1024]
                cp('act', dst, st, [f'stg{i % 3}'], [f'Wdn_{2 * c_}', f'Wdn_{2 * c_ + 1}'])
            issue_next()

        for _ in range(3):
            issue_next()
        for _ in range(12):
            cast_next()
        HALVES = [(0, 512), (512, CAP - 512)] if CAP > 512 else [(0, CAP)]

        def load_xr(ex):
            for a in range(NSLOT):
                dma('sp', XRb[a], xg_d[ex * CAP + a * 128:ex * CAP + (a + 1) * 128, :], [], [f'XR{a}'])

        def prep_xt(ex):
            for a in range(NSLOT):
                b = a % 2
                XR = XRb[a]
                for kc in range(8):
                    tp(bankb(b)[:, kc * 128:(kc + 1) * 128], XR[:, kc * 128:(kc + 1) * 128], identb,
                       [f'XR{a}', 'identb'], [pk(b)])
                cp('dve', XTv[:, :, a * 128:(a + 1) * 128], bankb(b).rearrange("p (kc s) -> p kc s", s=128),
                   [pk(b)], ['XT'])

        load_xr(0)
        prep_xt(0)
        for ex in range(NE):
            eb = ex % 2
            Wg = Wgu[eb]
            Wgv = Wg.rearrange("p (kc n) -> p kc n", n=2048)
            Wd = Wdn[0]
            Wdv = Wd.rearrange("p (kc n) -> p kc n", n=1024)
            dma('sp', BD, bd_d[ex:ex + 1, :].partition_broadcast(128), [], ['BD'])
            for m in range(8):
                if ex >= 1 and m < 4:
                    cast_next()
                if ex + 1 < NE:
                    cast_next()
                bg = BGA[:, ex * 16 + m:ex * 16 + m + 1]
                bu = BGA[:, ex * 16 + 8 + m:ex * 16 + 8 + m + 1]
                for hh, (h0_, hw2) in enumerate(HALVES):
                    cs_ = slice(h0_, h0_ + hw2)
                    gb = 2 + hh % 2
                    ub = 4 + hh % 2
                    for (bb_, c0) in ((gb, m * 128), (ub, 1024 + m * 128)):
                        for kc in range(8):
                            mm(bank(bb_)[:, 0:hw2], Wgv[:, kc, c0:c0 + 128], XTv[:, kc, cs_], kc == 0, kc == 7,
                               [f'Wgu{eb}_{kc}', 'XT'], [pk(bb_)])
                    act(sgm[:, 0:hw2], bank(gb)[:, 0:hw2], AF.Silu, [pk(gb), 'BGA'], ['sgm'], bias=bg, scale=1.702)
                    ts('dve', uq[:, 0:hw2], bank(ub)[:, 0:hw2], bu, ALU.add, [pk(ub), 'BGA'], ['uq'], s2=8.0, op1=ALU.min)
                    ts('dve', tq[:, 0:hw2], sgm[:, 0:hw2], 1.0 / 1.702, ALU.mult, ['sgm'], ['tq'], s2=F7, op1=ALU.min)
                    stt('dve', Av[:, m, cs_], uq[:, 0:hw2], -6.0, tq[:, 0:hw2], ALU.max, ALU.mult, ['uq', 'tq'], ['Aact'])
                if m == 0 and ex + 1 < NE:
                    load_xr(ex + 1)
            if ex + 1 < NE:
                prep_xt(ex + 1)
            for a in range(NSLOT):
                Yp = PS[3 if a % 2 == 0 else 0][:, :]
                yk = [pk(6), pk(7)] if a % 2 == 0 else [pk(0), pk(1)]
                for half in range(2):
                    for m in range(8):
                        mm(Yp[:, half * 512:(half + 1) * 512], Av[:, m, a * 128:(a + 1) * 128], Wdv[:, m, half * 512:(half + 1) * 512],
                           m == 0, m == 7, ['Aact', f'Wdn_{m}'], yk)
                tt('dve', yo[a % 2], Yp, BD, ALU.add, yk + ['BD'], [f'yo{a % 2}'])
                dma('sp', y_d[ex * CAP + a * 128:ex * CAP + (a + 1) * 128, :], yo[a % 2], [f'yo{a % 2}'], [f'yd{ex}_{a}'])
        S.barrier()
        off[0] = persist_small

        if stage == 'C':
            S.enabled = False
        YG2 = [[alloc(1024) for _ in range(4)] for _ in range(2)]
        x1b = [alloc(1024), alloc(1024)]
        accb = alloc(1024)
        tmp = alloc(1024)
        ob = [alloc(1024), alloc(1024)]
        LN2G = alloc(1024)
        LN2B = alloc(1024)
        lst = alloc(12)
        lmv = alloc(2)
        lsd = alloc(1)
        lrs = alloc(1)
        dma('sp', LN2G, ln2g_d.partition_broadcast(128), [], ['LN2G'])
        dma('sp', LN2B, ln2b_d.partition_broadcast(128), [], ['LN2B'])
        for n in range(NT):
            x1t = x1b[n % 2]
            YG = YG2[n % 2]
            dma('sp', x1t, x1_d[n * 128:(n + 1) * 128, :], [f'x1d{n}'], [f'x1b{n % 2}'])
            for k in range(4):
                op('pool', 'indirect_dma_start', [f'IDX{n}'], [f'YG{n % 2}_{k}'], dma=True,
                   out=YG[k], out_offset=None, in_=y_d[:, :],
                   in_offset=bass.IndirectOffsetOnAxis(ap=IDXT[:, n * 4 + k:n * 4 + k + 1], axis=0))
            act(accb, YG[0], AF.Identity, [f'YG{n % 2}_0', f'GATES{n}'], ['accb'], bias=0.0, scale=GATES[:, n * 4:n * 4 + 1])
            for k in range(1, 4):
                stt('dve', accb, YG[k], GATES[:, n * 4 + k:n * 4 + k + 1], accb, ALU.mult, ALU.add, [f'YG{n % 2}_{k}', f'GATES{n}', 'accb'], ['accb'])
            if "d_ff" in dbg_d:
                dma('sp', dbg_d["d_ff"][n * 128:(n + 1) * 128, :], accb, ['accb'], [])
            tt('dve', tmp, accb, GF1, ALU.mult, ['accb', 'modB'], ['tmp'])
            stt('dve', tmp, x1t, ALPHA, tmp, ALU.mult, ALU.add, [f'x1b{n % 2}', 'tmp'], ['tmp'])
            layer_norm('dve', tmp, LN2G, LN2B, ob[n % 2], ['tmp'], f'ob{n % 2}', 'LN2G', 'LN2B')
            dma('sp', out_d[n * 128:(n + 1) * 128, :], ob[n % 2], [f'ob{n % 2}'], [f'outd{n}'])

        sems = {}
        for e in ENGS:
            sems[('e', e)] = es.enter_context(nc.semaphore(f"se_{e}"))
        for k in S.dma_cnt:
            sems[k] = es.enter_context(nc.semaphore(f"sd_{k[1]}_{k[2]}"))
        final = S.all_tokens()
        blk = es.enter_context(nc.Block())
        bmap = {'pe': blk.tensor, 'act': blk.scalar, 'dve': blk.vector, 'pool': blk.gpsimd, 'sp': blk.sync}

        def make(eng):
            def body(e):
                for waits, fn, tok, inc in S.prog[eng]:
                    for k, v in waits:
                        e.wait_ge(sems[k], v)
                    inst = fn(e)
                    inst.then_inc(sems[tok[0]], inc)
                if eng == 'sp':
                    for k, v in final.items():
                        e.wait_ge(sems[k], v)
            return body
        for eng in ENGS:
            bmap[eng](make(eng))
    return nc


def host_consts():
    f32 = np.float32
    s = np.arange(128)[:, None]
    t = np.arange(128)[None, :]
    gam = (1.0 - 2.0 ** (-5.0 - np.arange(4))).astype(np.float64)
    dtab = np.zeros((128, 4, 128), f32)
    qd = np.zeros((128, 4, 128), f32)
    chtab = np.zeros((128, 4, 128), f32)
    kd = np.zeros((128, 4), f32)
    for h in range(4):
        diff = (t - s)
        dtab[:, h, :] = np.where(diff >= 0, gam[h] ** np.maximum(diff, 0), 0.0) / np.sqrt(128.0)
        qd[:, h, :] = (gam[h] ** (np.arange(128) + 1.0))[None, :]
        chtab[:, h, :] = gam[h] ** 128.0
        kd[:, h] = gam[h] ** (127.0 - np.arange(128)) / np.sqrt(128.0)
    inv = 1.0 / (10000.0 ** (np.arange(0, 128, 2, dtype=np.float32) / 128.0))
    ang = np.arange(L, dtype=np.float32)[:, None] * inv[None, :].astype(np.float32)
    cos = np.cos(ang).astype(f32)
    sin = np.sin(ang).astype(f32)
    negd = np.where(t > s, -1e30, 0.0).astype(f32)
    negd = np.where(np.arange(128)[None, :] > np.arange(128)[:, None], -1e30, 0.0).astype(f32)
    ut = (np.arange(128)[:, None] < np.arange(128)[None, :]).astype(f32)
    pow2 = np.tile((2.0 ** -np.arange(NIT + 2)).astype(f32)[None, :], (128, 1))
    ec = np.tile((np.arange(NE) * CAP).astype(f32)[None, :], (128, 1))
    ident = np.eye(128, dtype=f32)
    dist0 = t - s
    dist1 = t - s + 128
    b0 = t5_bucket_np(dist0)
    b1 = t5_bucket_np(dist1)
    return dict(dtab=dtab.reshape(128, 512), qdtab=qd.reshape(128, 512), chtab=chtab.reshape(128, 512), kdtab=kd,
                cos=cos, sin=sin, negd=negd, ut=ut, pow2=pow2, ec=ec, ident=ident), b0, b1, (dist0 >= 0)


_PERM = None


def _perm():
    idx = []
    for hh in [0, 4, 1, 5, 2, 6, 3, 7]:
        idx += list(range(hh * 64, hh * 64 + 64))
    idx += list(range(512, 640))
    idx += list(range(768, 1024))
    idx += list(range(1024, 1088)) * 2
    idx += list(range(640, 768))
    idx += list(range(1088, 1092))
    idx += list(range(1092, 2116))
    idx += list(range(2116, 2628))
    idx += list(range(2628, 3140))
    assert len(idx) == NCOL
    return np.array(idx)


def make_in_maps(inputs, cores):
    f32 = np.float32
    consts, b0, b1, causal = host_consts()
    rel_bias = np.asarray(inputs["rel_bias"], f32)
    bt = np.zeros((128, 2, 8, 128), f32)
    g0 = rel_bias[b0]
    g1 = rel_bias[b1]
    bt[:, 0] = np.where(causal[:, None, :], np.transpose(g0, (0, 2, 1)), 0.0)
    bt[:, 1] = np.transpose(g1, (0, 2, 1))
    c31 = np.tile(rel_bias[31][None, :], (128, 1)).astype(f32)
    w_in = np.ascontiguousarray(np.asarray(inputs["w_in"][0], f32)[:, _perm()])
    bgu = np.ascontiguousarray(np.asarray(inputs["b_gate_up"][0], f32).reshape(NE, 16, 128).transpose(2, 0, 1)).reshape(128, NE * 16)
    shared = dict(
        w_ada=np.ascontiguousarray(inputs["w_ada"][0], dtype=f32), b_ada=np.ascontiguousarray(inputs["b_ada"], dtype=f32).reshape(1, -1),
        w_in=w_in, w_out=np.ascontiguousarray(inputs["w_out"][0], dtype=f32),
        bt=bt.reshape(128, 2048), c31=c31,
        rng=np.asarray(inputs["ret_norm_g"], f32).reshape(1, 512),
        ln1g=np.asarray(inputs["ln1_g"], f32).reshape(1, D), ln1b=np.asarray(inputs["ln1_b"], f32).reshape(1, D),
        ln2g=np.asarray(inputs["ln2_g"], f32).reshape(1, D), ln2b=np.asarray(inputs["ln2_b"], f32).reshape(1, D),
        wr=np.ascontiguousarray(inputs["w_router"][0], dtype=f32), br=np.asarray(inputs["b_router"], f32).reshape(1, NE),
        wgu=np.ascontiguousarray(inputs["w_gate_up"][0], dtype=f32), bgu=bgu,
        wd=np.ascontiguousarray(inputs["w_down"][0], dtype=f32), bd=np.ascontiguousarray(inputs["b_down"][0], dtype=f32),
    )
    shared.update(consts)
    maps = []
    for b in cores:
        m = dict(shared)
        m["x"] = np.ascontiguousarray(inputs["x"][b], dtype=f32)
        m["c8"] = np.ascontiguousarray(np.asarray(inputs["c"][b], f32).reshape(8, 128).T)
        maps.append(m)
    return maps


def kernel(**inputs):
    nc = build_nc()
    maps = make_in_maps(inputs, list(range(8)))
    res = run_bass_kernel_spmd(nc, maps, core_ids=list(range(8)))
    out = np.stack([np.asarray(r["out"], np.float32) for r in res.results], axis=0)
    return out
```
